# Optimizing a Trainium2 kernel written in Bass

```python
import jax, jax.numpy as jnp
from jax import lax
import numpy as np

D_MODEL = 1024
BATCH = 8
SEQ = 2048
DEPTH = 2

HEAD_DIM = 64
NORM_EPS = 1e-6
D_FF = 2752
A_HEADS = 6
A_WIDTH = 384
A_DECAY_LORA = 32
A_AAA_LORA = 32
A_GATE_LORA = 64
A_MV_LORA = 16
A_GN_EPS = 64e-5
A_SLAB = (384, 384, 384, 32, 32, 64)
A_IN = 1280
B_HEADS = 6
B_WIDTH = 384
B_KV_LATENT = 128
B_IDX_HEADS = 8
B_IDX_DIM = 64
B_TOPK_MAX = 256
B_QUERY_BLOCK = 128
B_SLAB = (384, 128, 512, 64, 8)
B_IN = 1096
C_HEADS = 4
C_WIDTH = 256
C_CHUNK = 128
C_ROPE_BASE = 10000.0
C_SLAB = (256, 256, 256, 256)
C_IN = 1024
D_MIX = 1024
N_IN = 3400

kernel_name = 'hybrid_rwkv7_dsa_retention_macaron'


def _split(p, widths):
    out, start = [], 0
    for w in widths:
        out.append(p[..., start:start + w])
        start += w
    return out


def rmsnorm(x, g, eps=NORM_EPS):
    xf = x.astype(jnp.float32)
    y = xf * lax.rsqrt(jnp.mean(xf * xf, axis=-1, keepdims=True) + eps)
    return y.astype(x.dtype) * g


def swiglu(h, w_gate, w_up, w_down):
    return (jax.nn.silu(h @ w_gate) * (h @ w_up)) @ w_down


def shift_lerp(p, mu):
    prev = jnp.pad(p, ((0, 0), (1, 0), (0, 0)))[:, :-1]
    return p + (prev - p) * mu


def _head_layernorm(y, eps):
    mu = jnp.mean(y, axis=-1, keepdims=True)
    var = jnp.mean(jnp.square(y - mu), axis=-1, keepdims=True)
    return (y - mu) * lax.rsqrt(var + eps)


def _rwkv7_recurrence(r, decay, k, v, a, b):
    Bsz, T, H, N = r.shape

    def step(S, inp):
        r_t, w_t, k_t, v_t, a_t, b_t = inp
        sa = jnp.einsum('bhvk,bhk->bhv', S, a_t)
        S = S * w_t[:, :, None, :] + sa[..., None] * b_t[:, :, None, :] + v_t[..., None] * k_t[:, :, None, :]
        return S, jnp.einsum('bhvk,bhk->bhv', S, r_t)

    xs = tuple(jnp.moveaxis(z, 1, 0) for z in (r, decay, k, v, a, b))
    _, y = lax.scan(step, jnp.zeros((Bsz, H, N, N), jnp.float32), xs)
    return jnp.moveaxis(y, 0, 1)


def rwkv7_mixer(r, k, v, w_lo, a_lo, g_lo, w0, w2, a0, a2, g2, k_k, k_a, r_k, ln_w, ln_b):
    Bsz, T, _ = r.shape
    dt = r.dtype
    heads = lambda z: z.astype(jnp.float32).reshape(Bsz, T, A_HEADS, HEAD_DIM)
    w = -jax.nn.softplus(-(w0 + jnp.tanh(w_lo) @ w2)) - 0.5
    a = jax.nn.sigmoid(a0 + a_lo @ a2)
    g = jax.nn.sigmoid(g_lo) @ g2
    kk = heads(k * k_k)
    kk = kk / jnp.maximum(jnp.linalg.norm(kk, axis=-1, keepdims=True), 1e-12)
    k = k * (1.0 + (a - 1.0) * k_a)
    decay = jnp.exp(-jnp.exp(heads(w)))
    rh, kh, vh, ah = heads(r), heads(k), heads(v), heads(a)
    y = _rwkv7_recurrence(rh, decay, kh, vh, -kk, kk * ah)
    y = _head_layernorm(y, A_GN_EPS) * ln_w.reshape(A_HEADS, HEAD_DIM) + ln_b.reshape(A_HEADS, HEAD_DIM)
    y = y + jnp.sum(rh * kh * r_k, axis=-1, keepdims=True) * vh
    return y.reshape(Bsz, T, A_WIDTH).astype(dt) * g


def dsa_mixer(q, c_kv, iq, ik, iw, kv_norm, w_uk, w_uv):
    Bsz, T, _ = q.shape
    n_keys = T
    topk = min(B_TOPK_MAX, n_keys // 4)
    nb = T // B_QUERY_BLOCK
    c = rmsnorm(c_kv, kv_norm)
    qh = q.reshape(Bsz, T, B_HEADS, HEAD_DIM)
    q_lat = jnp.einsum('bthd,hdc->bthc', qh, w_uk) * HEAD_DIM ** -0.5
    iqh = iq.reshape(Bsz, T, B_IDX_HEADS, B_IDX_DIM) * B_IDX_DIM ** -0.5
    iwh = iw * B_IDX_HEADS ** -0.5
    key_pos = jnp.arange(n_keys)
    blocks = lambda z: jnp.moveaxis(z.reshape((Bsz, nb, B_QUERY_BLOCK) + z.shape[2:]), 1, 0)

    def one_block(args):
        q_lat_b, iq_b, iw_b, blk = args
        q_pos = blk * B_QUERY_BLOCK + jnp.arange(B_QUERY_BLOCK)
        rel = jax.nn.relu(jnp.einsum('bqhd,bsd->bqhs', iq_b, ik))
        score = jnp.einsum('bqh,bqhs->bqs', iw_b, rel).astype(jnp.float32)
        visible = key_pos[None, :] <= q_pos[:, None]
        score = jnp.where(visible[None], score, -jnp.inf)
        _, idx = lax.top_k(score, topk)
        valid = idx <= q_pos[None, :, None]
        c_sel = jax.vmap(lambda cb, ib: cb[ib])(c, idx)
        logits = jnp.einsum('bqhc,bqkc->bqhk', q_lat_b, c_sel).astype(jnp.float32)
        logits = jnp.where(valid[:, :, None, :], logits, -jnp.inf)
        prob = jax.nn.softmax(logits, axis=-1).astype(c.dtype)
        o_lat = jnp.einsum('bqhk,bqkc->bqhc', prob, c_sel)
        return jnp.einsum('bqhc,hcd->bqhd', o_lat, w_uv)

    out = lax.map(one_block, (blocks(q_lat), blocks(iqh), blocks(iwh), jnp.arange(nb)))
    return jnp.moveaxis(out, 0, 1).reshape(Bsz, T, B_WIDTH)


def _retnet_rotate(z):
    T = z.shape[1]
    half = HEAD_DIM // 2
    theta = C_ROPE_BASE ** (-jnp.linspace(0.0, 1.0, half, dtype=jnp.float32))
    ang = jnp.arange(T, dtype=jnp.float32)[:, None] * theta[None, :]
    cos, sin = jnp.cos(ang)[None, :, None, :], jnp.sin(ang)[None, :, None, :]
    z1, z2 = z[..., :half], z[..., half:]
    return jnp.concatenate([z1 * cos - z2 * sin, z1 * sin + z2 * cos], axis=-1)


def retention_mixer(q, k, v, g):
    Bsz, T, _ = q.shape
    nc = T // C_CHUNK
    dt = q.dtype
    f32 = jnp.float32
    heads = lambda z: z.astype(f32).reshape(Bsz, T, C_HEADS, HEAD_DIM)
    qh = _retnet_rotate(heads(q))
    kh = _retnet_rotate(heads(k)) * HEAD_DIM ** -0.5
    vh = heads(v)
    log_gamma = jnp.log(1.0 - 2.0 ** (-5.0 - jnp.arange(C_HEADS, dtype=f32)))
    n = jnp.arange(C_CHUNK, dtype=f32)
    diff = n[:, None] - n[None, :]
    intra = jnp.where(diff[None] >= 0, jnp.exp(jnp.maximum(diff, 0.0)[None] * log_gamma[:, None, None]), 0.0)
    q_decay = jnp.exp((n[:, None] + 1.0) * log_gamma[None, :])
    k_decay = jnp.exp((C_CHUNK - 1.0 - n)[:, None] * log_gamma[None, :])
    chunk_decay = jnp.exp(C_CHUNK * log_gamma)
    chunks = lambda z: jnp.moveaxis(z.reshape(Bsz, nc, C_CHUNK, C_HEADS, HEAD_DIM), 1, 0)

    def step(state, inp):
        qc, kc, vc = inp
        scores = jnp.einsum('bnhd,bmhd->bhnm', qc, kc) * intra[None]
        inner = jnp.einsum('bhnm,bmhe->bnhe', scores, vc)
        cross = jnp.einsum('bnhd,bhde->bnhe', qc, state) * q_decay[None, :, :, None]
        state = state * chunk_decay[None, :, None, None] + jnp.einsum('bmhd,bmhe->bhde', kc * k_decay[None, :, :, None], vc)
        return state, inner + cross

    state0 = jnp.zeros((Bsz, C_HEADS, HEAD_DIM, HEAD_DIM), f32)
    _, y = lax.scan(step, state0, (chunks(qh), chunks(kh), chunks(vh)))
    y = jnp.moveaxis(y, 0, 1).reshape(Bsz, T, C_HEADS, HEAD_DIM)
    y = y * lax.rsqrt(jnp.mean(y * y, axis=-1, keepdims=True) + NORM_EPS)
    return jax.nn.silu(g) * y.reshape(Bsz, T, C_WIDTH).astype(dt)


def setup_inputs(seed: int = 0) -> dict:
    key = jax.random.key(seed)
    counter = [0]

    def nk():
        counter[0] += 1
        return jax.random.fold_in(key, counter[0])

    def nrm(shape, scale):
        return jax.random.normal(nk(), shape, jnp.float32) * scale

    def gain(shape):
        return 1.0 + nrm(shape, 0.02)

    L = DEPTH
    return {
        'x': nrm((BATCH, SEQ, D_MODEL), 1.0),
        'ffn1_norm': gain((L, D_MODEL)),
        'ffn1_w_gate': nrm((L, D_MODEL, D_FF), D_MODEL ** -0.5),
        'ffn1_w_up': nrm((L, D_MODEL, D_FF), D_MODEL ** -0.5),
        'ffn1_w_down': nrm((L, D_FF, D_MODEL), D_FF ** -0.5),
        'mix_norm': gain((L, D_MODEL)),
        'w_in': nrm((L, D_MODEL, N_IN), D_MODEL ** -0.5),
        'w_out': nrm((L, D_MIX, D_MODEL), D_MIX ** -0.5),
        'rwkv_mu': jax.random.uniform(nk(), (L, A_IN), jnp.float32),
        'rwkv_w0': jnp.linspace(-6.0, -1.0, A_WIDTH, dtype=jnp.float32)[None, :] + nrm((L, A_WIDTH), 0.1),
        'rwkv_w2': nrm((L, A_DECAY_LORA, A_WIDTH), 0.1),
        'rwkv_a0': nrm((L, A_WIDTH), 0.1),
        'rwkv_a2': nrm((L, A_AAA_LORA, A_WIDTH), 0.1),
        'rwkv_g2': nrm((L, A_GATE_LORA, A_WIDTH), A_GATE_LORA ** -0.5),
        'rwkv_k_k': 0.85 + nrm((L, A_WIDTH), 0.02),
        'rwkv_k_a': 1.0 + nrm((L, A_WIDTH), 0.02),
        'rwkv_r_k': nrm((L, A_HEADS, HEAD_DIM), 0.1),
        'rwkv_ln_w': gain((L, A_WIDTH)),
        'rwkv_ln_b': nrm((L, A_WIDTH), 0.01),
        'rwkv_vres_w_in': nrm((L - 1, D_MODEL, A_MV_LORA), D_MODEL ** -0.5),
        'rwkv_vres_mu': jax.random.uniform(nk(), (L - 1, A_MV_LORA), jnp.float32),
        'rwkv_v0': 0.5 + nrm((L - 1, A_WIDTH), 0.1),
        'rwkv_v2': nrm((L - 1, A_MV_LORA, A_WIDTH), 0.1),
        'dsa_kv_norm': gain((L, B_KV_LATENT)),
        'dsa_w_uk': nrm((L, B_HEADS, HEAD_DIM, B_KV_LATENT), B_KV_LATENT ** -0.5),
        'dsa_w_uv': nrm((L, B_HEADS, B_KV_LATENT, HEAD_DIM), B_KV_LATENT ** -0.5),
        'ffn2_norm': gain((L, D_MODEL)),
        'ffn2_w_gate': nrm((L, D_MODEL, D_FF), D_MODEL ** -0.5),
        'ffn2_w_up': nrm((L, D_MODEL, D_FF), D_MODEL ** -0.5),
        'ffn2_w_down': nrm((L, D_FF, D_MODEL), D_FF ** -0.5),
        'final_norm': gain((D_MODEL,)),
    }


def reference(x, ffn1_norm, ffn1_w_gate, ffn1_w_up, ffn1_w_down, mix_norm, w_in, w_out,
              rwkv_mu, rwkv_w0, rwkv_w2, rwkv_a0, rwkv_a2, rwkv_g2, rwkv_k_k, rwkv_k_a, rwkv_r_k,
              rwkv_ln_w, rwkv_ln_b, rwkv_vres_w_in, rwkv_vres_mu, rwkv_v0, rwkv_v2,
              dsa_kv_norm, dsa_w_uk, dsa_w_uv,
              ffn2_norm, ffn2_w_gate, ffn2_w_up, ffn2_w_down, final_norm):
    v_first = None
    for l in range(DEPTH):
        x = x + 0.5 * swiglu(rmsnorm(x, ffn1_norm[l]), ffn1_w_gate[l], ffn1_w_up[l], ffn1_w_down[l])
        h = rmsnorm(x, mix_norm[l])
        if l == 0:
            p_a, p_b, p_c = _split(h @ w_in[l], (A_IN, B_IN, C_IN))
        else:
            w_comb = jnp.concatenate([w_in[l], rwkv_vres_w_in[l - 1]], axis=1)
            p_a, p_b, p_c, p_mv = _split(h @ w_comb, (A_IN, B_IN, C_IN, A_MV_LORA))
        r, k, v, w_lo, a_lo, g_lo = _split(shift_lerp(p_a, rwkv_mu[l]), A_SLAB)
        if l == 0:
            v_first = v
        else:
            mv = shift_lerp(p_mv, rwkv_vres_mu[l - 1])
            v = v + (v_first - v) * jax.nn.sigmoid(rwkv_v0[l - 1] + mv @ rwkv_v2[l - 1])
        o_a = rwkv7_mixer(r, k, v, w_lo, a_lo, g_lo, rwkv_w0[l], rwkv_w2[l], rwkv_a0[l], rwkv_a2[l],
                          rwkv_g2[l], rwkv_k_k[l], rwkv_k_a[l], rwkv_r_k[l], rwkv_ln_w[l], rwkv_ln_b[l])
        q_b, c_b, iq_b, ik_b, iw_b = _split(p_b, B_SLAB)
        o_b = dsa_mixer(q_b, c_b, iq_b, ik_b, iw_b, dsa_kv_norm[l], dsa_w_uk[l], dsa_w_uv[l])
        q_c, k_c, v_c, g_c = _split(p_c, C_SLAB)
        o_c = retention_mixer(q_c, k_c, v_c, g_c)
        x = x + jnp.concatenate([o_a, o_b, o_c], axis=-1) @ w_out[l]
        x = x + 0.5 * swiglu(rmsnorm(x, ffn2_norm[l]), ffn2_w_gate[l], ffn2_w_up[l], ffn2_w_down[l])
    return rmsnorm(x, final_norm)
```

```python
import math
from contextlib import ExitStack
import numpy as np
import concourse.bass as bass
import concourse.mybir as mybir
from concourse.bass_utils import run_bass_kernel_spmd

F32 = mybir.dt.float32
BF16 = mybir.dt.bfloat16
ALU = mybir.AluOpType
AF = mybir.ActivationFunctionType
AX = mybir.AxisListType

ENG = ('pe', 'act', 'dve', 'pool', 'sp')

D = 1024
T = 2048
L = 2
DFF = 2752
DFFP = 2816
NFC = 22
NT = 512
NTB = T // NT
NM = 256
NMB = T // NM
NQ = NM // 128
EPS = 1e-6
GN_EPS = 64e-5
C0 = math.exp(-0.5)
NCH = 29


class Res:
    __slots__ = ('name', 'w', 'r')

    def __init__(self, name=''):
        self.name = name
        self.w = None
        self.r = []


class KB:
    def __init__(self, nc, same_engine_sync=True, dma_ring=8):
        self.nc = nc
        self.ops = {e: [] for e in ENG}
        self.cnt = {e: 0 for e in ENG}
        self.waited = {e: {} for e in ENG}
        self.same = same_engine_sync
        self.ring = dma_ring
        self.dma_n = {e: 0 for e in ENG}
        self.semh = {}
        self.semkeys = [('e', e) for e in ENG]
        for e in ('sp', 'pool', 'act'):
            for i in range(dma_ring):
                self.semkeys.append(('d', e, i))
        self.last_dma = {}
        self.n_wait = 0

    def _wait(self, eng, ev):
        if ev is None:
            return
        key, val = ev
        if key == ('e', eng) and (eng == 'pe' or not self.same):
            return
        cur = self.waited[eng].get(key, 0)
        if cur >= val:
            return
        self.waited[eng][key] = val
        self.n_wait += 1
        self.ops[eng].append(('wait', key, val))

    def _deps(self, eng, reads, writes):
        for r in reads:
            self._wait(eng, r.w)
        for w in writes:
            self._wait(eng, w.w)
            for ev in w.r:
                self._wait(eng, ev)

    def _commit(self, ev, reads, writes):
        for w in writes:
            w.w = ev
            w.r = []
        for r in reads:
            if r not in writes:
                r.r.append(ev)
                if len(r.r) > 64:
                    r.r = r.r[-64:] if False else r.r

    def op(self, eng, fn, reads=(), writes=()):
        reads = list(reads)
        writes = list(writes)
        self._deps(eng, reads, writes)
        self.cnt[eng] += 1
        ev = (('e', eng), self.cnt[eng])
        self.ops[eng].append(('op', fn, ('e', eng), 1))
        self._commit(ev, reads, writes)
        return ev

    def dma(self, q, fn, reads=(), writes=()):
        reads = list(reads)
        writes = list(writes)
        self._deps(q, reads, writes)
        j = self.dma_n[q]
        self.dma_n[q] += 1
        slot = j % self.ring
        key = ('d', q, slot)
        tgt = 16 * (j // self.ring + 1)
        if j >= self.ring:
            self._wait(q, (key, tgt - 16))
        ev = (key, tgt)
        self.last_dma[key] = ev
        self.ops[q].append(('op', fn, key, 16))
        self._commit(ev, reads, writes)
        return ev

    def wait_all(self, eng, evs):
        for ev in evs:
            self._wait(eng, ev)

    def barrier(self):
        evs = [(('e', e), self.cnt[e]) for e in ENG if self.cnt[e] > 0]
        evs += list(self.last_dma.values())
        for e in ENG:
            for ev in evs:
                self._wait(e, ev)

    def finalize(self, stack):
        nc = self.nc
        for kk in self.semkeys:
            self.semh[kk] = stack.enter_context(nc.semaphore('s_' + '_'.join(str(x) for x in kk)))
        block = stack.enter_context(nc.Block())
        semh = self.semh

        def run(eng_name):
            def body(eng):
                for it in self.ops[eng_name]:
                    if it[0] == 'wait':
                        eng.wait_ge(semh[it[1]], it[2])
                    else:
                        ins = it[1](eng)
                        ins.then_inc(semh[it[2]], it[3])
            return body

        block.tensor(run('pe'))
        block.scalar(run('act'))
        block.vector(run('dve'))
        block.gpsimd(run('pool'))
        block.sync(run('sp'))


class Arena:
    def __init__(self, ap, nwords):
        self.ap = ap
        self.n = nwords
        self.top = 0
        self.peak = 0

    def mark(self):
        return self.top

    def release(self, m):
        self.top = m

    def f32(self, n):
        a = self.ap[:, self.top:self.top + n]
        self.top += n
        self.peak = max(self.peak, self.top)
        assert self.top <= self.n, ("arena overflow", self.top, self.n)
        return a

    def bf16(self, n):
        w = (n + 1) // 2
        return self.f32(w).bitcast(BF16)[:, 0:n]


def win_chunks():
    ch = []
    for j in range(10):
        ch.append(('a%d' % j, list(range(j * 128, (j + 1) * 128))))
    ch.append(('mv', 'mv'))
    for j in range(3):
        ch.append(('bq%d' % j, list(range(1280 + j * 128, 1280 + (j + 1) * 128))))
    ch.append(('bc', list(range(1664, 1792))))
    for j in range(4):
        ch.append(('biq%d' % j, list(range(1792 + j * 128, 1792 + (j + 1) * 128))))
    ch.append(('bik2', list(range(2304, 2368)) * 2))
    ch.append(('biw', list(range(2368, 2376))))
    for n, base in (('cq', 2376), ('ck', 2632), ('cv', 2888), ('cg', 3144)):
        for j in range(2):
            ch.append((n + str(j), list(range(base + j * 128, base + (j + 1) * 128))))
    return ch


CHUNKS = win_chunks()
CIDX = {c[0]: i for i, c in enumerate(CHUNKS)}

VL = {}
_o = 0
for _n, _w in (('ffn1', 8), ('ffn2', 8), ('mix', 8), ('mu', 10), ('mumv', 1), ('w0', 3), ('a0', 3), ('kk', 3), ('ka', 3),
               ('rk', 3), ('lnw', 3), ('lnb', 3), ('v0', 3), ('kvn', 1)):
    VL[_n] = _o
    _o += _w
VLN = _o

CF = {}
_o = 0
for _n, _w in (('maskL', 384), ('maskU', 384), ('maskUi', 384), ('intraT', 512), ('prot', 128), ('reset', NM), ('cmask', 128),
               ('sel', 1024), ('qdec', 2 * NM), ('kdec', 2 * NM), ('cdec', 2), ('negbig', 1)):
    CF[_n] = _o
    _o += _w
CFN = _o
CB = {}
_o = 0
for _n, _w in (('ident', 768), ('bones', 128), ('bmean', 128), ('ones', 128)):
    CB[_n] = _o
    _o += _w
CBN = _o


class B:
    def __init__(self, nlayers=L, do_ffn=True, do_mix=True, dbg=None, mixers=('a', 'b', 'c')):
        self.nlayers = nlayers
        self.do_ffn = do_ffn
        self.do_mix = do_mix
        self.dbg = dbg or {}
        self.mixers = mixers

    def mm(self, out, lhsT, rhs, start=True, stop=True, rd=(), wr=()):
        return self.k.op('pe', lambda e: e.matmul(out, lhsT=lhsT, rhs=rhs, start=start, stop=stop), rd, wr)

    def act(self, out, in_, func, rd=(), wr=(), **kw):
        return self.k.op('act', lambda e: e.activation(out=out, in_=in_, func=func, **kw), rd, wr)

    def tt(self, eng, out, in0, in1, op, rd=(), wr=()):
        return self.k.op(eng, lambda e: e.tensor_tensor(out=out, in0=in0, in1=in1, op=op), rd, wr)

    def stt(self, eng, out, in0, scalar, in1, op0, op1, rd=(), wr=()):
        return self.k.op(eng, lambda e: e.scalar_tensor_tensor(out=out, in0=in0, scalar=scalar, in1=in1, op0=op0, op1=op1), rd, wr)

    def ts(self, eng, out, in0, s1, s2, op0, op1=None, rd=(), wr=()):
        if op1 is None:
            return self.k.op(eng, lambda e: e.tensor_scalar(out=out, in0=in0, scalar1=s1, scalar2=None, op0=op0), rd, wr)
        return self.k.op(eng, lambda e: e.tensor_scalar(out=out, in0=in0, scalar1=s1, scalar2=s2, op0=op0, op1=op1), rd, wr)

    def cp(self, eng, out, in_, rd=(), wr=()):
        if eng == 'act':
            return self.k.op('act', lambda e: e.activation(out=out, in_=in_, func=AF.Copy), rd, wr)
        return self.k.op(eng, lambda e: e.tensor_copy(out=out, in_=in_), rd, wr)

    def recip(self, out, in_, rd=(), wr=()):
        return self.k.op('dve', lambda e: e.reciprocal(out=out, in_=in_), rd, wr)

    def dma(self, q, out, in_, rd=(), wr=()):
        return self.k.dma(q, lambda e: e.dma_start(out=out, in_=in_), rd, wr)

    def nb(self):
        b = self.rr[self.rri % len(self.rr)]
        self.rri += 1
        return b

    def dump(self, name, ap, reads):
        if name not in self.dbg:
            return
        shape = list(ap.shape)
        d = self.nc.dram_tensor("dbg_" + name, shape, ap.dtype, kind="ExternalOutput").ap()
        ev = self.k.dma('sp', lambda e: e.dma_start(out=d, in_=ap), reads=reads)
        self.dbg_evs.append(ev)

    def build(self):
        nc = bass.Bass("TRN2", target_bir_lowering=False)
        self.nc = nc
        nl = self.nlayers
        dr = {}
        dr['xT'] = nc.dram_tensor("xT", [8, 128, T], F32, kind="ExternalInput").ap()
        dr['outT'] = nc.dram_tensor("outT", [8, 128, T], F32, kind="ExternalOutput").ap()
        dr['wgu'] = nc.dram_tensor("wgu", [nl * 2, 2, NFC, 128, 8 * 128], F32, kind="ExternalInput").ap()
        dr['wd'] = nc.dram_tensor("wd", [nl * 2, 8, 128, NFC * 128], F32, kind="ExternalInput").ap()
        dr['wgu_b'] = nc.dram_tensor("wgu_b", [nl * 2, 2, NFC, 128, 8 * 128], BF16, kind="Internal").ap()
        dr['wd_b'] = nc.dram_tensor("wd_b", [nl * 2, 8, 128, NFC * 128], BF16, kind="Internal").ap()
        dr['win'] = nc.dram_tensor("win", [nl, NCH, 128, 8 * 128], F32, kind="ExternalInput").ap()
        dr['win_b'] = nc.dram_tensor("win_b", [nl, NCH, 128, 8 * 128], BF16, kind="Internal").ap()
        dr['wout'] = nc.dram_tensor("wout", [nl, 8, 128, 8 * 128], F32, kind="ExternalInput").ap()
        dr['wout_b'] = nc.dram_tensor("wout_b", [nl, 8, 128, 8 * 128], BF16, kind="Internal").ap()
        dr['smw'] = nc.dram_tensor("smw", [nl, 128, 384 + 384 + 384 + 384], F32, kind="ExternalInput").ap()
        dr['vec'] = nc.dram_tensor("vec", [128, VLN * nl + 8], F32, kind="ExternalInput").ap()
        dr['cf'] = nc.dram_tensor("cf", [128, CFN], F32, kind="ExternalInput").ap()
        dr['cb'] = nc.dram_tensor("cb", [128, CBN], F32, kind="ExternalInput").ap()
        dr['rot'] = nc.dram_tensor("rot", [128, 4, T], F32, kind="ExternalInput").ap()
        dr['vf'] = nc.dram_tensor("vf", [128, 3, T], F32, kind="Internal").ap()
        self.dr = dr

        with ExitStack() as st:
            self.st = st
            k = KB(nc)
            self.k = k
            self.xT = st.enter_context(nc.sbuf_tensor("xT_sb", [128, 8, T], F32))
            self.xres = [[Res() for _ in range(NMB)] for _ in range(8)]
            self.vec = st.enter_context(nc.sbuf_tensor("vec_sb", [128, VLN * nl + 8], F32))
            self.rvec = Res()
            self.cf = st.enter_context(nc.sbuf_tensor("cf_sb", [128, CFN], F32))
            self.cb = st.enter_context(nc.sbuf_tensor("cb_sb", [128, CBN], BF16))
            self.rconst = Res()
            self.ones_b = self.cb[:, CB['ones']:CB['ones'] + 128]
            self.ident_b = self.cb[:, CB['ident']:CB['ident'] + 128]
            self.bones_b = self.cb[:, CB['bones']:CB['bones'] + 128]
            self.bmean_b = self.cb[:, CB['bmean']:CB['bmean'] + 128]
            rem = nc.sbuf_bytes_remaining
            AW = (rem - 2048) // 4
            arena_t = st.enter_context(nc.sbuf_tensor("arena", [128, AW], F32))
            self.ar = Arena(arena_t, AW)
            self.banks = [st.enter_context(nc.psum_tensor("bank%d" % i, [128, 512], F32)) for i in range(8)]
            self.bres = [Res() for _ in range(8)]
            self.rr = list(range(8))
            self.rri = 0
            self.dbg_evs = []
            self.vfres = [Res() for _ in range(NMB)]

            self.prologue()
            for l in range(nl):
                if self.do_ffn:
                    self.ffn_phase(l, 0)
                if self.do_mix:
                    self.mix_phase(l)
                self.cast_layer(l + 1)
                if self.do_ffn and not self.dbg.get('skip_ffn2'):
                    self.ffn_phase(l, 1)
            self.final_phase()
            k.finalize(st)
        return nc

    def vc(self, l, name, j=0):
        c = VLN * l + VL[name] + j
        return self.vec[:, c:c + 1]

    def xr(self, c, t0, n):
        return [self.xres[c][b] for b in range(t0 // NM, (t0 + n) // NM)]

    def prologue(self):
        k, nc, dr = self.k, self.nc, self.dr
        for c in range(8):
            self.dma('sp', self.xT[:, c, :], dr['xT'][c], wr=[self.xres[c][b] for b in range(NMB)])
        self.dma('act', self.vec[:], dr['vec'][:, :], wr=[self.rvec])
        self.dma('act', self.cf[:], dr['cf'][:, :], wr=[self.rconst])
        self.dma('pool', self.cb[:], dr['cb'][:, :], wr=[self.rconst])
        self.rwgu = {}
        self.rwd = {}
        self.rwin = {}
        self.rwout = {}
        self.cast_layer(0)

    def cast_layer(self, l):
        if l >= self.nlayers:
            return
        if self.do_ffn:
            self.cast_ffn(2 * l)
        if self.do_mix:
            self.cast_mix(l)
        if self.do_ffn and not self.dbg.get('skip_ffn2'):
            self.cast_ffn(2 * l + 1)

    def cast_ffn(self, i):
        dr = self.dr
        for gu in range(2):
            for fc in range(NFC):
                r = Res()
                self.rwgu[(i, gu, fc)] = r
                self.dma('pool', dr['wgu_b'][i, gu, fc], dr['wgu'][i, gu, fc], wr=[r])
        for dc in range(8):
            r = Res()
            self.rwd[(i, dc)] = r
            self.dma('pool', dr['wd_b'][i, dc].rearrange("p (a f) -> (p a) f", a=2),
                     dr['wd'][i, dc].rearrange("p (a f) -> (p a) f", a=2), wr=[r])

    def cast_mix(self, l):
        dr = self.dr
        for ci in range(NCH):
            r = Res()
            self.rwin[(l, ci)] = r
            self.dma('pool', dr['win_b'][l, ci], dr['win'][l, ci], wr=[r])
        for dc in range(8):
            r = Res()
            self.rwout[(l, dc)] = r
            self.dma('pool', dr['wout_b'][l, dc], dr['wout'][l, dc], wr=[r])

    def rmsnorm_to(self, t0, n, gcol, hT, hres, sq, sqres, rstd, rres):
        k = self.k
        ts_ = slice(t0, t0 + n)
        bank = self.nb()
        ps = self.banks[bank]
        for c in range(8):
            s = c % 2
            self.act(sq[s], self.xT[:, c, ts_], AF.Square, rd=self.xr(c, t0, n), wr=[sqres[s]])
            self.mm(ps[:, :n], self.ones_b, sq[s], start=(c == 0), stop=(c == 7), rd=[sqres[s], self.rconst], wr=[self.bres[bank]])
        self.act(rstd, ps[:, :n], AF.Sqrt, rd=[self.bres[bank]], wr=[rres], bias=EPS, scale=1.0 / D)
        self.recip(rstd, rstd, rd=[rres], wr=[rres])
        for c in range(8):
            self.stt('dve', hT[:, c, :], self.xT[:, c, ts_], self.vec[:, gcol + c:gcol + c + 1], rstd, ALU.mult, ALU.mult,
                     rd=self.xr(c, t0, n) + [rres, self.rvec], wr=[hres[c]])

    def ffn_phase(self, l, j):
        k, nc, dr, ar = self.k, self.nc, self.dr, self.ar
        i = 2 * l + j
        k.barrier()
        m = ar.mark()
        self.rr = [0]
        hT = ar.bf16(8 * NT).rearrange("p (c t) -> p c t", c=8)
        hres = [Res() for _ in range(8)]
        aT = ar.bf16(NFC * NT).rearrange("p (c t) -> p c t", c=NFC)
        ares = [Res() for _ in range(NFC)]
        sq = [ar.bf16(NT) for _ in range(2)]
        sqres = [Res(), Res()]
        rstd = ar.f32(NT)
        rres = Res()
        NW = 3
        wg = [ar.bf16(8 * 128).rearrange("p (c f) -> p c f", c=8) for _ in range(NW)]
        wu = [ar.bf16(8 * 128).rearrange("p (c f) -> p c f", c=8) for _ in range(NW)]
        wgres = [Res() for _ in range(NW)]
        wures = [Res() for _ in range(NW)]
        wd = [ar.bf16(NFC * 128).rearrange("p (c f) -> p c f", c=NFC) for _ in range(2)]
        wdres = [Res(), Res()]
        sg = [ar.bf16(NT) for _ in range(2)]
        sgres = [Res(), Res()]
        gcol = VLN * l + VL['ffn1' if j == 0 else 'ffn2']
        B_ = self.banks
        for tb in range(NTB):
            t0 = tb * NT
            ts_ = slice(t0, t0 + NT)
            self.rmsnorm_to(t0, NT, gcol, hT, hres, sq, sqres, rstd, rres)
            for fc in range(NFC):
                s = fc % NW
                self.dma('sp', wg[s], dr['wgu_b'][i, 0, fc].rearrange("p (c f) -> p c f", c=8), rd=[self.rwgu[(i, 0, fc)]], wr=[wgres[s]])
                self.dma('sp', wu[s], dr['wgu_b'][i, 1, fc].rearrange("p (c f) -> p c f", c=8), rd=[self.rwgu[(i, 1, fc)]], wr=[wures[s]])
                gb = 1 + fc % 2
                ub = 3 + fc % 2
                for kc in range(8):
                    self.mm(B_[gb][:, :NT], wg[s][:, kc, :], hT[:, kc, :], start=(kc == 0), stop=(kc == 7),
                            rd=[wgres[s], hres[kc]], wr=[self.bres[gb]])
                for kc in range(8):
                    self.mm(B_[ub][:, :NT], wu[s][:, kc, :], hT[:, kc, :], start=(kc == 0), stop=(kc == 7),
                            rd=[wures[s], hres[kc]], wr=[self.bres[ub]])
                s2 = fc % 2
                self.act(sg[s2], B_[gb][:, :NT], AF.Silu, rd=[self.bres[gb]], wr=[sgres[s2]])
                self.tt('dve', aT[:, fc, :], B_[ub][:, :NT], sg[s2], ALU.mult, rd=[self.bres[ub], sgres[s2]], wr=[ares[fc]])
            for dc in range(8):
                s = dc % 2
                self.dma('sp', wd[s], dr['wd_b'][i, dc].rearrange("p (c f) -> p c f", c=NFC), rd=[self.rwd[(i, dc)]], wr=[wdres[s]])
                yb = 5 + dc % 2
                for fc in range(NFC):
                    self.mm(B_[yb][:, :NT], wd[s][:, fc, :], aT[:, fc, :], start=(fc == 0), stop=(fc == NFC - 1),
                            rd=[wdres[s], ares[fc]], wr=[self.bres[yb]])
                self.stt('dve', self.xT[:, dc, ts_], B_[yb][:, :NT], 0.5, self.xT[:, dc, ts_], ALU.mult, ALU.add,
                         rd=[self.bres[yb]] + self.xr(dc, t0, NT), wr=self.xr(dc, t0, NT))
        ar.release(m)

    def final_phase(self):
        k, nc, dr, ar = self.k, self.nc, self.dr, self.ar
        k.barrier()
        m = ar.mark()
        self.rr = [0, 1]
        sq = [ar.bf16(NT) for _ in range(2)]
        sqres = [Res(), Res()]
        rstd = ar.f32(NT)
        rres = Res()
        o = [ar.f32(8 * NT).rearrange("p (c t) -> p c t", c=8) for _ in range(2)]
        ores = [[Res() for _ in range(8)] for _ in range(2)]
        evs = []
        gcol = VLN * self.nlayers
        for tb in range(NTB):
            s = tb % 2
            t0 = tb * NT
            self.rmsnorm_to(t0, NT, gcol, o[s], ores[s], sq, sqres, rstd, rres)
            for c in range(8):
                evs.append(self.dma('sp', dr['outT'][c][:, t0:t0 + NT], o[s][:, c, :], rd=[ores[s][c]]))
        k.wait_all('sp', evs + self.dbg_evs)
        ar.release(m)

    def mix_phase(self, l):
        k, dr, ar = self.k, self.dr, self.ar
        k.barrier()
        m0 = ar.mark()
        self.rr = list(range(8))
        P = self.P = {}
        R = self.R = {}

        def alloc(name, kind, n):
            P[name] = ar.bf16(n) if kind == 'b' else ar.f32(n)
            R[name] = Res(name)
        alloc('pa', 'f', 11 * (NM + 1))
        alloc('Sf', 'f', 384)
        alloc('Sb', 'b', 384)
        alloc('rSf', 'f', 256)
        alloc('rSb', 'b', 256)
        alloc('cT', 'b', T)
        alloc('ctok', 'b', T)
        alloc('ik2', 'b', T)
        alloc('smw', 'b', 4 * 384)
        alloc('omka', 'f', 4)
        for nm in ('pa', 'Sf', 'Sb', 'rSf', 'rSb'):
            self.k.op('pool', lambda e, a=P[nm]: e.memset(a, 0.0), (), [R[nm]])
        self.dma('pool', P['smw'], dr['smw'][l], wr=[R['smw']])
        P['low'] = P['smw'][:, 0:384]
        P['v2'] = P['smw'][:, 384:768]
        P['wuk'] = P['smw'][:, 768:1152]
        P['wuv'] = P['smw'][:, 1152:1536]
        c = VLN * l + VL['ka']
        self.ts('dve', P['omka'][:, 0:3], self.vec[:, c:c + 3], -1.0, 1.0, ALU.mult, ALU.add, rd=[self.rvec], wr=[R['omka']])
        self.wring = [ar.bf16(1024) for _ in range(4)]
        self.wrres = [Res() for _ in range(4)]
        self.wri = 0
        for mb in range(NMB):
            self.mix_block(l, mb)
            if self.dbg.get('max_mb') is not None and mb >= self.dbg['max_mb']:
                break
        k.barrier()
        ar.release(m0)

    def proj(self, l, name, M, hT, hres):
        ci = CIDX[name]
        s = self.wri % 4
        self.wri += 1
        w = self.wring[s]
        self.dma('sp', w, self.dr['win_b'][l, ci], rd=[self.rwin[(l, ci)]], wr=[self.wrres[s]])
        b = self.nb()
        for kc in range(8):
            self.mm(self.banks[b][:M, :NM], w[:, kc * 128:kc * 128 + M], hT[:, kc, :], start=(kc == 0), stop=(kc == 7),
                    rd=[self.wrres[s], hres[kc]], wr=[self.bres[b]])
        return b

    def mix_block(self, l, mb):
        k, dr, ar = self.k, self.dr, self.ar
        t0 = mb * NM
        m = ar.mark()
        hT = ar.bf16(8 * NM).rearrange("p (c t) -> p c t", c=8)
        hres = [Res() for _ in range(8)]
        sq = [ar.bf16(NM) for _ in range(2)]
        sqres = [Res(), Res()]
        rstd = ar.f32(NM)
        rres = Res()
        oT = ar.bf16(8 * NM)
        ores = [Res() for _ in range(8)]
        self.rr = list(range(8))
        self.k.op('pool', lambda e: e.memset(oT, 0.0), (), ores)
        self.rmsnorm_to(t0, NM, VLN * l + VL['mix'], hT, hres, sq, sqres, rstd, rres)
        if 'a' in self.mixers:
            m1 = ar.mark()
            self.rwkv_block(l, mb, hT, hres, oT, ores)
            k.barrier()
            ar.release(m1)
        if 'c' in self.mixers:
            m1 = ar.mark()
            self.ret_block(l, mb, hT, hres, oT, ores)
            k.barrier()
            ar.release(m1)
        if 'b' in self.mixers:
            m1 = ar.mark()
            self.dsa_block(l, mb, hT, hres, oT, ores)
            k.barrier()
            ar.release(m1)
        if l == 0:
            self.dump('oT%d' % mb, oT, ores)
        self.rr = list(range(8))
        wo = [ar.bf16(1024) for _ in range(2)]
        wores = [Res(), Res()]
        for dc in range(8):
            s = dc % 2
            self.dma('sp', wo[s], dr['wout_b'][l, dc], rd=[self.rwout[(l, dc)]], wr=[wores[s]])
            b = self.nb()
            for mc in range(8):
                self.mm(self.banks[b][:, :NM], wo[s][:, mc * 128:(mc + 1) * 128], oT[:, mc * NM:(mc + 1) * NM],
                        start=(mc == 0), stop=(mc == 7), rd=[wores[s], ores[mc]], wr=[self.bres[b]])
            self.tt('dve', self.xT[:, dc, t0:t0 + NM], self.banks[b][:, :NM], self.xT[:, dc, t0:t0 + NM], ALU.add,
                    rd=[self.bres[b], self.xres[dc][mb]], wr=[self.xres[dc][mb]])
        k.barrier()
        ar.release(m)

    def rwkv_block(self, l, mb, hT, hres, oT, ores):
        ar, P, R, dr = self.ar, self.P, self.R, self.dr
        t0 = mb * NM
        N = NM
        B_ = self.banks
        bres = self.bres
        cf = self.cf
        rc = self.rconst
        pa = P['pa'].rearrange("p (c t) -> p c t", c=11)
        rpa = R['pa']
        nA = 11 if l > 0 else 10
        for j in range(nA):
            name = 'a%d' % j if j < 10 else 'mv'
            M = 128 if j < 10 else 16
            b = self.proj(l, name, M, hT, hres)
            self.cp('act', pa[:M, j, 1:N + 1], B_[b][:M, :N], rd=[bres[b]], wr=[rpa])
        xx = ar.f32(10 * N)
        rxx = [Res() for _ in range(10)]
        dtmp = [ar.f32(N) for _ in range(2)]
        rdt = [Res(), Res()]
        for j in range(10):
            s = j % 2
            self.tt('pool', dtmp[s], pa[:, j, 0:N], pa[:, j, 1:N + 1], ALU.subtract, rd=[rpa], wr=[rdt[s]])
            self.stt('dve', xx[:, j * N:(j + 1) * N], dtmp[s], self.vc(l, 'mu', j), pa[:, j, 1:N + 1], ALU.mult, ALU.add,
                     rd=[rdt[s], rpa, self.rvec], wr=[rxx[j]])
        if self.dbg.get('stop', 99) <= 1:
            return
        mv_b = ar.bf16(N)
        rmv = Res()
        vfb = None
        if l > 0:
            self.tt('pool', dtmp[0][:16], pa[:16, 10, 0:N], pa[:16, 10, 1:N + 1], ALU.subtract, rd=[rpa], wr=[rdt[0]])
            self.stt('dve', mv_b[:16], dtmp[0][:16], self.vc(l, 'mumv')[:16], pa[:16, 10, 1:N + 1], ALU.mult, ALU.add,
                     rd=[rdt[0], rpa, self.rvec], wr=[rmv])
            vfb = ar.f32(3 * N)
            rvfb = Res()
            self.dma('act', vfb.rearrange("p (c t) -> p c t", c=3), dr['vf'][:, :, t0:t0 + N], rd=[self.vfres[mb]], wr=[rvfb])
        for j in range(nA):
            M = 128 if j < 10 else 16
            self.cp('pool', pa[:M, j, 0:1], pa[:M, j, N:N + 1], rd=[], wr=[rpa])
        if self.dbg.get('stop', 99) <= 2:
            return
        lo = xx[:, 9 * N:10 * N]
        lo_b = ar.bf16(N)
        rlo = Res()
        self.act(lo_b[0:32], lo[0:32], AF.Tanh, rd=[rxx[9]], wr=[rlo])
        self.act(lo_b[32:64], lo[32:64], AF.Copy, rd=[rxx[9]], wr=[rlo])
        self.act(lo_b[64:128], lo[64:128], AF.Sigmoid, rd=[rxx[9]], wr=[rlo])
        low = P['low']
        rsw = R['smw']
        at_b = ar.bf16(3 * N)
        bt_b = ar.bf16(3 * N)
        kt_b = ar.bf16(3 * N)
        rt_b = ar.bf16(3 * N)
        gT = ar.bf16(3 * N)
        bonus = ar.f32(3 * N)
        rat, rbt, rkt, rrt, rg, rbo = [Res() for _ in range(6)]
        tok = [ar.bf16(3 * 384) for _ in range(NQ)]
        rtok = [Res() for _ in range(NQ)]
        Ptot = ar.f32(3 * NQ)
        rPt = Res()
        names = ['sgw', 'cs', 'csx', 'Pinc', 'Pexc', 'Pinv', 'Pend', 'a', 'nrm', 'kkn', 't1', 'kp', 'bb', 'sgv']
        tf = {n: ar.f32(N) for n in names}
        rf = {n: Res() for n in names}
        tb16 = {n: ar.bf16(N) for n in ('sqk', 'rk', 'bhT', 'khT', 'vb')}
        rb16 = {n: Res() for n in tb16}
        nbv = ar.f32(4)
        rnb = Res()
        for j in range(3):
            cs_ = slice(j * 128, (j + 1) * 128)
            fs = slice(j * N, (j + 1) * N)
            r_j = xx[:, (0 + j) * N:(1 + j) * N]
            k_j = xx[:, (3 + j) * N:(4 + j) * N]
            v_j = xx[:, (6 + j) * N:(7 + j) * N]
            rr_, rk_, rv_ = rxx[j], rxx[3 + j], rxx[6 + j]
            b = self.nb()
            self.mm(B_[b][:, :N], low[0:32, cs_], lo_b[0:32], rd=[rsw, rlo], wr=[bres[b]])
            self.act(tf['sgw'], B_[b][:, :N], AF.Sigmoid, rd=[bres[b], self.rvec], wr=[rf['sgw']], bias=self.vc(l, 'w0', j))
            self.k.op('dve', lambda e, o=tf['cs'], d0=cf[:, CF['reset']:CF['reset'] + N], d1=tf['sgw']: e.tensor_tensor_scan(
                out=o, data0=d0, data1=d1, initial=0.0, op0=ALU.mult, op1=ALU.add), [rf['sgw'], rc], [rf['cs']])
            self.tt('pool', tf['csx'], tf['cs'], tf['sgw'], ALU.subtract, rd=[rf['cs'], rf['sgw']], wr=[rf['csx']])
            self.act(tf['Pinc'], tf['cs'], AF.Exp, rd=[rf['cs']], wr=[rf['Pinc']], scale=-C0)
            self.act(tf['Pexc'], tf['csx'], AF.Exp, rd=[rf['csx']], wr=[rf['Pexc']], scale=-C0)
            self.act(tf['Pinv'], tf['cs'], AF.Exp, rd=[rf['cs']], wr=[rf['Pinv']], scale=C0)
            for q in range(NQ):
                self.ts('dve', nbv[:, q:q + 1], tf['cs'][:, q * 128 + 127:q * 128 + 128], -C0, None, ALU.mult, rd=[rf['cs']], wr=[rnb])
            for q in range(NQ):
                tq = slice(q * 128, (q + 1) * 128)
                self.act(tf['Pend'][:, tq], tf['cs'][:, tq], AF.Exp, rd=[rf['cs'], rnb], wr=[rf['Pend']], scale=C0, bias=nbv[:, q:q + 1])
            self.act(Ptot[:, j * NQ:(j + 1) * NQ], nbv[:, 0:NQ], AF.Exp, rd=[rnb], wr=[rPt])
            if self.dbg.get('stop', 99) <= 3:
                continue
            b = self.nb()
            self.mm(B_[b][:, :N], low[32:64, cs_], lo_b[32:64], rd=[rsw, rlo], wr=[bres[b]])
            self.act(tf['a'], B_[b][:, :N], AF.Sigmoid, rd=[bres[b], self.rvec], wr=[rf['a']], bias=self.vc(l, 'a0', j))
            b = self.nb()
            self.mm(B_[b][:, :N], low[64:128, cs_], lo_b[64:128], rd=[rsw, rlo], wr=[bres[b]])
            self.cp('act', gT[:, fs], B_[b][:, :N], rd=[bres[b]], wr=[rg])
            self.act(tb16['sqk'], k_j, AF.Square, rd=[rk_, self.rvec], wr=[rb16['sqk']], scale=self.vc(l, 'kk', j))
            b = self.nb()
            self.mm(B_[b][:, :N], self.bones_b, tb16['sqk'], rd=[rc, rb16['sqk']], wr=[bres[b]])
            self.act(tf['nrm'], B_[b][:, :N], AF.Sqrt, rd=[bres[b]], wr=[rf['nrm']])
            self.ts('dve', tf['nrm'], tf['nrm'], 1e-12, None, ALU.max, rd=[rf['nrm']], wr=[rf['nrm']])
            self.recip(tf['nrm'], tf['nrm'], rd=[rf['nrm']], wr=[rf['nrm']])
            self.stt('dve', tf['kkn'], k_j, self.vc(l, 'kk', j), tf['nrm'], ALU.mult, ALU.mult, rd=[rk_, self.rvec, rf['nrm']], wr=[rf['kkn']])
            self.ts('dve', tf['t1'], tf['a'], self.vc(l, 'ka', j), P['omka'][:, j:j + 1], ALU.mult, ALU.add,
                    rd=[rf['a'], self.rvec, R['omka']], wr=[rf['t1']])
            self.tt('pool', tf['kp'], tf['t1'], k_j, ALU.mult, rd=[rf['t1'], rk_], wr=[rf['kp']])
            if self.dbg.get('stop', 99) <= 4:
                continue
            self.stt('dve', at_b[:, fs], tf['kkn'], -1.0, tf['Pexc'], ALU.mult, ALU.mult, rd=[rf['kkn'], rf['Pexc']], wr=[rat])
            self.tt('pool', tf['bb'], tf['kkn'], tf['a'], ALU.mult, rd=[rf['kkn'], rf['a']], wr=[rf['bb']])
            self.tt('dve', bt_b[:, fs], tf['bb'], tf['Pinv'], ALU.mult, rd=[rf['bb'], rf['Pinv']], wr=[rbt])
            self.tt('pool', tb16['bhT'], tf['bb'], tf['Pend'], ALU.mult, rd=[rf['bb'], rf['Pend']], wr=[rb16['bhT']])
            self.tt('dve', kt_b[:, fs], tf['kp'], tf['Pinv'], ALU.mult, rd=[rf['kp'], rf['Pinv']], wr=[rkt])
            self.tt('pool', tb16['khT'], tf['kp'], tf['Pend'], ALU.mult, rd=[rf['kp'], rf['Pend']], wr=[rb16['khT']])
            self.tt('dve', rt_b[:, fs], r_j, tf['Pinc'], ALU.mult, rd=[rr_, rf['Pinc']], wr=[rrt])
            if l == 0:
                self.dma('act', dr['vf'][:, j, t0:t0 + N], v_j, rd=[rv_], wr=[self.vfres[mb]])
            else:
                b = self.nb()
                self.mm(B_[b][:, :N], P['v2'][0:16, cs_], mv_b[0:16], rd=[rsw, rmv], wr=[bres[b]])
                self.act(tf['sgv'], B_[b][:, :N], AF.Sigmoid, rd=[bres[b], self.rvec], wr=[rf['sgv']], bias=self.vc(l, 'v0', j))
                self.tt('pool', tf['t1'], vfb[:, fs], v_j, ALU.subtract, rd=[rvfb, rv_, rf['t1']], wr=[rf['t1']])
                self.tt('dve', tf['t1'], tf['t1'], tf['sgv'], ALU.mult, rd=[rf['t1'], rf['sgv']], wr=[rf['t1']])
                self.tt('pool', v_j, v_j, tf['t1'], ALU.add, rd=[rv_, rf['t1']], wr=[rv_])
            self.cp('pool', tb16['vb'], v_j, rd=[rv_], wr=[rb16['vb']])
            self.stt('dve', tb16['rk'], r_j, self.vc(l, 'rk', j), tf['kp'], ALU.mult, ALU.mult, rd=[rr_, self.rvec, rf['kp']], wr=[rb16['rk']])
            b = self.nb()
            self.mm(B_[b][:, :N], self.bones_b, tb16['rk'], rd=[rc, rb16['rk']], wr=[bres[b]])
            self.tt('dve', bonus[:, fs], B_[b][:, :N], v_j, ALU.mult, rd=[bres[b], rv_], wr=[rbo])
            if self.dbg.get('stop', 99) <= 5:
                continue
            for q in range(NQ):
                tq = slice(q * 128, (q + 1) * 128)
                b = self.nb()
                for x, nm in enumerate(('bhT', 'khT', 'vb')):
                    self.mm(B_[b][:, x * 128:(x + 1) * 128], tb16[nm][:, tq], self.ident_b, rd=[rb16[nm], rc], wr=[bres[b]])
                self.cp('act', tok[q].rearrange("p (x f) -> p x f", x=3)[:, :, cs_],
                        B_[b][:, 0:384].rearrange("p (x f) -> p x f", x=3), rd=[bres[b]], wr=[rtok[q]])
        if self.dbg.get('stop', 99) <= 6:
            return
        y_sb = ar.f32(3 * N)
        ry = Res()
        kinds = [('N', at_b, rat, bt_b, rbt, 'maskL'), ('Nt', bt_b, rbt, at_b, rat, 'maskU'), ('Aak', kt_b, rkt, at_b, rat, 'maskU'),
                 ('Arb', bt_b, rbt, rt_b, rrt, 'maskUi'), ('Ark', kt_b, rkt, rt_b, rrt, 'maskUi')]
        Am = {kd[0]: ar.bf16(768) for kd in kinds}
        rAm = {kd[0]: Res() for kd in kinds}
        Mx = [ar.bf16(768) for _ in range(2)]
        Mtx = [ar.bf16(768) for _ in range(2)]
        Qx = [ar.bf16(768) for _ in range(2)]
        rMx = [Res(), Res()]
        rMtx = [Res(), Res()]
        rQx = [Res(), Res()]
        W_sb = ar.bf16(384)
        U_sb = ar.bf16(384)
        rW, rU = Res(), Res()
        Sf, Sb = P['Sf'], P['Sb']
        rSf, rSb = R['Sf'], R['Sb']
        ident6 = self.cb[:, CB['ident']:CB['ident'] + 768]
        for q in range(NQ):
            tq = slice(q * 128, (q + 1) * 128)
            tokq = tok[q]
            for (nm, Lt, rL, Rt, rR, mk) in kinds:
                for e in range(2):
                    pr = slice(e * 64, (e + 1) * 64)
                    b = self.nb()
                    for j in range(3):
                        cols = slice(j * N + q * 128, j * N + (q + 1) * 128)
                        self.mm(B_[b][:, j * 128:(j + 1) * 128], Lt[pr, cols], Rt[pr, cols], rd=[rL, rR], wr=[bres[b]])
                    self.tt('dve', Am[nm].rearrange("p (j e t) -> p j e t", j=3, e=2)[:, :, e, :],
                            B_[b][:, 0:384].rearrange("p (j t) -> p j t", j=3),
                            cf[:, CF[mk]:CF[mk] + 384].rearrange("p (j t) -> p j t", j=3), ALU.mult,
                            rd=[bres[b], rc], wr=[rAm[nm]])
            if self.dbg.get('stop', 99) <= 7:
                continue
            Mc, Mtc, rMc, rMtc = Am['N'], Am['Nt'], rAm['N'], rAm['Nt']
            qi = 0
            self.tt('pool', Qx[qi], Am['Nt'], ident6, ALU.add, rd=[rAm['Nt'], rc], wr=[rQx[qi]])
            for lev in range(1, 7):
                mi = lev % 2
                for half in range(2):
                    hs = slice(half * 384, (half + 1) * 384)
                    b = self.nb()
                    for hh in range(3):
                        c_ = slice((half * 3 + hh) * 128, (half * 3 + hh + 1) * 128)
                        self.mm(B_[b][:, hh * 128:(hh + 1) * 128], Mtc[:, c_], Mc[:, c_], rd=[rMc, rMtc], wr=[bres[b]])
                    self.cp('act', Mx[mi][:, hs], B_[b][:, 0:384], rd=[bres[b]], wr=[rMx[mi]])
                    if lev < 6:
                        b = self.nb()
                        for hh in range(3):
                            c_ = slice((half * 3 + hh) * 128, (half * 3 + hh + 1) * 128)
                            self.mm(B_[b][:, hh * 128:(hh + 1) * 128], Mc[:, c_], Mtc[:, c_], rd=[rMc, rMtc], wr=[bres[b]])
                        self.cp('act', Mtx[mi][:, hs], B_[b][:, 0:384], rd=[bres[b]], wr=[rMtx[mi]])
                Mc, rMc = Mx[mi], rMx[mi]
                if lev < 6:
                    Mtc, rMtc = Mtx[mi], rMtx[mi]
                qn = 1 - qi
                for half in range(2):
                    hs = slice(half * 384, (half + 1) * 384)
                    b = self.nb()
                    for hh in range(3):
                        c_ = slice((half * 3 + hh) * 128, (half * 3 + hh + 1) * 128)
                        o_ = B_[b][:, hh * 128:(hh + 1) * 128]
                        self.mm(o_, self.ident_b, Qx[qi][:, c_], start=True, stop=False, rd=[rc, rQx[qi]], wr=[bres[b]])
                        self.mm(o_, Mc[:, c_], Qx[qi][:, c_], start=False, stop=True, rd=[rMc, rQx[qi]], wr=[bres[b]])
                    self.cp('dve', Qx[qn][:, hs], B_[b][:, 0:384], rd=[bres[b]], wr=[rQx[qn]])
                qi = qn
            Tt, rTt = Qx[qi], rQx[qi]
            if self.dbg.get('stop', 99) <= 8:
                continue
            b = self.nb()
            for j in range(3):
                cols = slice(j * N + q * 128, j * N + (q + 1) * 128)
                self.mm(B_[b][:, j * 128:(j + 1) * 128], at_b[:, cols], Sb[:, j * 128:(j + 1) * 128], start=True, stop=False,
                        rd=[rat, rSb], wr=[bres[b]])
                for e in range(2):
                    h = 2 * j + e
                    self.mm(B_[b][:, h * 64:(h + 1) * 64], Am['Aak'][:, h * 128:(h + 1) * 128], tokq[:, 768 + h * 64:768 + (h + 1) * 64],
                            start=False, stop=(e == 1), rd=[rAm['Aak'], rtok[q]], wr=[bres[b]])
            self.cp('act', W_sb, B_[b][:, 0:384], rd=[bres[b]], wr=[rW])
            b = self.nb()
            for h in range(6):
                self.mm(B_[b][:, h * 64:(h + 1) * 64], Tt[:, h * 128:(h + 1) * 128], W_sb[:, h * 64:(h + 1) * 64], rd=[rTt, rW], wr=[bres[b]])
            self.cp('dve', U_sb, B_[b][:, 0:384], rd=[bres[b]], wr=[rU])
            if self.dbg.get('stop', 99) <= 9:
                continue
            b = self.nb()
            for j in range(3):
                cols = slice(j * N + q * 128, j * N + (q + 1) * 128)
                self.mm(B_[b][:, j * 128:(j + 1) * 128], Sb[:, j * 128:(j + 1) * 128], rt_b[:, cols], start=True, stop=False,
                        rd=[rSb, rrt], wr=[bres[b]])
                for e in range(2):
                    h = 2 * j + e
                    pr = slice(e * 64, (e + 1) * 64)
                    o_ = B_[b][pr, j * 128:(j + 1) * 128]
                    self.mm(o_, U_sb[:, h * 64:(h + 1) * 64], Am['Arb'][:, h * 128:(h + 1) * 128], start=False, stop=False,
                            rd=[rU, rAm['Arb']], wr=[bres[b]])
                    self.mm(o_, tokq[:, 768 + h * 64:768 + (h + 1) * 64], Am['Ark'][:, h * 128:(h + 1) * 128], start=False, stop=True,
                            rd=[rtok[q], rAm['Ark']], wr=[bres[b]])
            self.cp('act', y_sb.rearrange("p (j t) -> p j t", j=3)[:, :, tq], B_[b][:, 0:384].rearrange("p (j t) -> p j t", j=3),
                    rd=[bres[b]], wr=[ry])
            if self.dbg.get('stop', 99) <= 10:
                continue
            b = self.nb()
            for h in range(6):
                j, e = h // 2, h % 2
                pr = slice(e * 64, (e + 1) * 64)
                o_ = B_[b][pr, j * 64:(j + 1) * 64]
                self.mm(o_, tokq[:, 0 + h * 64:0 + (h + 1) * 64], U_sb[:, h * 64:(h + 1) * 64], start=True, stop=False,
                        rd=[rtok[q], rU], wr=[bres[b]])
                self.mm(o_, tokq[:, 384 + h * 64:384 + (h + 1) * 64], tokq[:, 768 + h * 64:768 + (h + 1) * 64], start=False, stop=True,
                        rd=[rtok[q]], wr=[bres[b]])
            for j in range(3):
                for e in range(2):
                    pr = slice(e * 64, (e + 1) * 64)
                    sc = slice(j * 128 + e * 64, j * 128 + (e + 1) * 64)
                    self.stt('dve', Sf[pr, sc], Sf[pr, sc], Ptot[pr, j * NQ + q:j * NQ + q + 1],
                             B_[b][pr, j * 64:(j + 1) * 64], ALU.mult, ALU.add, rd=[rSf, rPt, bres[b]], wr=[rSf])
            self.cp('dve', Sb, Sf, rd=[rSf], wr=[rSb])
        if self.dbg.get('stop', 99) <= 11:
            return
        yb = ar.bf16(N)
        ryb = Res()
        yc = ar.f32(N)
        ryc = Res()
        sd = ar.f32(N)
        rsd = Res()
        for j in range(3):
            fs = slice(j * N, (j + 1) * N)
            yj = y_sb[:, fs]
            self.cp('act', yb, yj, rd=[ry], wr=[ryb])
            b = self.nb()
            self.mm(B_[b][:, :N], self.bmean_b, yb, rd=[rc, ryb], wr=[bres[b]])
            self.tt('dve', yc, yj, B_[b][:, :N], ALU.subtract, rd=[ry, bres[b]], wr=[ryc])
            self.act(yb, yc, AF.Square, rd=[ryc], wr=[ryb])
            b = self.nb()
            self.mm(B_[b][:, :N], self.bmean_b, yb, rd=[rc, ryb], wr=[bres[b]])
            self.act(sd, B_[b][:, :N], AF.Sqrt, rd=[bres[b]], wr=[rsd], bias=GN_EPS)
            self.recip(sd, sd, rd=[rsd], wr=[rsd])
            self.tt('dve', yc, yc, sd, ALU.mult, rd=[ryc, rsd], wr=[ryc])
            self.ts('dve', yc, yc, self.vc(l, 'lnw', j), self.vc(l, 'lnb', j), ALU.mult, ALU.add, rd=[ryc, self.rvec], wr=[ryc])
            self.tt('pool', yc, yc, bonus[:, fs], ALU.add, rd=[ryc, rbo], wr=[ryc])
            self.tt('dve', oT[:, j * N:(j + 1) * N], yc, gT[:, fs], ALU.mult, rd=[ryc, rg], wr=[ores[j]])

    def ret_block(self, l, mb, hT, hres, oT, ores):
        ar, P, R, dr = self.ar, self.P, self.R, self.dr
        t0 = mb * NM
        N = NM
        B_ = self.banks
        bres = self.bres
        cf = self.cf
        rc = self.rconst
        z = {}
        rz = {}
        for nm in ('cq', 'ck', 'cv', 'cg'):
            z[nm] = ar.f32(2 * N)
            rz[nm] = Res()
            for j in range(2):
                b = self.proj(l, nm + str(j), 128, hT, hres)
                self.cp('act', z[nm][:, j * N:(j + 1) * N], B_[b][:, :N], rd=[bres[b]], wr=[rz[nm]])
        rot = ar.f32(4 * N)
        rrot = Res()
        self.dma('act', rot.rearrange("p (c t) -> p c t", c=4), dr['rot'][:, :, t0:t0 + N], wr=[rrot])
        qr_b = ar.bf16(2 * N)
        qd_b = ar.bf16(2 * N)
        kr_b = ar.bf16(2 * N)
        kdT = ar.bf16(2 * N)
        cvb = ar.bf16(2 * N)
        rqr, rqd, rkr, rkd, rcvb = [Res() for _ in range(5)]
        t1 = ar.f32(N)
        t2 = ar.f32(N)
        zr = ar.f32(N)
        rt1, rt2, rzr = Res(), Res(), Res()
        prot = cf[:, CF['prot']:CF['prot'] + 128]
        for (nm, ci, si) in (('cq', 0, 1), ('ck', 2, 3)):
            for j in range(2):
                fs = slice(j * N, (j + 1) * N)
                zj = z[nm][:, fs]
                b = self.nb()
                self.mm(B_[b][:, :N], prot, zj, rd=[rc, rz[nm]], wr=[bres[b]])
                self.tt('pool', t1, zj, rot[:, ci * N:(ci + 1) * N], ALU.mult, rd=[rz[nm], rrot], wr=[rt1])
                self.tt('dve', t2, B_[b][:, :N], rot[:, si * N:(si + 1) * N], ALU.mult, rd=[bres[b], rrot], wr=[rt2])
                self.tt('dve', zr, t1, t2, ALU.add, rd=[rt1, rt2], wr=[rzr])
                if nm == 'cq':
                    self.cp('act', qr_b[:, fs], zr, rd=[rzr], wr=[rqr])
                    self.tt('pool', qd_b[:, fs], zr, cf[:, CF['qdec'] + j * N:CF['qdec'] + (j + 1) * N], ALU.mult, rd=[rzr, rc], wr=[rqd])
                else:
                    self.cp('act', kr_b[:, fs], zr, rd=[rzr], wr=[rkr])
                    self.tt('pool', kdT[:, fs], zr, cf[:, CF['kdec'] + j * N:CF['kdec'] + (j + 1) * N], ALU.mult, rd=[rzr, rc], wr=[rkd])
        self.cp('pool', cvb, z['cv'], rd=[rz['cv']], wr=[rcvb])
        tokr = [ar.bf16(512) for _ in range(NQ)]
        rtokr = [Res() for _ in range(NQ)]
        for q in range(NQ):
            b = self.nb()
            for x, (src, rs) in enumerate(((kdT, rkd), (cvb, rcvb))):
                for j in range(2):
                    self.mm(B_[b][:, (x * 2 + j) * 128:(x * 2 + j + 1) * 128], src[:, j * N + q * 128:j * N + (q + 1) * 128], self.ident_b,
                            rd=[rs, rc], wr=[bres[b]])
            self.cp('act', tokr[q], B_[b][:, 0:512], rd=[bres[b]], wr=[rtokr[q]])
        yret = ar.f32(2 * N)
        ryr = Res()
        sm = ar.bf16(512)
        rsm = Res()
        Sf, Sb = P['rSf'], P['rSb']
        rSf, rSb = R['rSf'], R['rSb']
        for q in range(NQ):
            tq = slice(q * 128, (q + 1) * 128)
            for e in range(2):
                pr = slice(e * 64, (e + 1) * 64)
                b = self.nb()
                for j in range(2):
                    cols = slice(j * N + q * 128, j * N + (q + 1) * 128)
                    self.mm(B_[b][:, j * 128:(j + 1) * 128], kr_b[pr, cols], qr_b[pr, cols], rd=[rkr, rqr], wr=[bres[b]])
                self.tt('dve', sm.rearrange("p (j e t) -> p j e t", j=2, e=2)[:, :, e, :],
                        B_[b][:, 0:256].rearrange("p (j t) -> p j t", j=2),
                        cf[:, CF['intraT']:CF['intraT'] + 512].rearrange("p (j e t) -> p j e t", j=2, e=2)[:, :, e, :], ALU.mult,
                        rd=[bres[b], rc], wr=[rsm])
            b = self.nb()
            for j in range(2):
                cols = slice(j * N + q * 128, j * N + (q + 1) * 128)
                self.mm(B_[b][:, j * 128:(j + 1) * 128], Sb[:, j * 128:(j + 1) * 128], qd_b[:, cols], start=True, stop=False,
                        rd=[rSb, rqd], wr=[bres[b]])
                for e in range(2):
                    h = 2 * j + e
                    pr = slice(e * 64, (e + 1) * 64)
                    self.mm(B_[b][pr, j * 128:(j + 1) * 128], tokr[q][:, 256 + h * 64:256 + (h + 1) * 64], sm[:, h * 128:(h + 1) * 128],
                            start=False, stop=True, rd=[rtokr[q], rsm], wr=[bres[b]])
            self.cp('act', yret.rearrange("p (j t) -> p j t", j=2)[:, :, tq], B_[b][:, 0:256].rearrange("p (j t) -> p j t", j=2),
                    rd=[bres[b]], wr=[ryr])
            b = self.nb()
            for h in range(4):
                j, e = h // 2, h % 2
                pr = slice(e * 64, (e + 1) * 64)
                self.mm(B_[b][pr, j * 64:(j + 1) * 64], tokr[q][:, h * 64:(h + 1) * 64], tokr[q][:, 256 + h * 64:256 + (h + 1) * 64],
                        rd=[rtokr[q]], wr=[bres[b]])
            for j in range(2):
                for e in range(2):
                    pr = slice(e * 64, (e + 1) * 64)
                    sc = slice(j * 128 + e * 64, j * 128 + (e + 1) * 64)
                    self.stt('dve', Sf[pr, sc], Sf[pr, sc], cf[pr, CF['cdec'] + j:CF['cdec'] + j + 1],
                             B_[b][pr, j * 64:(j + 1) * 64], ALU.mult, ALU.add, rd=[rSf, rc, bres[b]], wr=[rSf])
            self.cp('dve', Sb, Sf, rd=[rSf], wr=[rSb])
        sqb = ar.bf16(N)
        rsq = Res()
        sd = ar.f32(N)
        rsd = Res()
        sg = ar.f32(N)
        rsg = Res()
        for j in range(2):
            fs = slice(j * N, (j + 1) * N)
            self.act(sqb, yret[:, fs], AF.Square, rd=[ryr], wr=[rsq])
            b = self.nb()
            self.mm(B_[b][:, :N], self.bmean_b, sqb, rd=[rc, rsq], wr=[bres[b]])
            self.act(sd, B_[b][:, :N], AF.Sqrt, rd=[bres[b]], wr=[rsd], bias=EPS)
            self.recip(sd, sd, rd=[rsd], wr=[rsd])
            self.tt('dve', sd, sd, yret[:, fs], ALU.mult, rd=[rsd, ryr], wr=[rsd])
            self.act(sg, z['cg'][:, fs], AF.Silu, rd=[rz['cg']], wr=[rsg])
            self.tt('pool', oT[:, (6 + j) * N:(7 + j) * N], sd, sg, ALU.mult, rd=[rsd, rsg], wr=[ores[6 + j]])

    def dsa_block(self, l, mb, hT, hres, oT, ores):
        ar, P, R, dr = self.ar, self.P, self.R, self.dr
        t0 = mb * NM
        N = NM
        B_ = self.banks
        bres = self.bres
        cf = self.cf
        rc = self.rconst
        cT, ctok, ik2 = P['cT'], P['ctok'], P['ik2']
        rcT, rctok, rik2 = R['cT'], R['ctok'], R['ik2']
        self.rr = [0, 1, 2, 3]
        qT_b = ar.bf16(3 * N)
        rq = Res()
        for j in range(3):
            b = self.proj(l, 'bq%d' % j, 128, hT, hres)
            self.cp('act', qT_b[:, j * N:(j + 1) * N], B_[b][:, :N], rd=[bres[b]], wr=[rq])
        ckv = ar.f32(N)
        rckv = Res()
        b = self.proj(l, 'bc', 128, hT, hres)
        self.cp('act', ckv, B_[b][:, :N], rd=[bres[b]], wr=[rckv])
        sqb = ar.bf16(N)
        rsq = Res()
        sd = ar.f32(N)
        rsd = Res()
        self.act(sqb, ckv, AF.Square, rd=[rckv], wr=[rsq])
        b = self.nb()
        self.mm(B_[b][:, :N], self.ones_b, sqb, rd=[rc, rsq], wr=[bres[b]])
        self.act(sd, B_[b][:, :N], AF.Sqrt, rd=[bres[b]], wr=[rsd], bias=EPS, scale=1.0 / 128)
        self.recip(sd, sd, rd=[rsd], wr=[rsd])
        self.stt('dve', cT[:, t0:t0 + N], ckv, self.vc(l, 'kvn'), sd, ALU.mult, ALU.mult, rd=[rckv, self.rvec, rsd], wr=[rcT])
        for q in range(NQ):
            gq = t0 // 128 + q
            b = self.nb()
            self.mm(B_[b][:, :128], cT[:, gq * 128:(gq + 1) * 128], self.ident_b, rd=[rcT, rc], wr=[bres[b]])
            self.cp('act', ctok[:, gq * 128:(gq + 1) * 128], B_[b][:, :128], rd=[bres[b]], wr=[rctok])
        iq_b = ar.bf16(4 * N)
        riq = Res()
        for j in range(4):
            b = self.proj(l, 'biq%d' % j, 128, hT, hres)
            self.cp('act', iq_b[:, j * N:(j + 1) * N], B_[b][:, :N], rd=[bres[b]], wr=[riq])
        b = self.proj(l, 'bik2', 128, hT, hres)
        self.cp('act', ik2[:, t0:t0 + N], B_[b][:, :N], rd=[bres[b]], wr=[rik2])
        iw_f = ar.f32(N)
        riw = Res()
        b = self.proj(l, 'biw', 8, hT, hres)
        self.cp('act', iw_f[:8], B_[b][:8, :N], rd=[bres[b]], wr=[riw])
        iwbc = ar.f32(8 * N)
        riwbc = Res()
        SC = (8.0 ** -0.5) * (64.0 ** -0.5)
        for h8 in range(8):
            b = self.nb()
            self.mm(B_[b][:, :N], cf[0:8, CF['sel'] + h8 * 128:CF['sel'] + (h8 + 1) * 128], iw_f[:8], rd=[rc, riw], wr=[bres[b]])
            self.act(iwbc[:, h8 * N:(h8 + 1) * N], B_[b][:, :N], AF.Copy, rd=[bres[b]], wr=[riwbc], scale=SC)
        qlT = ar.bf16(NQ * 768)
        rql = Res()
        for h in range(6):
            j, e = h // 2, h % 2
            pr = slice(e * 64, (e + 1) * 64)
            b = self.nb()
            self.mm(B_[b][:, :N], P['wuk'][pr, j * 128:(j + 1) * 128], qT_b[pr, j * N:(j + 1) * N], rd=[R['smw'], rq], wr=[bres[b]])
            for q in range(NQ):
                self.act(qlT[:, (q * 6 + h) * 128:(q * 6 + h + 1) * 128], B_[b][:, q * 128:(q + 1) * 128], AF.Copy,
                         rd=[bres[b]], wr=[rql], scale=0.125)
        score = ar.f32(T)
        rsc = Res()
        work = [ar.f32(T), ar.f32(T)]
        rwk = [Res(), Res()]
        negm = ar.bf16(T)
        rng = Res()
        tmp = [ar.bf16(1024) for _ in range(2)]
        rtmp = [Res(), Res()]
        ex = [ar.bf16(768) for _ in range(2)]
        rex = [Res(), Res()]
        mx8 = ar.f32(8)
        rmx = Res()
        rden = ar.f32(768)
        rrd = Res()
        oln = ar.bf16(768)
        roln = Res()
        ident4 = self.cb[:, CB['ident']:CB['ident'] + 512]
        iwbc3 = iwbc.rearrange("p (h t) -> p h t", h=8)
        for q in range(NQ):
            gq = t0 // 128 + q
            nS = gq + 1
            ncols = nS * 128
            tq = slice(q * 128, (q + 1) * 128)
            self.rr = [0, 1, 2, 3]
            sb = None
            for si in range(nS):
                zb = [self.nb(), self.nb()]
                for h8 in range(8):
                    e = h8 % 2
                    pr = slice(e * 64, (e + 1) * 64)
                    jj = h8 // 2
                    self.mm(B_[zb[e]][:, jj * 128:(jj + 1) * 128], ik2[pr, si * 128:(si + 1) * 128],
                            iq_b[pr, jj * N + q * 128:jj * N + (q + 1) * 128], rd=[rik2, riq], wr=[bres[zb[e]]])
                ts_ = si % 2
                for half in range(2):
                    self.stt('dve', tmp[ts_].rearrange("p (j e t) -> p j e t", j=4, e=2)[:, :, half, :],
                             B_[zb[half]][:, 0:512].rearrange("p (h t) -> p h t", h=4), 0.0,
                             iwbc.rearrange("p (j e t) -> p j e t", j=4, e=2)[:, :, half, tq], ALU.max, ALU.mult,
                             rd=[bres[zb[half]], riwbc], wr=[rtmp[ts_]])
                if si % 4 == 0:
                    sb = 4 + (si // 4) % 2
                for h8 in range(8):
                    self.mm(B_[sb][:, (si % 4) * 128:(si % 4 + 1) * 128], tmp[ts_][:, h8 * 128:(h8 + 1) * 128], self.ident_b,
                            start=(h8 == 0), stop=(h8 == 7), rd=[rtmp[ts_], rc], wr=[bres[sb]])
                if si % 4 == 3 or si == nS - 1:
                    c0 = (si // 4) * 512
                    nc_ = (si % 4 + 1) * 128
                    self.cp('act', score[:, c0:c0 + nc_], B_[sb][:, 0:nc_], rd=[bres[sb]], wr=[rsc])
            self.tt('pool', score[:, gq * 128:(gq + 1) * 128], score[:, gq * 128:(gq + 1) * 128], cf[:, CF['cmask']:CF['cmask'] + 128],
                    ALU.add, rd=[rsc, rc], wr=[rsc])
            if gq >= 2 and not self.dbg.get('notopk'):
                cur, rcur = score, rsc
                for r_ in range(32):
                    self.k.op('dve', lambda e, o=mx8, i=cur[:, :ncols]: e.max(out=o, in_=i), [rcur], [rmx])
                    if r_ < 31:
                        nxt, rnxt = work[r_ % 2], rwk[r_ % 2]
                        self.k.op('dve', lambda e, o=nxt[:, :ncols], i=cur[:, :ncols], m8=mx8: e.match_replace(
                            out=o, in_to_replace=m8, in_values=i, imm_value=-1e30), [rmx, rcur], [rnxt])
                        cur, rcur = nxt, rnxt
                thr = mx8[:, 7:8]
                self.ts('dve', negm[:, :ncols], score[:, :ncols], thr, -30000.0, ALU.is_lt, ALU.mult, rd=[rsc, rmx], wr=[rng])
            else:
                self.ts('dve', negm[:, :ncols], score[:, :ncols], -1e29, -30000.0, ALU.is_lt, ALU.mult, rd=[rsc], wr=[rng])
            for si in range(nS):
                s_ = slice(si * 128, (si + 1) * 128)
                la, lb = self.nb(), self.nb()
                xs = si % 2
                self.mm(B_[la][:, 0:512], cT[:, s_], qlT[:, q * 768:q * 768 + 512], start=True, stop=False, rd=[rcT, rql], wr=[bres[la]])
                self.mm(B_[la][:, 0:512], negm[:, s_], ident4, start=False, stop=True, rd=[rng, rc], wr=[bres[la]])
                self.mm(B_[lb][:, 0:256], cT[:, s_], qlT[:, q * 768 + 512:q * 768 + 768], start=True, stop=False, rd=[rcT, rql], wr=[bres[lb]])
                self.mm(B_[lb][:, 0:256], negm[:, s_], ident4[:, 0:256], start=False, stop=True, rd=[rng, rc], wr=[bres[lb]])
                self.act(ex[xs][:, 0:512], B_[la][:, 0:512], AF.Exp, rd=[bres[la]], wr=[rex[xs]])
                self.act(ex[xs][:, 512:768], B_[lb][:, 0:256], AF.Exp, rd=[bres[lb]], wr=[rex[xs]])
                st_, sp_ = (si == 0), (si == nS - 1)
                self.mm(B_[4][:, 0:512], ctok[:, s_], ex[xs][:, 0:512], start=st_, stop=sp_, rd=[rctok, rex[xs]], wr=[bres[4]])
                self.mm(B_[5][:, 0:256], ctok[:, s_], ex[xs][:, 512:768], start=st_, stop=sp_, rd=[rctok, rex[xs]], wr=[bres[5]])
                self.mm(B_[6][:, 0:512], self.ones_b, ex[xs][:, 0:512], start=st_, stop=sp_, rd=[rc, rex[xs]], wr=[bres[6]])
                self.mm(B_[7][:, 0:256], self.ones_b, ex[xs][:, 512:768], start=st_, stop=sp_, rd=[rc, rex[xs]], wr=[bres[7]])
            self.recip(rden[:, 0:512], B_[6][:, 0:512], rd=[bres[6]], wr=[rrd])
            self.recip(rden[:, 512:768], B_[7][:, 0:256], rd=[bres[7]], wr=[rrd])
            self.tt('dve', oln[:, 0:512], B_[4][:, 0:512], rden[:, 0:512], ALU.mult, rd=[bres[4], rrd], wr=[roln])
            self.tt('dve', oln[:, 512:768], B_[5][:, 0:256], rden[:, 512:768], ALU.mult, rd=[bres[5], rrd], wr=[roln])
            b = self.nb()
            for h in range(6):
                j, e = h // 2, h % 2
                pr = slice(e * 64, (e + 1) * 64)
                self.mm(B_[b][pr, j * 128:(j + 1) * 128], P['wuv'][:, h * 64:(h + 1) * 64], oln[:, h * 128:(h + 1) * 128],
                        rd=[R['smw'], roln], wr=[bres[b]])
            for j in range(3):
                self.cp('act', oT[:, (3 + j) * N + q * 128:(3 + j) * N + (q + 1) * 128], B_[b][:, j * 128:(j + 1) * 128],
                        rd=[bres[b]], wr=[ores[3 + j]])
        self.rr = list(range(8))


def _prep_ffn(wg, wu, wd):
    def gu(w):
        wp = np.zeros((D, DFFP), np.float32)
        wp[:, :DFF] = w
        return np.ascontiguousarray(wp.reshape(8, 128, NFC, 128).transpose(2, 1, 0, 3)).reshape(NFC, 128, 8 * 128)
    wdp = np.zeros((DFFP, D), np.float32)
    wdp[:DFF] = wd
    wdt = np.ascontiguousarray(wdp.reshape(NFC, 128, 8, 128).transpose(2, 1, 0, 3)).reshape(8, 128, NFC * 128)
    return np.stack([gu(wg), gu(wu)]), wdt


def _col(v, n):
    return np.ascontiguousarray(np.asarray(v, np.float32).reshape(n, 128).T)


def _consts():
    cf = np.zeros((128, CFN), np.float32)
    cb = np.zeros((128, CBN), np.float32)
    r = np.arange(128)[:, None]
    c = np.arange(128)[None, :]
    cf[:, CF['maskL']:CF['maskL'] + 384] = np.tile((c < r).astype(np.float32), (1, 3))
    cf[:, CF['maskU']:CF['maskU'] + 384] = np.tile((r < c).astype(np.float32), (1, 3))
    cf[:, CF['maskUi']:CF['maskUi'] + 384] = np.tile((r <= c).astype(np.float32), (1, 3))
    gam = 1.0 - 2.0 ** (-5.0 - np.arange(4, dtype=np.float64))
    lg = np.log(gam)
    for h in range(4):
        dm = (c - r).astype(np.float64)
        cf[:, CF['intraT'] + h * 128:CF['intraT'] + (h + 1) * 128] = np.where(dm >= 0, np.exp(np.maximum(dm, 0) * lg[h]), 0.0)
    p = np.arange(128)
    partner = np.where((p % 64) < 32, p + 32, p - 32)
    prot = np.zeros((128, 128), np.float32)
    prot[partner, p] = 1.0
    cf[:, CF['prot']:CF['prot'] + 128] = prot
    rs = np.ones((128, NM), np.float32)
    rs[:, 0::128] = 0.0
    cf[:, CF['reset']:CF['reset'] + NM] = rs
    cf[:, CF['cmask']:CF['cmask'] + 128] = np.where(c <= r, 0.0, -1e30)
    sel = np.zeros((128, 1024), np.float32)
    for h in range(8):
        sel[h, h * 128:(h + 1) * 128] = 1.0
    cf[:, CF['sel']:CF['sel'] + 1024] = sel
    n = (np.arange(NM) % 128).astype(np.float64)
    for j in range(2):
        hh = 2 * j + (p // 64)
        cf[:, CF['qdec'] + j * NM:CF['qdec'] + (j + 1) * NM] = np.exp((n[None, :] + 1.0) * lg[hh][:, None])
        cf[:, CF['kdec'] + j * NM:CF['kdec'] + (j + 1) * NM] = np.exp((127.0 - n[None, :]) * lg[hh][:, None])
        cf[:, CF['cdec'] + j] = np.exp(128.0 * lg[hh])
    cf[:, CF['negbig']] = -1e29
    cb[:, CB['ident']:CB['ident'] + 768] = np.tile(np.eye(128, dtype=np.float32), (1, 6))
    bo = np.zeros((128, 128), np.float32)
    bo[:64, :64] = 1.0
    bo[64:, 64:] = 1.0
    cb[:, CB['bones']:CB['bones'] + 128] = bo
    cb[:, CB['bmean']:CB['bmean'] + 128] = bo / 64.0
    cb[:, CB['ones']:CB['ones'] + 128] = 1.0
    half = 32
    theta = 10000.0 ** (-np.linspace(0.0, 1.0, half))
    i = p % 32
    ang = np.arange(T, dtype=np.float64)[None, :] * theta[i][:, None]
    ang32 = (np.arange(T, dtype=np.float32)[None, :] * theta.astype(np.float32)[i][:, None]).astype(np.float64)
    sign = np.where((p % 64) < 32, -1.0, 1.0)[:, None]
    rot = np.zeros((128, 4, T), np.float32)
    rot[:, 0] = np.cos(ang32)
    rot[:, 1] = np.sin(ang32) * sign
    rot[:, 2] = np.cos(ang32) * 0.125
    rot[:, 3] = np.sin(ang32) * sign * 0.125
    return cf, cb, rot


def make_inputs(bld, inp):
    nl = bld.nlayers
    wgu = np.zeros((nl * 2, 2, NFC, 128, 8 * 128), np.float32)
    wd = np.zeros((nl * 2, 8, 128, NFC * 128), np.float32)
    win = np.zeros((nl, NCH, 128, 8 * 128), np.float32)
    wout = np.zeros((nl, 8, 128, 8 * 128), np.float32)
    smw = np.zeros((nl, 128, 4 * 384), np.float32)
    vec = np.zeros((128, VLN * nl + 8), np.float32)
    for l in range(nl):
        for j, nm in enumerate(('ffn1', 'ffn2')):
            a, b = _prep_ffn(inp[nm + '_w_gate'][l], inp[nm + '_w_up'][l], inp[nm + '_w_down'][l])
            wgu[2 * l + j] = a
            wd[2 * l + j] = b
        w_in = inp['w_in'][l]
        for ci, (name, cols) in enumerate(CHUNKS):
            if cols == 'mv':
                if l == 0:
                    continue
                src = inp['rwkv_vres_w_in'][l - 1]
            else:
                src = w_in[:, cols]
            M = src.shape[1]
            img = np.zeros((8, 128, 128), np.float32)
            img[:, :, :M] = src.reshape(8, 128, M)
            win[l, ci] = img.transpose(1, 0, 2).reshape(128, 8 * 128)
        wo = inp['w_out'][l]
        wout[l] = wo.reshape(8, 128, 8, 128).transpose(2, 1, 0, 3).reshape(8, 128, 8 * 128)
        smw[l, 0:32, 0:384] = inp['rwkv_w2'][l]
        smw[l, 32:64, 0:384] = inp['rwkv_a2'][l]
        smw[l, 64:128, 0:384] = inp['rwkv_g2'][l]
        if l > 0:
            smw[l, 0:16, 384:768] = inp['rwkv_v2'][l - 1]
        wuk = inp['dsa_w_uk'][l]
        for h in range(6):
            j, e = h // 2, h % 2
            smw[l, e * 64:(e + 1) * 64, 768 + j * 128:768 + (j + 1) * 128] = wuk[h]
        wuv = inp['dsa_w_uv'][l]
        for h in range(6):
            smw[l, :, 1152 + h * 64:1152 + (h + 1) * 64] = wuv[h]
        o = VLN * l
        vec[:, o + VL['ffn1']:o + VL['ffn1'] + 8] = _col(inp['ffn1_norm'][l], 8)
        vec[:, o + VL['ffn2']:o + VL['ffn2'] + 8] = _col(inp['ffn2_norm'][l], 8)
        vec[:, o + VL['mix']:o + VL['mix'] + 8] = _col(inp['mix_norm'][l], 8)
        vec[:, o + VL['mu']:o + VL['mu'] + 10] = _col(inp['rwkv_mu'][l], 10)
        if l > 0:
            vec[:16, o + VL['mumv']] = inp['rwkv_vres_mu'][l - 1]
            vec[:, o + VL['v0']:o + VL['v0'] + 3] = _col(inp['rwkv_v0'][l - 1], 3)
        vec[:, o + VL['w0']:o + VL['w0'] + 3] = _col(inp['rwkv_w0'][l], 3)
        vec[:, o + VL['a0']:o + VL['a0'] + 3] = _col(inp['rwkv_a0'][l], 3)
        vec[:, o + VL['kk']:o + VL['kk'] + 3] = _col(inp['rwkv_k_k'][l], 3)
        vec[:, o + VL['ka']:o + VL['ka'] + 3] = _col(inp['rwkv_k_a'][l], 3)
        vec[:, o + VL['rk']:o + VL['rk'] + 3] = _col(inp['rwkv_r_k'][l].reshape(-1), 3)
        vec[:, o + VL['lnw']:o + VL['lnw'] + 3] = _col(inp['rwkv_ln_w'][l], 3)
        vec[:, o + VL['lnb']:o + VL['lnb'] + 3] = _col(inp['rwkv_ln_b'][l], 3)
        vec[:, o + VL['kvn']] = inp['dsa_kv_norm'][l]
    vec[:, VLN * nl:VLN * nl + 8] = _col(inp['final_norm'], 8)
    cf, cb, rot = _consts()
    return {'wgu': wgu, 'wd': wd, 'win': win, 'wout': wout, 'smw': smw, 'vec': vec, 'cf': cf, 'cb': cb, 'rot': rot}


def kernel(**inputs):
    inp = {k_: np.asarray(v) for k_, v in inputs.items()}
    bld = B()
    nc = bld.build()
    shared = make_inputs(bld, inp)
    x = inp['x']
    in_maps = []
    for b in range(8):
        m = dict(shared)
        m['xT'] = np.ascontiguousarray(x[b].T).reshape(8, 128, T)
        in_maps.append(m)
    res = run_bass_kernel_spmd(nc, in_maps, core_ids=list(range(8)))
    out = np.stack([np.ascontiguousarray(np.asarray(r['outT']).reshape(D, T).T) for r in res.results])
    return out.astype(np.float32)
```

```python
import math
from contextlib import ExitStack
import numpy as np
import concourse.bass as bass
import concourse.mybir as mybir
from concourse.bass_utils import run_bass_kernel_spmd

F32 = mybir.dt.float32
BF16 = mybir.dt.bfloat16
ALU = mybir.AluOpType
AF = mybir.ActivationFunctionType
AX = mybir.AxisListType

ENG = ('pe', 'act', 'dve', 'pool', 'sp')

D = 1024
T = 2048
L = 2
DFF = 2752
DFFP = 2816
NFC = 22
NT = 512
NTB = T // NT
NM = 256
NMB = T // NM
NQ = NM // 128
EPS = 1e-6
GN_EPS = 64e-5
C0 = math.exp(-0.5)
NCH = 29


class Res:
    __slots__ = ('name', 'w', 'r')

    def __init__(self, name=''):
        self.name = name
        self.w = None
        self.r = []


class KB:
    def __init__(self, nc, same_engine_sync=True, dma_ring=8):
        self.nc = nc
        self.ops = {e: [] for e in ENG}
        self.cnt = {e: 0 for e in ENG}
        self.waited = {e: {} for e in ENG}
        self.same = same_engine_sync
        self.ring = dma_ring
        self.dma_n = {e: 0 for e in ENG}
        self.semh = {}
        self.semkeys = [('e', e) for e in ENG]
        for e in ('sp', 'pool', 'act'):
            for i in range(dma_ring):
                self.semkeys.append(('d', e, i))
        self.last_dma = {}
        self.n_wait = 0

    def _wait(self, eng, ev):
        if ev is None:
            return
        key, val = ev
        if key == ('e', eng) and (eng == 'pe' or not self.same):
            return
        cur = self.waited[eng].get(key, 0)
        if cur >= val:
            return
        self.waited[eng][key] = val
        self.n_wait += 1
        self.ops[eng].append(('wait', key, val))

    def _deps(self, eng, reads, writes):
        for r in reads:
            self._wait(eng, r.w)
        for w in writes:
            self._wait(eng, w.w)
            for ev in w.r:
                self._wait(eng, ev)

    def _commit(self, ev, reads, writes):
        for w in writes:
            w.w = ev
            w.r = []
        for r in reads:
            if r not in writes:
                r.r.append(ev)
                if len(r.r) > 64:
                    r.r = r.r[-64:] if False else r.r

    def op(self, eng, fn, reads=(), writes=()):
        reads = list(reads)
        writes = list(writes)
        self._deps(eng, reads, writes)
        self.cnt[eng] += 1
        ev = (('e', eng), self.cnt[eng])
        self.ops[eng].append(('op', fn, ('e', eng), 1))
        self._commit(ev, reads, writes)
        return ev

    def dma(self, q, fn, reads=(), writes=()):
        reads = list(reads)
        writes = list(writes)
        self._deps(q, reads, writes)
        j = self.dma_n[q]
        self.dma_n[q] += 1
        slot = j % self.ring
        key = ('d', q, slot)
        tgt = 16 * (j // self.ring + 1)
        if j >= self.ring:
            self._wait(q, (key, tgt - 16))
        ev = (key, tgt)
        self.last_dma[key] = ev
        self.ops[q].append(('op', fn, key, 16))
        self._commit(ev, reads, writes)
        return ev

    def wait_all(self, eng, evs):
        for ev in evs:
            self._wait(eng, ev)

    def barrier(self):
        evs = [(('e', e), self.cnt[e]) for e in ENG if self.cnt[e] > 0]
        evs += list(self.last_dma.values())
        for e in ENG:
            for ev in evs:
                self._wait(e, ev)

    def finalize(self, stack):
        nc = self.nc
        for kk in self.semkeys:
            self.semh[kk] = stack.enter_context(nc.semaphore('s_' + '_'.join(str(x) for x in kk)))
        block = stack.enter_context(nc.Block())
        semh = self.semh

        def run(eng_name):
            def body(eng):
                for it in self.ops[eng_name]:
                    if it[0] == 'wait':
                        eng.wait_ge(semh[it[1]], it[2])
                    else:
                        ins = it[1](eng)
                        ins.then_inc(semh[it[2]], it[3])
            return body

        block.tensor(run('pe'))
        block.scalar(run('act'))
        block.vector(run('dve'))
        block.gpsimd(run('pool'))
        block.sync(run('sp'))


class Arena:
    def __init__(self, ap, nwords):
        self.ap = ap
        self.n = nwords
        self.top = 0
        self.peak = 0

    def mark(self):
        return self.top

    def release(self, m):
        self.top = m

    def f32(self, n):
        a = self.ap[:, self.top:self.top + n]
        self.top += n
        self.peak = max(self.peak, self.top)
        assert self.top <= self.n, ("arena overflow", self.top, self.n)
        return a

    def bf16(self, n):
        w = (n + 1) // 2
        return self.f32(w).bitcast(BF16)[:, 0:n]


def win_chunks():
    ch = []
    for j in range(10):
        ch.append(('a%d' % j, list(range(j * 128, (j + 1) * 128))))
    ch.append(('mv', 'mv'))
    for j in range(3):
        ch.append(('bq%d' % j, list(range(1280 + j * 128, 1280 + (j + 1) * 128))))
    ch.append(('bc', list(range(1664, 1792))))
    for j in range(4):
        ch.append(('biq%d' % j, list(range(1792 + j * 128, 1792 + (j + 1) * 128))))
    ch.append(('bik2', list(range(2304, 2368)) * 2))
    ch.append(('biw', list(range(2368, 2376))))
    for n, base in (('cq', 2376), ('ck', 2632), ('cv', 2888), ('cg', 3144)):
        for j in range(2):
            ch.append((n + str(j), list(range(base + j * 128, base + (j + 1) * 128))))
    return ch


CHUNKS = win_chunks()
CIDX = {c[0]: i for i, c in enumerate(CHUNKS)}

VL = {}
_o = 0
for _n, _w in (('ffn1', 8), ('ffn2', 8), ('mix', 8), ('mu', 10), ('mumv', 1), ('w0', 3), ('a0', 3), ('kk', 3), ('ka', 3),
               ('rk', 3), ('lnw', 3), ('lnb', 3), ('v0', 3), ('kvn', 1)):
    VL[_n] = _o
    _o += _w
VLN = _o

CF = {}
_o = 0
for _n, _w in (('maskL', 384), ('maskU', 384), ('maskUi', 384), ('intraT', 512), ('prot', 128), ('reset', NM), ('cmask', 128),
               ('sel', 1024), ('qdec', 2 * NM), ('kdec', 2 * NM), ('cdec', 2), ('negbig', 1)):
    CF[_n] = _o
    _o += _w
CFN = _o
CB = {}
_o = 0
for _n, _w in (('ident', 768), ('bones', 128), ('bmean', 128), ('ones', 128)):
    CB[_n] = _o
    _o += _w
CBN = _o


class B:
    def __init__(self, nlayers=L, do_ffn=True, do_mix=True, dbg=None, mixers=('a', 'b', 'c')):
        self.nlayers = nlayers
        self.do_ffn = do_ffn
        self.do_mix = do_mix
        self.dbg = dbg or {}
        self.mixers = mixers

    def mm(self, out, lhsT, rhs, start=True, stop=True, rd=(), wr=()):
        return self.k.op('pe', lambda e: e.matmul(out, lhsT=lhsT, rhs=rhs, start=start, stop=stop), rd, wr)

    def act(self, out, in_, func, rd=(), wr=(), **kw):
        return self.k.op('act', lambda e: e.activation(out=out, in_=in_, func=func, **kw), rd, wr)

    def tt(self, eng, out, in0, in1, op, rd=(), wr=()):
        return self.k.op(eng, lambda e: e.tensor_tensor(out=out, in0=in0, in1=in1, op=op), rd, wr)

    def stt(self, eng, out, in0, scalar, in1, op0, op1, rd=(), wr=()):
        return self.k.op(eng, lambda e: e.scalar_tensor_tensor(out=out, in0=in0, scalar=scalar, in1=in1, op0=op0, op1=op1), rd, wr)

    def ts(self, eng, out, in0, s1, s2, op0, op1=None, rd=(), wr=()):
        if op1 is None:
            return self.k.op(eng, lambda e: e.tensor_scalar(out=out, in0=in0, scalar1=s1, scalar2=None, op0=op0), rd, wr)
        return self.k.op(eng, lambda e: e.tensor_scalar(out=out, in0=in0, scalar1=s1, scalar2=s2, op0=op0, op1=op1), rd, wr)

    def cp(self, eng, out, in_, rd=(), wr=()):
        if eng == 'act':
            return self.k.op('act', lambda e: e.activation(out=out, in_=in_, func=AF.Copy), rd, wr)
        return self.k.op(eng, lambda e: e.tensor_copy(out=out, in_=in_), rd, wr)

    def recip(self, out, in_, rd=(), wr=()):
        return self.k.op('dve', lambda e: e.reciprocal(out=out, in_=in_), rd, wr)

    def dma(self, q, out, in_, rd=(), wr=()):
        return self.k.dma(q, lambda e: e.dma_start(out=out, in_=in_), rd, wr)

    def nb(self):
        b = self.rr[self.rri % len(self.rr)]
        self.rri += 1
        return b

    def dump(self, name, ap, reads):
        if name not in self.dbg:
            return
        shape = list(ap.shape)
        d = self.nc.dram_tensor("dbg_" + name, shape, ap.dtype, kind="ExternalOutput").ap()
        ev = self.k.dma('sp', lambda e: e.dma_start(out=d, in_=ap), reads=reads)
        self.dbg_evs.append(ev)

    def build(self):
        nc = bass.Bass("TRN2", target_bir_lowering=False)
        self.nc = nc
        nl = self.nlayers
        dr = {}
        dr['xT'] = nc.dram_tensor("xT", [8, 128, T], F32, kind="ExternalInput").ap()
        dr['outT'] = nc.dram_tensor("outT", [8, 128, T], F32, kind="ExternalOutput").ap()
        dr['wgu'] = nc.dram_tensor("wgu", [nl * 2, 2, NFC, 128, 8 * 128], F32, kind="ExternalInput").ap()
        dr['wd'] = nc.dram_tensor("wd", [nl * 2, 8, 128, NFC * 128], F32, kind="ExternalInput").ap()
        dr['wgu_b'] = nc.dram_tensor("wgu_b", [nl * 2, 2, NFC, 128, 8 * 128], BF16, kind="Internal").ap()
        dr['wd_b'] = nc.dram_tensor("wd_b", [nl * 2, 8, 128, NFC * 128], BF16, kind="Internal").ap()
        dr['win'] = nc.dram_tensor("win", [nl, NCH, 128, 8 * 128], F32, kind="ExternalInput").ap()
        dr['win_b'] = nc.dram_tensor("win_b", [nl, NCH, 128, 8 * 128], BF16, kind="Internal").ap()
        dr['wout'] = nc.dram_tensor("wout", [nl, 8, 128, 8 * 128], F32, kind="ExternalInput").ap()
        dr['wout_b'] = nc.dram_tensor("wout_b", [nl, 8, 128, 8 * 128], BF16, kind="Internal").ap()
        dr['smw'] = nc.dram_tensor("smw", [nl, 128, 384 + 384 + 384 + 384], F32, kind="ExternalInput").ap()
        dr['vec'] = nc.dram_tensor("vec", [128, VLN * nl + 8], F32, kind="ExternalInput").ap()
        dr['cf'] = nc.dram_tensor("cf", [128, CFN], F32, kind="ExternalInput").ap()
        dr['cb'] = nc.dram_tensor("cb", [128, CBN], F32, kind="ExternalInput").ap()
        dr['rot'] = nc.dram_tensor("rot", [128, 4, T], F32, kind="ExternalInput").ap()
        dr['vf'] = nc.dram_tensor("vf", [128, 3, T], F32, kind="Internal").ap()
        self.dr = dr

        with ExitStack() as st:
            self.st = st
            k = KB(nc, same_engine_sync=not self.dbg.get('nosame'))
            self.k = k
            self.xT = st.enter_context(nc.sbuf_tensor("xT_sb", [128, 8, T], F32))
            self.xres = [[Res() for _ in range(NMB)] for _ in range(8)]
            self.vec = st.enter_context(nc.sbuf_tensor("vec_sb", [128, VLN * nl + 8], F32))
            self.rvec = Res()
            self.cf = st.enter_context(nc.sbuf_tensor("cf_sb", [128, CFN], F32))
            self.cb = st.enter_context(nc.sbuf_tensor("cb_sb", [128, CBN], BF16))
            self.rconst = Res()
            self.ones_b = self.cb[:, CB['ones']:CB['ones'] + 128]
            self.ident_b = self.cb[:, CB['ident']:CB['ident'] + 128]
            self.bones_b = self.cb[:, CB['bones']:CB['bones'] + 128]
            self.bmean_b = self.cb[:, CB['bmean']:CB['bmean'] + 128]
            rem = nc.sbuf_bytes_remaining
            AW = (rem - 2048) // 4
            arena_t = st.enter_context(nc.sbuf_tensor("arena", [128, AW], F32))
            self.ar = Arena(arena_t, AW)
            self.banks = [st.enter_context(nc.psum_tensor("bank%d" % i, [128, 512], F32)) for i in range(8)]
            self.bres = [Res() for _ in range(8)]
            self.rr = list(range(8))
            self.rri = 0
            self.dbg_evs = []
            self.vfres = [Res() for _ in range(NMB)]

            self.prologue()
            for l in range(nl):
                if self.do_ffn:
                    self.ffn_phase(l, 0)
                self.cast_layer(l + 1, defer=True)
                if self.do_mix:
                    self.mix_phase(l)
                self.flush_casts()
                if self.do_ffn and not self.dbg.get('skip_ffn2'):
                    self.ffn_phase(l, 1)
            self.final_phase()
            k.finalize(st)
        return nc

    def vc(self, l, name, j=0):
        c = VLN * l + VL[name] + j
        return self.vec[:, c:c + 1]

    def xr(self, c, t0, n):
        return [self.xres[c][b] for b in range(t0 // NM, (t0 + n) // NM)]

    def prologue(self):
        k, nc, dr = self.k, self.nc, self.dr
        for c in range(8):
            self.dma('sp', self.xT[:, c, :], dr['xT'][c], wr=[self.xres[c][b] for b in range(NMB)])
        self.dma('act', self.vec[:], dr['vec'][:, :], wr=[self.rvec])
        self.dma('act', self.cf[:], dr['cf'][:, :], wr=[self.rconst])
        self.dma('pool', self.cb[:], dr['cb'][:, :], wr=[self.rconst])
        self.rwgu = {}
        self.rwd = {}
        self.rwin = {}
        self.rwout = {}
        self.cast_layer(0)

    def cast_layer(self, l, defer=False):
        if l >= self.nlayers:
            return
        th = []
        if self.do_ffn:
            th += self.cast_ffn(2 * l)
        if self.do_mix:
            th += self.cast_mix(l)
        if self.do_ffn and not self.dbg.get('skip_ffn2'):
            th += self.cast_ffn(2 * l + 1)
        if defer:
            self.pending = th
        else:
            for f in th:
                f()

    def flush_casts(self, n=None):
        p = getattr(self, 'pending', [])
        n = len(p) if n is None else min(n, len(p))
        for f in p[:n]:
            f()
        self.pending = p[n:]

    def cast_ffn(self, i):
        dr = self.dr
        th = []
        for gu in range(2):
            for fc in range(NFC):
                r = Res()
                self.rwgu[(i, gu, fc)] = r
                th.append(lambda i=i, gu=gu, fc=fc, r=r: self.dma('pool', dr['wgu_b'][i, gu, fc], dr['wgu'][i, gu, fc], wr=[r]))
        for dc in range(8):
            r = Res()
            self.rwd[(i, dc)] = r
            th.append(lambda i=i, dc=dc, r=r: self.dma('pool', dr['wd_b'][i, dc].rearrange("p (a f) -> (p a) f", a=2),
                                                      dr['wd'][i, dc].rearrange("p (a f) -> (p a) f", a=2), wr=[r]))
        return th

    def cast_mix(self, l):
        dr = self.dr
        th = []
        for ci in range(NCH):
            r = Res()
            self.rwin[(l, ci)] = r
            th.append(lambda l=l, ci=ci, r=r: self.dma('pool', dr['win_b'][l, ci], dr['win'][l, ci], wr=[r]))
        for dc in range(8):
            r = Res()
            self.rwout[(l, dc)] = r
            th.append(lambda l=l, dc=dc, r=r: self.dma('pool', dr['wout_b'][l, dc], dr['wout'][l, dc], wr=[r]))
        return th

    def rmsnorm_to(self, t0, n, gcol, hT, hres, sq, sqres, rstd, rres):
        k = self.k
        ts_ = slice(t0, t0 + n)
        bank = self.nb()
        ps = self.banks[bank]
        for c in range(8):
            s = c % 2
            self.act(sq[s], self.xT[:, c, ts_], AF.Square, rd=self.xr(c, t0, n), wr=[sqres[s]])
            self.mm(ps[:, :n], self.ones_b, sq[s], start=(c == 0), stop=(c == 7), rd=[sqres[s], self.rconst], wr=[self.bres[bank]])
        self.act(rstd, ps[:, :n], AF.Sqrt, rd=[self.bres[bank]], wr=[rres], bias=EPS, scale=1.0 / D)
        self.recip(rstd, rstd, rd=[rres], wr=[rres])
        for c in range(8):
            self.stt('dve', hT[:, c, :], self.xT[:, c, ts_], self.vec[:, gcol + c:gcol + c + 1], rstd, ALU.mult, ALU.mult,
                     rd=self.xr(c, t0, n) + [rres, self.rvec], wr=[hres[c]])

    def ffn_phase(self, l, j):
        k, nc, dr, ar = self.k, self.nc, self.dr, self.ar
        i = 2 * l + j
        k.barrier()
        m = ar.mark()
        self.rr = [0]
        hT = ar.bf16(8 * NT).rearrange("p (c t) -> p c t", c=8)
        hres = [Res() for _ in range(8)]
        aT = ar.bf16(NFC * NT).rearrange("p (c t) -> p c t", c=NFC)
        ares = [Res() for _ in range(NFC)]
        sq = [ar.bf16(NT) for _ in range(2)]
        sqres = [Res(), Res()]
        rstd = ar.f32(NT)
        rres = Res()
        NW = 3
        wg = [ar.bf16(8 * 128).rearrange("p (c f) -> p c f", c=8) for _ in range(NW)]
        wu = [ar.bf16(8 * 128).rearrange("p (c f) -> p c f", c=8) for _ in range(NW)]
        wgres = [Res() for _ in range(NW)]
        wures = [Res() for _ in range(NW)]
        wd = [ar.bf16(NFC * 128).rearrange("p (c f) -> p c f", c=NFC) for _ in range(2)]
        wdres = [Res(), Res()]
        sg = [ar.bf16(NT) for _ in range(2)]
        sgres = [Res(), Res()]
        gcol = VLN * l + VL['ffn1' if j == 0 else 'ffn2']
        B_ = self.banks
        for tb in range(NTB):
            t0 = tb * NT
            ts_ = slice(t0, t0 + NT)
            self.rmsnorm_to(t0, NT, gcol, hT, hres, sq, sqres, rstd, rres)
            for fc in range(NFC):
                s = fc % NW
                self.dma('sp', wg[s], dr['wgu_b'][i, 0, fc].rearrange("p (c f) -> p c f", c=8), rd=[self.rwgu[(i, 0, fc)]], wr=[wgres[s]])
                self.dma('sp', wu[s], dr['wgu_b'][i, 1, fc].rearrange("p (c f) -> p c f", c=8), rd=[self.rwgu[(i, 1, fc)]], wr=[wures[s]])
                gb = 1 + fc % 2
                ub = 3 + fc % 2
                for kc in range(8):
                    self.mm(B_[gb][:, :NT], wg[s][:, kc, :], hT[:, kc, :], start=(kc == 0), stop=(kc == 7),
                            rd=[wgres[s], hres[kc]], wr=[self.bres[gb]])
                for kc in range(8):
                    self.mm(B_[ub][:, :NT], wu[s][:, kc, :], hT[:, kc, :], start=(kc == 0), stop=(kc == 7),
                            rd=[wures[s], hres[kc]], wr=[self.bres[ub]])
                s2 = fc % 2
                self.act(sg[s2], B_[gb][:, :NT], AF.Silu, rd=[self.bres[gb]], wr=[sgres[s2]])
                self.tt('dve', aT[:, fc, :], B_[ub][:, :NT], sg[s2], ALU.mult, rd=[self.bres[ub], sgres[s2]], wr=[ares[fc]])
            for dc in range(8):
                s = dc % 2
                self.dma('sp', wd[s], dr['wd_b'][i, dc].rearrange("p (c f) -> p c f", c=NFC), rd=[self.rwd[(i, dc)]], wr=[wdres[s]])
                yb = 5 + dc % 2
                for fc in range(NFC):
                    self.mm(B_[yb][:, :NT], wd[s][:, fc, :], aT[:, fc, :], start=(fc == 0), stop=(fc == NFC - 1),
                            rd=[wdres[s], ares[fc]], wr=[self.bres[yb]])
                self.stt('dve', self.xT[:, dc, ts_], B_[yb][:, :NT], 0.5, self.xT[:, dc, ts_], ALU.mult, ALU.add,
                         rd=[self.bres[yb]] + self.xr(dc, t0, NT), wr=self.xr(dc, t0, NT))
        ar.release(m)

    def final_phase(self):
        k, nc, dr, ar = self.k, self.nc, self.dr, self.ar
        k.barrier()
        m = ar.mark()
        self.rr = [0, 1]
        sq = [ar.bf16(NT) for _ in range(2)]
        sqres = [Res(), Res()]
        rstd = ar.f32(NT)
        rres = Res()
        o = [ar.f32(8 * NT).rearrange("p (c t) -> p c t", c=8) for _ in range(2)]
        ores = [[Res() for _ in range(8)] for _ in range(2)]
        evs = []
        gcol = VLN * self.nlayers
        for tb in range(NTB):
            s = tb % 2
            t0 = tb * NT
            self.rmsnorm_to(t0, NT, gcol, o[s], ores[s], sq, sqres, rstd, rres)
            for c in range(8):
                evs.append(self.dma('sp', dr['outT'][c][:, t0:t0 + NT], o[s][:, c, :], rd=[ores[s][c]]))
        k.wait_all('sp', evs + self.dbg_evs)
        ar.release(m)

    def mix_phase(self, l):
        k, dr, ar = self.k, self.dr, self.ar
        k.barrier()
        m0 = ar.mark()
        self.rr = list(range(8))
        P = self.P = {}
        R = self.R = {}

        def alloc(name, kind, n):
            P[name] = ar.bf16(n) if kind == 'b' else ar.f32(n)
            R[name] = Res(name)
        alloc('pa', 'f', 11 * (NM + 1))
        alloc('Sf', 'f', 384)
        alloc('Sb', 'b', 384)
        alloc('rSf', 'f', 256)
        alloc('rSb', 'b', 256)
        alloc('cT', 'b', T)
        alloc('ctok', 'b', T)
        alloc('ik2', 'b', T)
        alloc('smw', 'b', 4 * 384)
        alloc('omka', 'f', 4)
        for nm in ('pa', 'Sf', 'Sb', 'rSf', 'rSb'):
            self.k.op('pool', lambda e, a=P[nm]: e.memset(a, 0.0), (), [R[nm]])
        self.dma('pool', P['smw'], dr['smw'][l], wr=[R['smw']])
        P['low'] = P['smw'][:, 0:384]
        P['v2'] = P['smw'][:, 384:768]
        P['wuk'] = P['smw'][:, 768:1152]
        P['wuv'] = P['smw'][:, 1152:1536]
        c = VLN * l + VL['ka']
        self.ts('dve', P['omka'][:, 0:3], self.vec[:, c:c + 3], -1.0, 1.0, ALU.mult, ALU.add, rd=[self.rvec], wr=[R['omka']])
        self.wring = [ar.bf16(1024) for _ in range(4)]
        self.wrres = [Res() for _ in range(4)]
        self.wri = 0
        for mb in range(NMB):
            self.mix_block(l, mb)
            if self.dbg.get('max_mb') is not None and mb >= self.dbg['max_mb']:
                break
        k.barrier()
        ar.release(m0)

    def proj(self, l, name, M, hT, hres):
        ci = CIDX[name]
        s = self.wri % 4
        self.wri += 1
        w = self.wring[s]
        self.dma('sp', w, self.dr['win_b'][l, ci], rd=[self.rwin[(l, ci)]], wr=[self.wrres[s]])
        b = self.nb()
        for kc in range(8):
            self.mm(self.banks[b][:M, :NM], w[:, kc * 128:kc * 128 + M], hT[:, kc, :], start=(kc == 0), stop=(kc == 7),
                    rd=[self.wrres[s], hres[kc]], wr=[self.bres[b]])
        return b

    def mix_block(self, l, mb):
        k, dr, ar = self.k, self.dr, self.ar
        t0 = mb * NM
        m = ar.mark()
        hT = ar.bf16(8 * NM).rearrange("p (c t) -> p c t", c=8)
        hres = [Res() for _ in range(8)]
        sq = [ar.bf16(NM) for _ in range(2)]
        sqres = [Res(), Res()]
        rstd = ar.f32(NM)
        rres = Res()
        oT = ar.bf16(8 * NM)
        ores = [Res() for _ in range(8)]
        self.rr = list(range(8))
        self.k.op('pool', lambda e: e.memset(oT, 0.0), (), ores)
        self.rmsnorm_to(t0, NM, VLN * l + VL['mix'], hT, hres, sq, sqres, rstd, rres)
        self.flush_casts((len(getattr(self, 'pending', [])) + (NMB - mb) - 1) // (NMB - mb))
        if 'a' in self.mixers:
            m1 = ar.mark()
            self.rwkv_block(l, mb, hT, hres, oT, ores)
            k.barrier()
            ar.release(m1)
        if 'c' in self.mixers:
            m1 = ar.mark()
            self.ret_block(l, mb, hT, hres, oT, ores)
            k.barrier()
            ar.release(m1)
        if 'b' in self.mixers:
            m1 = ar.mark()
            self.dsa_block(l, mb, hT, hres, oT, ores)
            k.barrier()
            ar.release(m1)
        if l == 0:
            self.dump('oT%d' % mb, oT, ores)
        self.rr = list(range(8))
        wo = [ar.bf16(1024) for _ in range(2)]
        wores = [Res(), Res()]
        for dc in range(8):
            s = dc % 2
            self.dma('sp', wo[s], dr['wout_b'][l, dc], rd=[self.rwout[(l, dc)]], wr=[wores[s]])
            b = self.nb()
            for mc in range(8):
                self.mm(self.banks[b][:, :NM], wo[s][:, mc * 128:(mc + 1) * 128], oT[:, mc * NM:(mc + 1) * NM],
                        start=(mc == 0), stop=(mc == 7), rd=[wores[s], ores[mc]], wr=[self.bres[b]])
            self.tt('dve', self.xT[:, dc, t0:t0 + NM], self.banks[b][:, :NM], self.xT[:, dc, t0:t0 + NM], ALU.add,
                    rd=[self.bres[b], self.xres[dc][mb]], wr=[self.xres[dc][mb]])
        k.barrier()
        ar.release(m)

    def rwkv_block(self, l, mb, hT, hres, oT, ores):
        ar, P, R, dr = self.ar, self.P, self.R, self.dr
        t0 = mb * NM
        N = NM
        B_ = self.banks
        bres = self.bres
        cf = self.cf
        rc = self.rconst
        pa = P['pa'].rearrange("p (c t) -> p c t", c=11)
        rpa = R['pa']
        nA = 11 if l > 0 else 10
        for j in range(nA):
            name = 'a%d' % j if j < 10 else 'mv'
            M = 128 if j < 10 else 16
            b = self.proj(l, name, M, hT, hres)
            self.cp('act', pa[:M, j, 1:N + 1], B_[b][:M, :N], rd=[bres[b]], wr=[rpa])
        xx = ar.f32(10 * N)
        rxx = [Res() for _ in range(10)]
        dtmp = [ar.f32(N) for _ in range(2)]
        rdt = [Res(), Res()]
        for j in range(10):
            s = j % 2
            self.tt('pool', dtmp[s], pa[:, j, 0:N], pa[:, j, 1:N + 1], ALU.subtract, rd=[rpa], wr=[rdt[s]])
            self.stt('dve', xx[:, j * N:(j + 1) * N], dtmp[s], self.vc(l, 'mu', j), pa[:, j, 1:N + 1], ALU.mult, ALU.add,
                     rd=[rdt[s], rpa, self.rvec], wr=[rxx[j]])
        if self.dbg.get('stop', 99) <= 1:
            return
        mv_b = ar.bf16(N)
        rmv = Res()
        vfb = None
        if l > 0:
            self.tt('pool', dtmp[0][:16], pa[:16, 10, 0:N], pa[:16, 10, 1:N + 1], ALU.subtract, rd=[rpa], wr=[rdt[0]])
            self.stt('dve', mv_b[:16], dtmp[0][:16], self.vc(l, 'mumv')[:16], pa[:16, 10, 1:N + 1], ALU.mult, ALU.add,
                     rd=[rdt[0], rpa, self.rvec], wr=[rmv])
            vfb = ar.f32(3 * N)
            rvfb = Res()
            self.dma('act', vfb.rearrange("p (c t) -> p c t", c=3), dr['vf'][:, :, t0:t0 + N], rd=[self.vfres[mb]], wr=[rvfb])
        for j in range(nA):
            M = 128 if j < 10 else 16
            self.cp('pool', pa[:M, j, 0:1], pa[:M, j, N:N + 1], rd=[], wr=[rpa])
        if self.dbg.get('stop', 99) <= 2:
            return
        lo = xx[:, 9 * N:10 * N]
        lo_b = ar.bf16(N)
        rlo = Res()
        self.act(lo_b[0:32], lo[0:32], AF.Tanh, rd=[rxx[9]], wr=[rlo])
        self.act(lo_b[32:64], lo[32:64], AF.Copy, rd=[rxx[9]], wr=[rlo])
        self.act(lo_b[64:128], lo[64:128], AF.Sigmoid, rd=[rxx[9]], wr=[rlo])
        low = P['low']
        rsw = R['smw']
        at_b = ar.bf16(3 * N)
        bt_b = ar.bf16(3 * N)
        kt_b = ar.bf16(3 * N)
        rt_b = ar.bf16(3 * N)
        gT = ar.bf16(3 * N)
        bonus = ar.f32(3 * N)
        rat, rbt, rkt, rrt, rg, rbo = [Res() for _ in range(6)]
        tok = [ar.bf16(3 * 384) for _ in range(NQ)]
        rtok = [Res() for _ in range(NQ)]
        Ptot = ar.f32(3 * NQ)
        rPt = Res()
        names = ['sgw', 'cs', 'csx', 'Pinc', 'Pexc', 'Pinv', 'Pend', 'a', 'nrm', 'kkn', 't1', 'kp', 'bb', 'sgv']
        tf = {n: ar.f32(N) for n in names}
        rf = {n: Res() for n in names}
        tb16 = {n: ar.bf16(N) for n in ('sqk', 'rk', 'bhT', 'khT', 'vb')}
        rb16 = {n: Res() for n in tb16}
        nbv = ar.f32(4)
        rnb = Res()
        for j in range(3):
            cs_ = slice(j * 128, (j + 1) * 128)
            fs = slice(j * N, (j + 1) * N)
            r_j = xx[:, (0 + j) * N:(1 + j) * N]
            k_j = xx[:, (3 + j) * N:(4 + j) * N]
            v_j = xx[:, (6 + j) * N:(7 + j) * N]
            rr_, rk_, rv_ = rxx[j], rxx[3 + j], rxx[6 + j]
            b = self.nb()
            self.mm(B_[b][:, :N], low[0:32, cs_], lo_b[0:32], rd=[rsw, rlo], wr=[bres[b]])
            self.act(tf['sgw'], B_[b][:, :N], AF.Sigmoid, rd=[bres[b], self.rvec], wr=[rf['sgw']], bias=self.vc(l, 'w0', j))
            self.k.op('dve', lambda e, o=tf['cs'], d0=cf[:, CF['reset']:CF['reset'] + N], d1=tf['sgw']: e.tensor_tensor_scan(
                out=o, data0=d0, data1=d1, initial=0.0, op0=ALU.mult, op1=ALU.add), [rf['sgw'], rc], [rf['cs']])
            self.tt('pool', tf['csx'], tf['cs'], tf['sgw'], ALU.subtract, rd=[rf['cs'], rf['sgw']], wr=[rf['csx']])
            self.act(tf['Pinc'], tf['cs'], AF.Exp, rd=[rf['cs']], wr=[rf['Pinc']], scale=-C0)
            self.act(tf['Pexc'], tf['csx'], AF.Exp, rd=[rf['csx']], wr=[rf['Pexc']], scale=-C0)
            self.act(tf['Pinv'], tf['cs'], AF.Exp, rd=[rf['cs']], wr=[rf['Pinv']], scale=C0)
            for q in range(NQ):
                self.ts('dve', nbv[:, q:q + 1], tf['cs'][:, q * 128 + 127:q * 128 + 128], -C0, None, ALU.mult, rd=[rf['cs']], wr=[rnb])
            for q in range(NQ):
                tq = slice(q * 128, (q + 1) * 128)
                self.act(tf['Pend'][:, tq], tf['cs'][:, tq], AF.Exp, rd=[rf['cs'], rnb], wr=[rf['Pend']], scale=C0, bias=nbv[:, q:q + 1])
            self.act(Ptot[:, j * NQ:(j + 1) * NQ], nbv[:, 0:NQ], AF.Exp, rd=[rnb], wr=[rPt])
            if self.dbg.get('stop', 99) <= 3:
                continue
            b = self.nb()
            self.mm(B_[b][:, :N], low[32:64, cs_], lo_b[32:64], rd=[rsw, rlo], wr=[bres[b]])
            self.act(tf['a'], B_[b][:, :N], AF.Sigmoid, rd=[bres[b], self.rvec], wr=[rf['a']], bias=self.vc(l, 'a0', j))
            b = self.nb()
            self.mm(B_[b][:, :N], low[64:128, cs_], lo_b[64:128], rd=[rsw, rlo], wr=[bres[b]])
            self.cp('act', gT[:, fs], B_[b][:, :N], rd=[bres[b]], wr=[rg])
            self.act(tb16['sqk'], k_j, AF.Square, rd=[rk_, self.rvec], wr=[rb16['sqk']], scale=self.vc(l, 'kk', j))
            b = self.nb()
            self.mm(B_[b][:, :N], self.bones_b, tb16['sqk'], rd=[rc, rb16['sqk']], wr=[bres[b]])
            self.act(tf['nrm'], B_[b][:, :N], AF.Sqrt, rd=[bres[b]], wr=[rf['nrm']])
            self.ts('dve', tf['nrm'], tf['nrm'], 1e-12, None, ALU.max, rd=[rf['nrm']], wr=[rf['nrm']])
            self.recip(tf['nrm'], tf['nrm'], rd=[rf['nrm']], wr=[rf['nrm']])
            self.stt('dve', tf['kkn'], k_j, self.vc(l, 'kk', j), tf['nrm'], ALU.mult, ALU.mult, rd=[rk_, self.rvec, rf['nrm']], wr=[rf['kkn']])
            self.ts('dve', tf['t1'], tf['a'], self.vc(l, 'ka', j), P['omka'][:, j:j + 1], ALU.mult, ALU.add,
                    rd=[rf['a'], self.rvec, R['omka']], wr=[rf['t1']])
            self.tt('pool', tf['kp'], tf['t1'], k_j, ALU.mult, rd=[rf['t1'], rk_], wr=[rf['kp']])
            if self.dbg.get('stop', 99) <= 4:
                continue
            self.stt('dve', at_b[:, fs], tf['kkn'], -1.0, tf['Pexc'], ALU.mult, ALU.mult, rd=[rf['kkn'], rf['Pexc']], wr=[rat])
            self.tt('pool', tf['bb'], tf['kkn'], tf['a'], ALU.mult, rd=[rf['kkn'], rf['a']], wr=[rf['bb']])
            self.tt('dve', bt_b[:, fs], tf['bb'], tf['Pinv'], ALU.mult, rd=[rf['bb'], rf['Pinv']], wr=[rbt])
            self.tt('pool', tb16['bhT'], tf['bb'], tf['Pend'], ALU.mult, rd=[rf['bb'], rf['Pend']], wr=[rb16['bhT']])
            self.tt('dve', kt_b[:, fs], tf['kp'], tf['Pinv'], ALU.mult, rd=[rf['kp'], rf['Pinv']], wr=[rkt])
            self.tt('pool', tb16['khT'], tf['kp'], tf['Pend'], ALU.mult, rd=[rf['kp'], rf['Pend']], wr=[rb16['khT']])
            self.tt('dve', rt_b[:, fs], r_j, tf['Pinc'], ALU.mult, rd=[rr_, rf['Pinc']], wr=[rrt])
            if l == 0:
                self.dma('act', dr['vf'][:, j, t0:t0 + N], v_j, rd=[rv_], wr=[self.vfres[mb]])
            else:
                b = self.nb()
                self.mm(B_[b][:, :N], P['v2'][0:16, cs_], mv_b[0:16], rd=[rsw, rmv], wr=[bres[b]])
                self.act(tf['sgv'], B_[b][:, :N], AF.Sigmoid, rd=[bres[b], self.rvec], wr=[rf['sgv']], bias=self.vc(l, 'v0', j))
                self.tt('pool', tf['t1'], vfb[:, fs], v_j, ALU.subtract, rd=[rvfb, rv_, rf['t1']], wr=[rf['t1']])
                self.tt('dve', tf['t1'], tf['t1'], tf['sgv'], ALU.mult, rd=[rf['t1'], rf['sgv']], wr=[rf['t1']])
                self.tt('pool', v_j, v_j, tf['t1'], ALU.add, rd=[rv_, rf['t1']], wr=[rv_])
            self.cp('pool', tb16['vb'], v_j, rd=[rv_], wr=[rb16['vb']])
            self.stt('dve', tb16['rk'], r_j, self.vc(l, 'rk', j), tf['kp'], ALU.mult, ALU.mult, rd=[rr_, self.rvec, rf['kp']], wr=[rb16['rk']])
            b = self.nb()
            self.mm(B_[b][:, :N], self.bones_b, tb16['rk'], rd=[rc, rb16['rk']], wr=[bres[b]])
            self.tt('dve', bonus[:, fs], B_[b][:, :N], v_j, ALU.mult, rd=[bres[b], rv_], wr=[rbo])
            if self.dbg.get('stop', 99) <= 5:
                continue
            for q in range(NQ):
                tq = slice(q * 128, (q + 1) * 128)
                b = self.nb()
                for x, nm in enumerate(('bhT', 'khT', 'vb')):
                    self.mm(B_[b][:, x * 128:(x + 1) * 128], tb16[nm][:, tq], self.ident_b, rd=[rb16[nm], rc], wr=[bres[b]])
                self.cp('act', tok[q].rearrange("p (x f) -> p x f", x=3)[:, :, cs_],
                        B_[b][:, 0:384].rearrange("p (x f) -> p x f", x=3), rd=[bres[b]], wr=[rtok[q]])
        if self.dbg.get('stop', 99) <= 6:
            return
        y_sb = ar.f32(3 * N)
        ry = Res()
        kinds = [('N', at_b, rat, bt_b, rbt, 'maskL'), ('Nt', bt_b, rbt, at_b, rat, 'maskU'), ('Aak', kt_b, rkt, at_b, rat, 'maskU'),
                 ('Arb', bt_b, rbt, rt_b, rrt, 'maskUi'), ('Ark', kt_b, rkt, rt_b, rrt, 'maskUi')]
        Am = {kd[0]: ar.bf16(768) for kd in kinds}
        rAm = {kd[0]: Res() for kd in kinds}
        Mx = [ar.bf16(768) for _ in range(2)]
        Mtx = [ar.bf16(768) for _ in range(2)]
        Qx = [ar.bf16(768) for _ in range(2)]
        rMx = [Res(), Res()]
        rMtx = [Res(), Res()]
        rQx = [Res(), Res()]
        W_sb = ar.bf16(384)
        U_sb = ar.bf16(384)
        rW, rU = Res(), Res()
        Sf, Sb = P['Sf'], P['Sb']
        rSf, rSb = R['Sf'], R['Sb']
        ident6 = self.cb[:, CB['ident']:CB['ident'] + 768]
        for q in range(NQ):
            tq = slice(q * 128, (q + 1) * 128)
            tokq = tok[q]
            for (nm, Lt, rL, Rt, rR, mk) in kinds:
                for e in range(2):
                    pr = slice(e * 64, (e + 1) * 64)
                    b = self.nb()
                    for j in range(3):
                        cols = slice(j * N + q * 128, j * N + (q + 1) * 128)
                        self.mm(B_[b][:, j * 128:(j + 1) * 128], Lt[pr, cols], Rt[pr, cols], rd=[rL, rR], wr=[bres[b]])
                    self.tt('dve', Am[nm].rearrange("p (j e t) -> p j e t", j=3, e=2)[:, :, e, :],
                            B_[b][:, 0:384].rearrange("p (j t) -> p j t", j=3),
                            cf[:, CF[mk]:CF[mk] + 384].rearrange("p (j t) -> p j t", j=3), ALU.mult,
                            rd=[bres[b], rc], wr=[rAm[nm]])
            if self.dbg.get('stop', 99) <= 7:
                continue
            Mc, Mtc, rMc, rMtc = Am['N'], Am['Nt'], rAm['N'], rAm['Nt']
            qi = 0
            self.tt('pool', Qx[qi], Am['Nt'], ident6, ALU.add, rd=[rAm['Nt'], rc], wr=[rQx[qi]])
            for lev in range(1, 7):
                mi = lev % 2
                for half in range(2):
                    hs = slice(half * 384, (half + 1) * 384)
                    b = self.nb()
                    for hh in range(3):
                        c_ = slice((half * 3 + hh) * 128, (half * 3 + hh + 1) * 128)
                        self.mm(B_[b][:, hh * 128:(hh + 1) * 128], Mtc[:, c_], Mc[:, c_], rd=[rMc, rMtc], wr=[bres[b]])
                    self.cp('act', Mx[mi][:, hs], B_[b][:, 0:384], rd=[bres[b]], wr=[rMx[mi]])
                    if lev < 6:
                        b = self.nb()
                        for hh in range(3):
                            c_ = slice((half * 3 + hh) * 128, (half * 3 + hh + 1) * 128)
                            self.mm(B_[b][:, hh * 128:(hh + 1) * 128], Mc[:, c_], Mtc[:, c_], rd=[rMc, rMtc], wr=[bres[b]])
                        self.cp('act', Mtx[mi][:, hs], B_[b][:, 0:384], rd=[bres[b]], wr=[rMtx[mi]])
                Mc, rMc = Mx[mi], rMx[mi]
                if lev < 6:
                    Mtc, rMtc = Mtx[mi], rMtx[mi]
                qn = 1 - qi
                for half in range(2):
                    hs = slice(half * 384, (half + 1) * 384)
                    b = self.nb()
                    for hh in range(3):
                        c_ = slice((half * 3 + hh) * 128, (half * 3 + hh + 1) * 128)
                        o_ = B_[b][:, hh * 128:(hh + 1) * 128]
                        self.mm(o_, self.ident_b, Qx[qi][:, c_], start=True, stop=False, rd=[rc, rQx[qi]], wr=[bres[b]])
                        self.mm(o_, Mc[:, c_], Qx[qi][:, c_], start=False, stop=True, rd=[rMc, rQx[qi]], wr=[bres[b]])
                    self.cp('dve', Qx[qn][:, hs], B_[b][:, 0:384], rd=[bres[b]], wr=[rQx[qn]])
                qi = qn
            Tt, rTt = Qx[qi], rQx[qi]
            if self.dbg.get('stop', 99) <= 8:
                continue
            b = self.nb()
            for j in range(3):
                cols = slice(j * N + q * 128, j * N + (q + 1) * 128)
                self.mm(B_[b][:, j * 128:(j + 1) * 128], at_b[:, cols], Sb[:, j * 128:(j + 1) * 128], start=True, stop=False,
                        rd=[rat, rSb], wr=[bres[b]])
                for e in range(2):
                    h = 2 * j + e
                    self.mm(B_[b][:, h * 64:(h + 1) * 64], Am['Aak'][:, h * 128:(h + 1) * 128], tokq[:, 768 + h * 64:768 + (h + 1) * 64],
                            start=False, stop=(e == 1), rd=[rAm['Aak'], rtok[q]], wr=[bres[b]])
            self.cp('act', W_sb, B_[b][:, 0:384], rd=[bres[b]], wr=[rW])
            b = self.nb()
            for h in range(6):
                self.mm(B_[b][:, h * 64:(h + 1) * 64], Tt[:, h * 128:(h + 1) * 128], W_sb[:, h * 64:(h + 1) * 64], rd=[rTt, rW], wr=[bres[b]])
            self.cp('dve', U_sb, B_[b][:, 0:384], rd=[bres[b]], wr=[rU])
            if self.dbg.get('stop', 99) <= 9:
                continue
            b = self.nb()
            for j in range(3):
                cols = slice(j * N + q * 128, j * N + (q + 1) * 128)
                self.mm(B_[b][:, j * 128:(j + 1) * 128], Sb[:, j * 128:(j + 1) * 128], rt_b[:, cols], start=True, stop=False,
                        rd=[rSb, rrt], wr=[bres[b]])
                for e in range(2):
                    h = 2 * j + e
                    pr = slice(e * 64, (e + 1) * 64)
                    o_ = B_[b][pr, j * 128:(j + 1) * 128]
                    self.mm(o_, U_sb[:, h * 64:(h + 1) * 64], Am['Arb'][:, h * 128:(h + 1) * 128], start=False, stop=False,
                            rd=[rU, rAm['Arb']], wr=[bres[b]])
                    self.mm(o_, tokq[:, 768 + h * 64:768 + (h + 1) * 64], Am['Ark'][:, h * 128:(h + 1) * 128], start=False, stop=True,
                            rd=[rtok[q], rAm['Ark']], wr=[bres[b]])
            self.cp('act', y_sb.rearrange("p (j t) -> p j t", j=3)[:, :, tq], B_[b][:, 0:384].rearrange("p (j t) -> p j t", j=3),
                    rd=[bres[b]], wr=[ry])
            if self.dbg.get('stop', 99) <= 10:
                continue
            b = self.nb()
            for h in range(6):
                j, e = h // 2, h % 2
                pr = slice(e * 64, (e + 1) * 64)
                o_ = B_[b][pr, j * 64:(j + 1) * 64]
                self.mm(o_, tokq[:, 0 + h * 64:0 + (h + 1) * 64], U_sb[:, h * 64:(h + 1) * 64], start=True, stop=False,
                        rd=[rtok[q], rU], wr=[bres[b]])
                self.mm(o_, tokq[:, 384 + h * 64:384 + (h + 1) * 64], tokq[:, 768 + h * 64:768 + (h + 1) * 64], start=False, stop=True,
                        rd=[rtok[q]], wr=[bres[b]])
            for j in range(3):
                for e in range(2):
                    pr = slice(e * 64, (e + 1) * 64)
                    sc = slice(j * 128 + e * 64, j * 128 + (e + 1) * 64)
                    self.stt('dve', Sf[pr, sc], Sf[pr, sc], Ptot[pr, j * NQ + q:j * NQ + q + 1],
                             B_[b][pr, j * 64:(j + 1) * 64], ALU.mult, ALU.add, rd=[rSf, rPt, bres[b]], wr=[rSf])
            self.cp('dve', Sb, Sf, rd=[rSf], wr=[rSb])
        if self.dbg.get('stop', 99) <= 11:
            return
        yb = ar.bf16(N)
        ryb = Res()
        yc = ar.f32(N)
        ryc = Res()
        sd = ar.f32(N)
        rsd = Res()
        for j in range(3):
            fs = slice(j * N, (j + 1) * N)
            yj = y_sb[:, fs]
            self.cp('act', yb, yj, rd=[ry], wr=[ryb])
            b = self.nb()
            self.mm(B_[b][:, :N], self.bmean_b, yb, rd=[rc, ryb], wr=[bres[b]])
            self.tt('dve', yc, yj, B_[b][:, :N], ALU.subtract, rd=[ry, bres[b]], wr=[ryc])
            self.act(yb, yc, AF.Square, rd=[ryc], wr=[ryb])
            b = self.nb()
            self.mm(B_[b][:, :N], self.bmean_b, yb, rd=[rc, ryb], wr=[bres[b]])
            self.act(sd, B_[b][:, :N], AF.Sqrt, rd=[bres[b]], wr=[rsd], bias=GN_EPS)
            self.recip(sd, sd, rd=[rsd], wr=[rsd])
            self.tt('dve', yc, yc, sd, ALU.mult, rd=[ryc, rsd], wr=[ryc])
            self.ts('dve', yc, yc, self.vc(l, 'lnw', j), self.vc(l, 'lnb', j), ALU.mult, ALU.add, rd=[ryc, self.rvec], wr=[ryc])
            self.tt('pool', yc, yc, bonus[:, fs], ALU.add, rd=[ryc, rbo], wr=[ryc])
            self.tt('dve', oT[:, j * N:(j + 1) * N], yc, gT[:, fs], ALU.mult, rd=[ryc, rg], wr=[ores[j]])

    def ret_block(self, l, mb, hT, hres, oT, ores):
        ar, P, R, dr = self.ar, self.P, self.R, self.dr
        t0 = mb * NM
        N = NM
        B_ = self.banks
        bres = self.bres
        cf = self.cf
        rc = self.rconst
        z = {}
        rz = {}
        for nm in ('cq', 'ck', 'cv', 'cg'):
            z[nm] = ar.f32(2 * N)
            rz[nm] = Res()
            for j in range(2):
                b = self.proj(l, nm + str(j), 128, hT, hres)
                self.cp('act', z[nm][:, j * N:(j + 1) * N], B_[b][:, :N], rd=[bres[b]], wr=[rz[nm]])
        rot = ar.f32(4 * N)
        rrot = Res()
        self.dma('act', rot.rearrange("p (c t) -> p c t", c=4), dr['rot'][:, :, t0:t0 + N], wr=[rrot])
        qr_b = ar.bf16(2 * N)
        qd_b = ar.bf16(2 * N)
        kr_b = ar.bf16(2 * N)
        kdT = ar.bf16(2 * N)
        cvb = ar.bf16(2 * N)
        rqr, rqd, rkr, rkd, rcvb = [Res() for _ in range(5)]
        t1 = ar.f32(N)
        t2 = ar.f32(N)
        zr = ar.f32(N)
        rt1, rt2, rzr = Res(), Res(), Res()
        prot = cf[:, CF['prot']:CF['prot'] + 128]
        for (nm, ci, si) in (('cq', 0, 1), ('ck', 2, 3)):
            for j in range(2):
                fs = slice(j * N, (j + 1) * N)
                zj = z[nm][:, fs]
                b = self.nb()
                self.mm(B_[b][:, :N], prot, zj, rd=[rc, rz[nm]], wr=[bres[b]])
                self.tt('pool', t1, zj, rot[:, ci * N:(ci + 1) * N], ALU.mult, rd=[rz[nm], rrot], wr=[rt1])
                self.tt('dve', t2, B_[b][:, :N], rot[:, si * N:(si + 1) * N], ALU.mult, rd=[bres[b], rrot], wr=[rt2])
                self.tt('dve', zr, t1, t2, ALU.add, rd=[rt1, rt2], wr=[rzr])
                if nm == 'cq':
                    self.cp('act', qr_b[:, fs], zr, rd=[rzr], wr=[rqr])
                    self.tt('pool', qd_b[:, fs], zr, cf[:, CF['qdec'] + j * N:CF['qdec'] + (j + 1) * N], ALU.mult, rd=[rzr, rc], wr=[rqd])
                else:
                    self.cp('act', kr_b[:, fs], zr, rd=[rzr], wr=[rkr])
                    self.tt('pool', kdT[:, fs], zr, cf[:, CF['kdec'] + j * N:CF['kdec'] + (j + 1) * N], ALU.mult, rd=[rzr, rc], wr=[rkd])
        self.cp('pool', cvb, z['cv'], rd=[rz['cv']], wr=[rcvb])
        tokr = [ar.bf16(512) for _ in range(NQ)]
        rtokr = [Res() for _ in range(NQ)]
        for q in range(NQ):
            b = self.nb()
            for x, (src, rs) in enumerate(((kdT, rkd), (cvb, rcvb))):
                for j in range(2):
                    self.mm(B_[b][:, (x * 2 + j) * 128:(x * 2 + j + 1) * 128], src[:, j * N + q * 128:j * N + (q + 1) * 128], self.ident_b,
                            rd=[rs, rc], wr=[bres[b]])
            self.cp('act', tokr[q], B_[b][:, 0:512], rd=[bres[b]], wr=[rtokr[q]])
        yret = ar.f32(2 * N)
        ryr = Res()
        sm = ar.bf16(512)
        rsm = Res()
        Sf, Sb = P['rSf'], P['rSb']
        rSf, rSb = R['rSf'], R['rSb']
        for q in range(NQ):
            tq = slice(q * 128, (q + 1) * 128)
            for e in range(2):
                pr = slice(e * 64, (e + 1) * 64)
                b = self.nb()
                for j in range(2):
                    cols = slice(j * N + q * 128, j * N + (q + 1) * 128)
                    self.mm(B_[b][:, j * 128:(j + 1) * 128], kr_b[pr, cols], qr_b[pr, cols], rd=[rkr, rqr], wr=[bres[b]])
                self.tt('dve', sm.rearrange("p (j e t) -> p j e t", j=2, e=2)[:, :, e, :],
                        B_[b][:, 0:256].rearrange("p (j t) -> p j t", j=2),
                        cf[:, CF['intraT']:CF['intraT'] + 512].rearrange("p (j e t) -> p j e t", j=2, e=2)[:, :, e, :], ALU.mult,
                        rd=[bres[b], rc], wr=[rsm])
            b = self.nb()
            for j in range(2):
                cols = slice(j * N + q * 128, j * N + (q + 1) * 128)
                self.mm(B_[b][:, j * 128:(j + 1) * 128], Sb[:, j * 128:(j + 1) * 128], qd_b[:, cols], start=True, stop=False,
                        rd=[rSb, rqd], wr=[bres[b]])
                for e in range(2):
                    h = 2 * j + e
                    pr = slice(e * 64, (e + 1) * 64)
                    self.mm(B_[b][pr, j * 128:(j + 1) * 128], tokr[q][:, 256 + h * 64:256 + (h + 1) * 64], sm[:, h * 128:(h + 1) * 128],
                            start=False, stop=True, rd=[rtokr[q], rsm], wr=[bres[b]])
            self.cp('act', yret.rearrange("p (j t) -> p j t", j=2)[:, :, tq], B_[b][:, 0:256].rearrange("p (j t) -> p j t", j=2),
                    rd=[bres[b]], wr=[ryr])
            b = self.nb()
            for h in range(4):
                j, e = h // 2, h % 2
                pr = slice(e * 64, (e + 1) * 64)
                self.mm(B_[b][pr, j * 64:(j + 1) * 64], tokr[q][:, h * 64:(h + 1) * 64], tokr[q][:, 256 + h * 64:256 + (h + 1) * 64],
                        rd=[rtokr[q]], wr=[bres[b]])
            for j in range(2):
                for e in range(2):
                    pr = slice(e * 64, (e + 1) * 64)
                    sc = slice(j * 128 + e * 64, j * 128 + (e + 1) * 64)
                    self.stt('dve', Sf[pr, sc], Sf[pr, sc], cf[pr, CF['cdec'] + j:CF['cdec'] + j + 1],
                             B_[b][pr, j * 64:(j + 1) * 64], ALU.mult, ALU.add, rd=[rSf, rc, bres[b]], wr=[rSf])
            self.cp('dve', Sb, Sf, rd=[rSf], wr=[rSb])
        sqb = ar.bf16(N)
        rsq = Res()
        sd = ar.f32(N)
        rsd = Res()
        sg = ar.f32(N)
        rsg = Res()
        for j in range(2):
            fs = slice(j * N, (j + 1) * N)
            self.act(sqb, yret[:, fs], AF.Square, rd=[ryr], wr=[rsq])
            b = self.nb()
            self.mm(B_[b][:, :N], self.bmean_b, sqb, rd=[rc, rsq], wr=[bres[b]])
            self.act(sd, B_[b][:, :N], AF.Sqrt, rd=[bres[b]], wr=[rsd], bias=EPS)
            self.recip(sd, sd, rd=[rsd], wr=[rsd])
            self.tt('dve', sd, sd, yret[:, fs], ALU.mult, rd=[rsd, ryr], wr=[rsd])
            self.act(sg, z['cg'][:, fs], AF.Silu, rd=[rz['cg']], wr=[rsg])
            self.tt('pool', oT[:, (6 + j) * N:(7 + j) * N], sd, sg, ALU.mult, rd=[rsd, rsg], wr=[ores[6 + j]])

    def dsa_block(self, l, mb, hT, hres, oT, ores):
        ar, P, R, dr = self.ar, self.P, self.R, self.dr
        t0 = mb * NM
        N = NM
        B_ = self.banks
        bres = self.bres
        cf = self.cf
        rc = self.rconst
        cT, ctok, ik2 = P['cT'], P['ctok'], P['ik2']
        rcT, rctok, rik2 = R['cT'], R['ctok'], R['ik2']
        self.rr = [0, 1, 2, 3]
        qT_b = ar.bf16(3 * N)
        rq = Res()
        for j in range(3):
            b = self.proj(l, 'bq%d' % j, 128, hT, hres)
            self.cp('act', qT_b[:, j * N:(j + 1) * N], B_[b][:, :N], rd=[bres[b]], wr=[rq])
        ckv = ar.f32(N)
        rckv = Res()
        b = self.proj(l, 'bc', 128, hT, hres)
        self.cp('act', ckv, B_[b][:, :N], rd=[bres[b]], wr=[rckv])
        sqb = ar.bf16(N)
        rsq = Res()
        sd = ar.f32(N)
        rsd = Res()
        self.act(sqb, ckv, AF.Square, rd=[rckv], wr=[rsq])
        b = self.nb()
        self.mm(B_[b][:, :N], self.ones_b, sqb, rd=[rc, rsq], wr=[bres[b]])
        self.act(sd, B_[b][:, :N], AF.Sqrt, rd=[bres[b]], wr=[rsd], bias=EPS, scale=1.0 / 128)
        self.recip(sd, sd, rd=[rsd], wr=[rsd])
        self.stt('dve', cT[:, t0:t0 + N], ckv, self.vc(l, 'kvn'), sd, ALU.mult, ALU.mult, rd=[rckv, self.rvec, rsd], wr=[rcT])
        for q in range(NQ):
            gq = t0 // 128 + q
            b = self.nb()
            self.mm(B_[b][:, :128], cT[:, gq * 128:(gq + 1) * 128], self.ident_b, rd=[rcT, rc], wr=[bres[b]])
            self.cp('act', ctok[:, gq * 128:(gq + 1) * 128], B_[b][:, :128], rd=[bres[b]], wr=[rctok])
        iq_b = ar.bf16(4 * N)
        riq = Res()
        for j in range(4):
            b = self.proj(l, 'biq%d' % j, 128, hT, hres)
            self.cp('act', iq_b[:, j * N:(j + 1) * N], B_[b][:, :N], rd=[bres[b]], wr=[riq])
        b = self.proj(l, 'bik2', 128, hT, hres)
        self.cp('act', ik2[:, t0:t0 + N], B_[b][:, :N], rd=[bres[b]], wr=[rik2])
        iw_f = ar.f32(N)
        riw = Res()
        b = self.proj(l, 'biw', 8, hT, hres)
        self.cp('act', iw_f[:8], B_[b][:8, :N], rd=[bres[b]], wr=[riw])
        iwbc = ar.f32(8 * N)
        riwbc = Res()
        SC = (8.0 ** -0.5) * (64.0 ** -0.5)
        for h8 in range(8):
            b = self.nb()
            self.mm(B_[b][:, :N], cf[0:8, CF['sel'] + h8 * 128:CF['sel'] + (h8 + 1) * 128], iw_f[:8], rd=[rc, riw], wr=[bres[b]])
            self.act(iwbc[:, h8 * N:(h8 + 1) * N], B_[b][:, :N], AF.Copy, rd=[bres[b]], wr=[riwbc], scale=SC)
        qlT = ar.bf16(NQ * 768)
        rql = Res()
        for h in range(6):
            j, e = h // 2, h % 2
            pr = slice(e * 64, (e + 1) * 64)
            b = self.nb()
            self.mm(B_[b][:, :N], P['wuk'][pr, j * 128:(j + 1) * 128], qT_b[pr, j * N:(j + 1) * N], rd=[R['smw'], rq], wr=[bres[b]])
            for q in range(NQ):
                self.act(qlT[:, (q * 6 + h) * 128:(q * 6 + h + 1) * 128], B_[b][:, q * 128:(q + 1) * 128], AF.Copy,
                         rd=[bres[b]], wr=[rql], scale=0.125)
        score = ar.f32(T)
        rsc = Res()
        junk = ar.bf16(T)
        rjk = Res()
        bs = ar.f32(8)
        rbs = Res()
        negm = ar.bf16(T)
        rng = Res()
        tmp = [ar.bf16(1024) for _ in range(2)]
        rtmp = [Res(), Res()]
        ex = [ar.bf16(768) for _ in range(2)]
        rex = [Res(), Res()]
        mx8 = ar.f32(8)
        rmx = Res()
        rden = ar.f32(768)
        rrd = Res()
        oln = ar.bf16(768)
        roln = Res()
        ident4 = self.cb[:, CB['ident']:CB['ident'] + 512]
        iwbc3 = iwbc.rearrange("p (h t) -> p h t", h=8)
        for q in range(NQ):
            gq = t0 // 128 + q
            nS = gq + 1
            ncols = nS * 128
            tq = slice(q * 128, (q + 1) * 128)
            self.rr = [0, 1, 2, 3]
            sb = None
            for si in range(nS):
                zb = [self.nb(), self.nb()]
                for h8 in range(8):
                    e = h8 % 2
                    pr = slice(e * 64, (e + 1) * 64)
                    jj = h8 // 2
                    self.mm(B_[zb[e]][:, jj * 128:(jj + 1) * 128], ik2[pr, si * 128:(si + 1) * 128],
                            iq_b[pr, jj * N + q * 128:jj * N + (q + 1) * 128], rd=[rik2, riq], wr=[bres[zb[e]]])
                ts_ = si % 2
                for half in range(2):
                    self.stt('dve', tmp[ts_].rearrange("p (j e t) -> p j e t", j=4, e=2)[:, :, half, :],
                             B_[zb[half]][:, 0:512].rearrange("p (h t) -> p h t", h=4), 0.0,
                             iwbc.rearrange("p (j e t) -> p j e t", j=4, e=2)[:, :, half, tq], ALU.max, ALU.mult,
                             rd=[bres[zb[half]], riwbc], wr=[rtmp[ts_]])
                if si % 4 == 0:
                    sb = 4 + (si // 4) % 2
                for h8 in range(8):
                    self.mm(B_[sb][:, (si % 4) * 128:(si % 4 + 1) * 128], tmp[ts_][:, h8 * 128:(h8 + 1) * 128], self.ident_b,
                            start=(h8 == 0), stop=(h8 == 7), rd=[rtmp[ts_], rc], wr=[bres[sb]])
                if si % 4 == 3 or si == nS - 1:
                    c0 = (si // 4) * 512
                    nc_ = (si % 4 + 1) * 128
                    self.cp('act', score[:, c0:c0 + nc_], B_[sb][:, 0:nc_], rd=[bres[sb]], wr=[rsc])
            self.tt('pool', score[:, gq * 128:(gq + 1) * 128], score[:, gq * 128:(gq + 1) * 128], cf[:, CF['cmask']:CF['cmask'] + 128],
                    ALU.add, rd=[rsc, rc], wr=[rsc])
            if gq >= 2 and not self.dbg.get('notopk'):
                sc_ = score[:, :ncols]
                self.k.op('dve', lambda e, o=bs[:, 1:2], i=sc_: e.tensor_reduce(out=o, in_=i, axis=AX.X, op=ALU.max), [rsc], [rbs])
                self.ts('dve', junk[:, :ncols], sc_, -1.0e4, None, ALU.max, rd=[rsc], wr=[rjk])
                self.k.op('dve', lambda e, o=bs[:, 0:1], i=junk[:, :ncols]: e.tensor_reduce(out=o, in_=i, axis=AX.X, op=ALU.min), [rjk], [rbs])
                self.ts('dve', bs[:, 0:1], bs[:, 0:1], -1.0, None, ALU.add, rd=[rbs], wr=[rbs])
                self.ts('dve', bs[:, 1:2], bs[:, 1:2], 1.0, None, ALU.add, rd=[rbs], wr=[rbs])
                self.tt('dve', bs[:, 1:2], bs[:, 1:2], bs[:, 0:1], ALU.subtract, rd=[rbs], wr=[rbs])
                NIT = 22
                for it in range(NIT):
                    c_ = 2.0 ** -(it + 1)
                    self.stt('dve', bs[:, 2:3], bs[:, 1:2], c_, bs[:, 0:1], ALU.mult, ALU.add, rd=[rbs], wr=[rbs])
                    self.k.op('dve', lambda e, o=junk[:, :ncols], i=sc_, m_=bs[:, 2:3], a_=bs[:, 3:4]: e.tensor_scalar(
                        out=o, in0=i, scalar1=m_, scalar2=0.0, op0=ALU.is_ge, op1=ALU.add, accum_out=a_), [rsc, rbs, rjk], [rjk, rbs])
                    self.ts('dve', bs[:, 4:5], bs[:, 3:4], 255.5, c_, ALU.is_ge, ALU.mult, rd=[rbs], wr=[rbs])
                    self.stt('dve', bs[:, 0:1], bs[:, 4:5], bs[:, 1:2], bs[:, 0:1], ALU.mult, ALU.add, rd=[rbs], wr=[rbs])
                thr = bs[:, 0:1]
                rmx = rbs
                self.ts('dve', negm[:, :ncols], score[:, :ncols], thr, -30000.0, ALU.is_lt, ALU.mult, rd=[rsc, rmx], wr=[rng])
            else:
                self.ts('dve', negm[:, :ncols], score[:, :ncols], -1e29, -30000.0, ALU.is_lt, ALU.mult, rd=[rsc], wr=[rng])
            for si in range(nS):
                s_ = slice(si * 128, (si + 1) * 128)
                la, lb = self.nb(), self.nb()
                xs = si % 2
                self.mm(B_[la][:, 0:512], cT[:, s_], qlT[:, q * 768:q * 768 + 512], start=True, stop=False, rd=[rcT, rql], wr=[bres[la]])
                self.mm(B_[la][:, 0:512], negm[:, s_], ident4, start=False, stop=True, rd=[rng, rc], wr=[bres[la]])
                self.mm(B_[lb][:, 0:256], cT[:, s_], qlT[:, q * 768 + 512:q * 768 + 768], start=True, stop=False, rd=[rcT, rql], wr=[bres[lb]])
                self.mm(B_[lb][:, 0:256], negm[:, s_], ident4[:, 0:256], start=False, stop=True, rd=[rng, rc], wr=[bres[lb]])
                self.act(ex[xs][:, 0:512], B_[la][:, 0:512], AF.Exp, rd=[bres[la]], wr=[rex[xs]])
                self.act(ex[xs][:, 512:768], B_[lb][:, 0:256], AF.Exp, rd=[bres[lb]], wr=[rex[xs]])
                st_, sp_ = (si == 0), (si == nS - 1)
                self.mm(B_[4][:, 0:512], ctok[:, s_], ex[xs][:, 0:512], start=st_, stop=sp_, rd=[rctok, rex[xs]], wr=[bres[4]])
                self.mm(B_[5][:, 0:256], ctok[:, s_], ex[xs][:, 512:768], start=st_, stop=sp_, rd=[rctok, rex[xs]], wr=[bres[5]])
                self.mm(B_[6][:, 0:512], self.ones_b, ex[xs][:, 0:512], start=st_, stop=sp_, rd=[rc, rex[xs]], wr=[bres[6]])
                self.mm(B_[7][:, 0:256], self.ones_b, ex[xs][:, 512:768], start=st_, stop=sp_, rd=[rc, rex[xs]], wr=[bres[7]])
            self.recip(rden[:, 0:512], B_[6][:, 0:512], rd=[bres[6]], wr=[rrd])
            self.recip(rden[:, 512:768], B_[7][:, 0:256], rd=[bres[7]], wr=[rrd])
            self.tt('dve', oln[:, 0:512], B_[4][:, 0:512], rden[:, 0:512], ALU.mult, rd=[bres[4], rrd], wr=[roln])
            self.tt('dve', oln[:, 512:768], B_[5][:, 0:256], rden[:, 512:768], ALU.mult, rd=[bres[5], rrd], wr=[roln])
            b = self.nb()
            for h in range(6):
                j, e = h // 2, h % 2
                pr = slice(e * 64, (e + 1) * 64)
                self.mm(B_[b][pr, j * 128:(j + 1) * 128], P['wuv'][:, h * 64:(h + 1) * 64], oln[:, h * 128:(h + 1) * 128],
                        rd=[R['smw'], roln], wr=[bres[b]])
            for j in range(3):
                self.cp('act', oT[:, (3 + j) * N + q * 128:(3 + j) * N + (q + 1) * 128], B_[b][:, j * 128:(j + 1) * 128],
                        rd=[bres[b]], wr=[ores[3 + j]])
        self.rr = list(range(8))


def _prep_ffn(wg, wu, wd):
    def gu(w):
        wp = np.zeros((D, DFFP), np.float32)
        wp[:, :DFF] = w
        return np.ascontiguousarray(wp.reshape(8, 128, NFC, 128).transpose(2, 1, 0, 3)).reshape(NFC, 128, 8 * 128)
    wdp = np.zeros((DFFP, D), np.float32)
    wdp[:DFF] = wd
    wdt = np.ascontiguousarray(wdp.reshape(NFC, 128, 8, 128).transpose(2, 1, 0, 3)).reshape(8, 128, NFC * 128)
    return np.stack([gu(wg), gu(wu)]), wdt


def _col(v, n):
    return np.ascontiguousarray(np.asarray(v, np.float32).reshape(n, 128).T)


def _consts():
    cf = np.zeros((128, CFN), np.float32)
    cb = np.zeros((128, CBN), np.float32)
    r = np.arange(128)[:, None]
    c = np.arange(128)[None, :]
    cf[:, CF['maskL']:CF['maskL'] + 384] = np.tile((c < r).astype(np.float32), (1, 3))
    cf[:, CF['maskU']:CF['maskU'] + 384] = np.tile((r < c).astype(np.float32), (1, 3))
    cf[:, CF['maskUi']:CF['maskUi'] + 384] = np.tile((r <= c).astype(np.float32), (1, 3))
    gam = 1.0 - 2.0 ** (-5.0 - np.arange(4, dtype=np.float64))
    lg = np.log(gam)
    for h in range(4):
        dm = (c - r).astype(np.float64)
        cf[:, CF['intraT'] + h * 128:CF['intraT'] + (h + 1) * 128] = np.where(dm >= 0, np.exp(np.maximum(dm, 0) * lg[h]), 0.0)
    p = np.arange(128)
    partner = np.where((p % 64) < 32, p + 32, p - 32)
    prot = np.zeros((128, 128), np.float32)
    prot[partner, p] = 1.0
    cf[:, CF['prot']:CF['prot'] + 128] = prot
    rs = np.ones((128, NM), np.float32)
    rs[:, 0::128] = 0.0
    cf[:, CF['reset']:CF['reset'] + NM] = rs
    cf[:, CF['cmask']:CF['cmask'] + 128] = np.where(c <= r, 0.0, -1e30)
    sel = np.zeros((128, 1024), np.float32)
    for h in range(8):
        sel[h, h * 128:(h + 1) * 128] = 1.0
    cf[:, CF['sel']:CF['sel'] + 1024] = sel
    n = (np.arange(NM) % 128).astype(np.float64)
    for j in range(2):
        hh = 2 * j + (p // 64)
        cf[:, CF['qdec'] + j * NM:CF['qdec'] + (j + 1) * NM] = np.exp((n[None, :] + 1.0) * lg[hh][:, None])
        cf[:, CF['kdec'] + j * NM:CF['kdec'] + (j + 1) * NM] = np.exp((127.0 - n[None, :]) * lg[hh][:, None])
        cf[:, CF['cdec'] + j] = np.exp(128.0 * lg[hh])
    cf[:, CF['negbig']] = -1e29
    cb[:, CB['ident']:CB['ident'] + 768] = np.tile(np.eye(128, dtype=np.float32), (1, 6))
    bo = np.zeros((128, 128), np.float32)
    bo[:64, :64] = 1.0
    bo[64:, 64:] = 1.0
    cb[:, CB['bones']:CB['bones'] + 128] = bo
    cb[:, CB['bmean']:CB['bmean'] + 128] = bo / 64.0
    cb[:, CB['ones']:CB['ones'] + 128] = 1.0
    half = 32
    theta = 10000.0 ** (-np.linspace(0.0, 1.0, half))
    i = p % 32
    ang = np.arange(T, dtype=np.float64)[None, :] * theta[i][:, None]
    ang32 = (np.arange(T, dtype=np.float32)[None, :] * theta.astype(np.float32)[i][:, None]).astype(np.float64)
    sign = np.where((p % 64) < 32, -1.0, 1.0)[:, None]
    rot = np.zeros((128, 4, T), np.float32)
    rot[:, 0] = np.cos(ang32)
    rot[:, 1] = np.sin(ang32) * sign
    rot[:, 2] = np.cos(ang32) * 0.125
    rot[:, 3] = np.sin(ang32) * sign * 0.125
    return cf, cb, rot


def make_inputs(bld, inp):
    nl = bld.nlayers
    wgu = np.zeros((nl * 2, 2, NFC, 128, 8 * 128), np.float32)
    wd = np.zeros((nl * 2, 8, 128, NFC * 128), np.float32)
    win = np.zeros((nl, NCH, 128, 8 * 128), np.float32)
    wout = np.zeros((nl, 8, 128, 8 * 128), np.float32)
    smw = np.zeros((nl, 128, 4 * 384), np.float32)
    vec = np.zeros((128, VLN * nl + 8), np.float32)
    for l in range(nl):
        for j, nm in enumerate(('ffn1', 'ffn2')):
            a, b = _prep_ffn(inp[nm + '_w_gate'][l], inp[nm + '_w_up'][l], inp[nm + '_w_down'][l])
            wgu[2 * l + j] = a
            wd[2 * l + j] = b
        w_in = inp['w_in'][l]
        for ci, (name, cols) in enumerate(CHUNKS):
            if cols == 'mv':
                if l == 0:
                    continue
                src = inp['rwkv_vres_w_in'][l - 1]
            else:
                src = w_in[:, cols]
            M = src.shape[1]
            img = np.zeros((8, 128, 128), np.float32)
            img[:, :, :M] = src.reshape(8, 128, M)
            win[l, ci] = img.transpose(1, 0, 2).reshape(128, 8 * 128)
        wo = inp['w_out'][l]
        wout[l] = wo.reshape(8, 128, 8, 128).transpose(2, 1, 0, 3).reshape(8, 128, 8 * 128)
        smw[l, 0:32, 0:384] = inp['rwkv_w2'][l]
        smw[l, 32:64, 0:384] = inp['rwkv_a2'][l]
        smw[l, 64:128, 0:384] = inp['rwkv_g2'][l]
        if l > 0:
            smw[l, 0:16, 384:768] = inp['rwkv_v2'][l - 1]
        wuk = inp['dsa_w_uk'][l]
        for h in range(6):
            j, e = h // 2, h % 2
            smw[l, e * 64:(e + 1) * 64, 768 + j * 128:768 + (j + 1) * 128] = wuk[h]
        wuv = inp['dsa_w_uv'][l]
        for h in range(6):
            smw[l, :, 1152 + h * 64:1152 + (h + 1) * 64] = wuv[h]
        o = VLN * l
        vec[:, o + VL['ffn1']:o + VL['ffn1'] + 8] = _col(inp['ffn1_norm'][l], 8)
        vec[:, o + VL['ffn2']:o + VL['ffn2'] + 8] = _col(inp['ffn2_norm'][l], 8)
        vec[:, o + VL['mix']:o + VL['mix'] + 8] = _col(inp['mix_norm'][l], 8)
        vec[:, o + VL['mu']:o + VL['mu'] + 10] = _col(inp['rwkv_mu'][l], 10)
        if l > 0:
            vec[:16, o + VL['mumv']] = inp['rwkv_vres_mu'][l - 1]
            vec[:, o + VL['v0']:o + VL['v0'] + 3] = _col(inp['rwkv_v0'][l - 1], 3)
        vec[:, o + VL['w0']:o + VL['w0'] + 3] = _col(inp['rwkv_w0'][l], 3)
        vec[:, o + VL['a0']:o + VL['a0'] + 3] = _col(inp['rwkv_a0'][l], 3)
        vec[:, o + VL['kk']:o + VL['kk'] + 3] = _col(inp['rwkv_k_k'][l], 3)
        vec[:, o + VL['ka']:o + VL['ka'] + 3] = _col(inp['rwkv_k_a'][l], 3)
        vec[:, o + VL['rk']:o + VL['rk'] + 3] = _col(inp['rwkv_r_k'][l].reshape(-1), 3)
        vec[:, o + VL['lnw']:o + VL['lnw'] + 3] = _col(inp['rwkv_ln_w'][l], 3)
        vec[:, o + VL['lnb']:o + VL['lnb'] + 3] = _col(inp['rwkv_ln_b'][l], 3)
        vec[:, o + VL['kvn']] = inp['dsa_kv_norm'][l]
    vec[:, VLN * nl:VLN * nl + 8] = _col(inp['final_norm'], 8)
    cf, cb, rot = _consts()
    return {'wgu': wgu, 'wd': wd, 'win': win, 'wout': wout, 'smw': smw, 'vec': vec, 'cf': cf, 'cb': cb, 'rot': rot}


def kernel(**inputs):
    inp = {k_: np.asarray(v) for k_, v in inputs.items()}
    bld = B()
    nc = bld.build()
    shared = make_inputs(bld, inp)
    x = inp['x']
    in_maps = []
    for b in range(8):
        m = dict(shared)
        m['xT'] = np.ascontiguousarray(x[b].T).reshape(8, 128, T)
        in_maps.append(m)
    res = run_bass_kernel_spmd(nc, in_maps, core_ids=list(range(8)))
    out = np.stack([np.ascontiguousarray(np.asarray(r['outT']).reshape(D, T).T) for r in res.results])
    return out.astype(np.float32)
```

```python
import math
from contextlib import ExitStack
import numpy as np
import concourse.bass as bass
import concourse.mybir as mybir
from concourse.bass_utils import run_bass_kernel_spmd

F32 = mybir.dt.float32
BF16 = mybir.dt.bfloat16
ALU = mybir.AluOpType
AF = mybir.ActivationFunctionType
AX = mybir.AxisListType

ENG = ('pe', 'act', 'dve', 'pool', 'sp')

D = 1024
T = 2048
L = 2
DFF = 2752
DFFP = 2816
NFC = 22
NT = 512
NTB = T // NT
NM = 256
NMB = T // NM
NQ = NM // 128
EPS = 1e-6
GN_EPS = 64e-5
C0 = math.exp(-0.5)
NCH = 29
NIT_TOPK = 12


class Res:
    __slots__ = ('name', 'w', 'r')
    REG = []
    INIT = []

    def __init__(self, name=''):
        self.name = name
        self.w = None
        self.r = list(Res.INIT)


def soft_barrier(k):
    evs = [(('e', e), k.cnt[e]) for e in ENG if k.cnt[e] > 0]
    evs += list(k.last_dma.values())
    Res.INIT = evs
    Res.REG = []


class KB:
    def __init__(self, nc, same_engine_sync=True, dma_ring=8):
        self.nc = nc
        self.ops = {e: [] for e in ENG}
        self.cnt = {e: 0 for e in ENG}
        self.waited = {e: {} for e in ENG}
        self.same = same_engine_sync
        self.same_only = None
        self.ring = dma_ring
        self.dma_n = {e: 0 for e in ENG}
        self.semh = {}
        self.semkeys = [('e', e) for e in ENG]
        for e in ('sp', 'pool', 'act'):
            for i in range(dma_ring):
                self.semkeys.append(('d', e, i))
        self.last_dma = {}
        self.n_wait = 0

    def _wait(self, eng, ev):
        if ev is None:
            return
        key, val = ev
        if key == ('e', eng) and (eng == 'pe' or not self.same or (self.same_only is not None and eng not in self.same_only)):
            return
        cur = self.waited[eng].get(key, 0)
        if cur >= val:
            return
        self.waited[eng][key] = val
        self.n_wait += 1
        self.ops[eng].append(('wait', key, val))

    def _deps(self, eng, reads, writes):
        for r in reads:
            self._wait(eng, r.w)
        for w in writes:
            self._wait(eng, w.w)
            for ev in w.r:
                self._wait(eng, ev)

    def _commit(self, ev, reads, writes):
        for w in writes:
            w.w = ev
            w.r = []
        for r in reads:
            if r not in writes:
                r.r.append(ev)
                if len(r.r) > 64:
                    r.r = r.r[-64:] if False else r.r

    def op(self, eng, fn, reads=(), writes=()):
        reads = list(reads)
        writes = list(writes)
        self._deps(eng, reads, writes)
        self.cnt[eng] += 1
        ev = (('e', eng), self.cnt[eng])
        self.ops[eng].append(('op', fn, ('e', eng), 1))
        self._commit(ev, reads, writes)
        return ev

    def dma(self, q, fn, reads=(), writes=()):
        reads = list(reads)
        writes = list(writes)
        self._deps(q, reads, writes)
        j = self.dma_n[q]
        self.dma_n[q] += 1
        slot = j % self.ring
        key = ('d', q, slot)
        tgt = 16 * (j // self.ring + 1)
        if j >= self.ring:
            self._wait(q, (key, tgt - 16))
        ev = (key, tgt)
        self.last_dma[key] = ev
        self.ops[q].append(('op', fn, key, 16))
        self._commit(ev, reads, writes)
        return ev

    def wait_all(self, eng, evs):
        for ev in evs:
            self._wait(eng, ev)

    def barrier(self):
        Res.INIT = []
        Res.REG = []
        evs = [(('e', e), self.cnt[e]) for e in ENG if self.cnt[e] > 0]
        evs += list(self.last_dma.values())
        for e in ENG:
            for ev in evs:
                self._wait(e, ev)

    def finalize(self, stack):
        nc = self.nc
        for kk in self.semkeys:
            self.semh[kk] = stack.enter_context(nc.semaphore('s_' + '_'.join(str(x) for x in kk)))
        block = stack.enter_context(nc.Block())
        semh = self.semh

        def run(eng_name):
            def body(eng):
                for it in self.ops[eng_name]:
                    if it[0] == 'wait':
                        eng.wait_ge(semh[it[1]], it[2])
                    else:
                        ins = it[1](eng)
                        ins.then_inc(semh[it[2]], it[3])
            return body

        block.tensor(run('pe'))
        block.scalar(run('act'))
        block.vector(run('dve'))
        block.gpsimd(run('pool'))
        block.sync(run('sp'))


class Arena:
    def __init__(self, ap, nwords):
        self.ap = ap
        self.n = nwords
        self.top = 0
        self.peak = 0

    def mark(self):
        return self.top

    def release(self, m):
        self.top = m

    def f32(self, n):
        a = self.ap[:, self.top:self.top + n]
        self.top += n
        self.peak = max(self.peak, self.top)
        assert self.top <= self.n, ("arena overflow", self.top, self.n)
        return a

    def bf16(self, n):
        w = (n + 1) // 2
        return self.f32(w).bitcast(BF16)[:, 0:n]


def win_chunks():
    ch = []
    for j in range(10):
        ch.append(('a%d' % j, list(range(j * 128, (j + 1) * 128))))
    ch.append(('mv', 'mv'))
    for j in range(3):
        ch.append(('bq%d' % j, list(range(1280 + j * 128, 1280 + (j + 1) * 128))))
    ch.append(('bc', list(range(1664, 1792))))
    for j in range(4):
        ch.append(('biq%d' % j, list(range(1792 + j * 128, 1792 + (j + 1) * 128))))
    ch.append(('bik2', list(range(2304, 2368)) * 2))
    ch.append(('biw', list(range(2368, 2376))))
    for n, base in (('cq', 2376), ('ck', 2632), ('cv', 2888), ('cg', 3144)):
        for j in range(2):
            ch.append((n + str(j), list(range(base + j * 128, base + (j + 1) * 128))))
    return ch


CHUNKS = win_chunks()
CIDX = {c[0]: i for i, c in enumerate(CHUNKS)}

VL = {}
_o = 0
for _n, _w in (('ffn1', 8), ('ffn2', 8), ('mix', 8), ('mu', 10), ('mumv', 1), ('w0', 3), ('a0', 3), ('kk', 3), ('ka', 3),
               ('rk', 3), ('lnw', 3), ('lnb', 3), ('v0', 3), ('kvn', 1)):
    VL[_n] = _o
    _o += _w
VLN = _o

CF = {}
_o = 0
for _n, _w in (('maskL', 384), ('maskU', 384), ('maskUi', 384), ('intraT', 512), ('prot', 128), ('reset', NM), ('cmask', 128),
               ('sel', 1024), ('qdec', 2 * NM), ('kdec', 2 * NM), ('cdec', 2), ('negbig', 1)):
    CF[_n] = _o
    _o += _w
CFN = _o
CB = {}
_o = 0
for _n, _w in (('ident', 768), ('bones', 128), ('bmean', 128), ('ones', 128)):
    CB[_n] = _o
    _o += _w
CBN = _o


class B:
    def __init__(self, nlayers=L, do_ffn=True, do_mix=True, dbg=None, mixers=('a', 'b', 'c')):
        self.nlayers = nlayers
        self.do_ffn = do_ffn
        self.do_mix = do_mix
        self.dbg = dbg or {}
        self.mixers = mixers

    def mm(self, out, lhsT, rhs, start=True, stop=True, rd=(), wr=()):
        return self.k.op('pe', lambda e: e.matmul(out, lhsT=lhsT, rhs=rhs, start=start, stop=stop), rd, wr)

    def act(self, out, in_, func, rd=(), wr=(), **kw):
        return self.k.op('act', lambda e: e.activation(out=out, in_=in_, func=func, **kw), rd, wr)

    def tt(self, eng, out, in0, in1, op, rd=(), wr=()):
        return self.k.op(eng, lambda e: e.tensor_tensor(out=out, in0=in0, in1=in1, op=op), rd, wr)

    def stt(self, eng, out, in0, scalar, in1, op0, op1, rd=(), wr=()):
        return self.k.op(eng, lambda e: e.scalar_tensor_tensor(out=out, in0=in0, scalar=scalar, in1=in1, op0=op0, op1=op1), rd, wr)

    def ts(self, eng, out, in0, s1, s2, op0, op1=None, rd=(), wr=()):
        if op1 is None:
            return self.k.op(eng, lambda e: e.tensor_scalar(out=out, in0=in0, scalar1=s1, scalar2=None, op0=op0), rd, wr)
        return self.k.op(eng, lambda e: e.tensor_scalar(out=out, in0=in0, scalar1=s1, scalar2=s2, op0=op0, op1=op1), rd, wr)

    def cp(self, eng, out, in_, rd=(), wr=()):
        if eng == 'act':
            return self.k.op('act', lambda e: e.activation(out=out, in_=in_, func=AF.Copy), rd, wr)
        return self.k.op(eng, lambda e: e.tensor_copy(out=out, in_=in_), rd, wr)

    def recip(self, out, in_, rd=(), wr=()):
        return self.k.op('dve', lambda e: e.reciprocal(out=out, in_=in_), rd, wr)

    def dma(self, q, out, in_, rd=(), wr=()):
        return self.k.dma(q, lambda e: e.dma_start(out=out, in_=in_), rd, wr)

    def nb(self):
        b = self.rr[self.rri % len(self.rr)]
        self.rri += 1
        return b

    def dump(self, name, ap, reads):
        if name not in self.dbg:
            return
        shape = list(ap.shape)
        d = self.nc.dram_tensor("dbg_" + name, shape, ap.dtype, kind="ExternalOutput").ap()
        ev = self.k.dma('sp', lambda e: e.dma_start(out=d, in_=ap), reads=reads)
        self.dbg_evs.append(ev)

    def build(self):
        nc = bass.Bass("TRN2", target_bir_lowering=False)
        self.nc = nc
        nl = self.nlayers
        Res.REG = []
        Res.INIT = []
        dr = {}
        dr['xT'] = nc.dram_tensor("xT", [8, 128, T], F32, kind="ExternalInput").ap()
        dr['outT'] = nc.dram_tensor("outT", [8, 128, T], F32, kind="ExternalOutput").ap()
        dr['wgu'] = nc.dram_tensor("wgu", [nl * 2, 2, NFC, 128, 8 * 128], F32, kind="ExternalInput").ap()
        dr['wd'] = nc.dram_tensor("wd", [nl * 2, 8, 128, NFC * 128], F32, kind="ExternalInput").ap()
        dr['wgu_b'] = nc.dram_tensor("wgu_b", [nl * 2, 2, NFC, 128, 8 * 128], BF16, kind="Internal").ap()
        dr['wd_b'] = nc.dram_tensor("wd_b", [nl * 2, 8, 128, NFC * 128], BF16, kind="Internal").ap()
        dr['win'] = nc.dram_tensor("win", [nl, NCH, 128, 8 * 128], F32, kind="ExternalInput").ap()
        dr['win_b'] = nc.dram_tensor("win_b", [nl, NCH, 128, 8 * 128], BF16, kind="Internal").ap()
        dr['wout'] = nc.dram_tensor("wout", [nl, 8, 128, 8 * 128], F32, kind="ExternalInput").ap()
        dr['wout_b'] = nc.dram_tensor("wout_b", [nl, 8, 128, 8 * 128], BF16, kind="Internal").ap()
        dr['smw'] = nc.dram_tensor("smw", [nl, 128, 384 + 384 + 384 + 384], F32, kind="ExternalInput").ap()
        dr['vec'] = nc.dram_tensor("vec", [128, VLN * nl + 8], F32, kind="ExternalInput").ap()
        dr['cf'] = nc.dram_tensor("cf", [128, CFN], F32, kind="ExternalInput").ap()
        dr['cb'] = nc.dram_tensor("cb", [128, CBN], F32, kind="ExternalInput").ap()
        dr['rot'] = nc.dram_tensor("rot", [128, 4, T], F32, kind="ExternalInput").ap()
        dr['vf'] = nc.dram_tensor("vf", [128, 3, T], F32, kind="Internal").ap()
        self.dr = dr

        with ExitStack() as st:
            self.st = st
            k = KB(nc, same_engine_sync=not self.dbg.get('nosame'))
            k.same_only = self.dbg.get('same_only')
            self.k = k
            self.xT = st.enter_context(nc.sbuf_tensor("xT_sb", [128, 8, T], F32))
            self.xres = [[Res() for _ in range(NMB)] for _ in range(8)]
            self.vec = st.enter_context(nc.sbuf_tensor("vec_sb", [128, VLN * nl + 8], F32))
            self.rvec = Res()
            self.cf = st.enter_context(nc.sbuf_tensor("cf_sb", [128, CFN], F32))
            self.cb = st.enter_context(nc.sbuf_tensor("cb_sb", [128, CBN], BF16))
            self.rconst = Res()
            self.ones_b = self.cb[:, CB['ones']:CB['ones'] + 128]
            self.ident_b = self.cb[:, CB['ident']:CB['ident'] + 128]
            self.bones_b = self.cb[:, CB['bones']:CB['bones'] + 128]
            self.bmean_b = self.cb[:, CB['bmean']:CB['bmean'] + 128]
            rem = nc.sbuf_bytes_remaining
            AW = (rem - 2048) // 4
            arena_t = st.enter_context(nc.sbuf_tensor("arena", [128, AW], F32))
            self.ar = Arena(arena_t, AW)
            self.banks = [st.enter_context(nc.psum_tensor("bank%d" % i, [128, 512], F32)) for i in range(8)]
            self.bres = [Res() for _ in range(8)]
            self.rr = list(range(8))
            self.rri = 0
            self.dbg_evs = []
            self.vfres = [Res() for _ in range(NMB)]

            self.prologue()
            for l in range(nl):
                if self.do_ffn:
                    self.ffn_phase(l, 0)
                self.cast_layer(l + 1, defer=True)
                if self.do_mix:
                    self.mix_phase(l)
                self.flush_casts()
                if self.do_ffn and not self.dbg.get('skip_ffn2'):
                    self.ffn_phase(l, 1)
            self.final_phase()
            k.finalize(st)
        return nc

    def vc(self, l, name, j=0):
        c = VLN * l + VL[name] + j
        return self.vec[:, c:c + 1]

    def xr(self, c, t0, n):
        return [self.xres[c][b] for b in range(t0 // NM, (t0 + n) // NM)]

    def prologue(self):
        k, nc, dr = self.k, self.nc, self.dr
        for c in range(8):
            self.dma('sp', self.xT[:, c, :], dr['xT'][c], wr=[self.xres[c][b] for b in range(NMB)])
        self.dma('act', self.vec[:], dr['vec'][:, :], wr=[self.rvec])
        self.dma('act', self.cf[:], dr['cf'][:, :], wr=[self.rconst])
        self.dma('pool', self.cb[:], dr['cb'][:, :], wr=[self.rconst])
        self.rwgu = {}
        self.rwd = {}
        self.rwin = {}
        self.rwout = {}
        self.cast_layer(0)

    def cast_layer(self, l, defer=False):
        if l >= self.nlayers:
            return
        th = []
        if self.do_ffn:
            th += self.cast_ffn(2 * l)
        if self.do_mix:
            th += self.cast_mix(l)
        if self.do_ffn and not self.dbg.get('skip_ffn2'):
            th += self.cast_ffn(2 * l + 1)
        if defer:
            self.pending = th
        else:
            for f in th:
                f()

    def flush_casts(self, n=None):
        p = getattr(self, 'pending', [])
        n = len(p) if n is None else min(n, len(p))
        for f in p[:n]:
            f()
        self.pending = p[n:]

    def cast_ffn(self, i):
        dr = self.dr
        th = []
        for gu in range(2):
            for fc in range(NFC):
                r = Res()
                self.rwgu[(i, gu, fc)] = r
                th.append(lambda i=i, gu=gu, fc=fc, r=r: self.dma('pool', dr['wgu_b'][i, gu, fc], dr['wgu'][i, gu, fc], wr=[r]))
        for dc in range(8):
            r = Res()
            self.rwd[(i, dc)] = r
            th.append(lambda i=i, dc=dc, r=r: self.dma('pool', dr['wd_b'][i, dc].rearrange("p (a f) -> (p a) f", a=2),
                                                      dr['wd'][i, dc].rearrange("p (a f) -> (p a) f", a=2), wr=[r]))
        return th

    def cast_mix(self, l):
        dr = self.dr
        th = []
        for ci in range(NCH):
            r = Res()
            self.rwin[(l, ci)] = r
            th.append(lambda l=l, ci=ci, r=r: self.dma('pool', dr['win_b'][l, ci], dr['win'][l, ci], wr=[r]))
        for dc in range(8):
            r = Res()
            self.rwout[(l, dc)] = r
            th.append(lambda l=l, dc=dc, r=r: self.dma('pool', dr['wout_b'][l, dc], dr['wout'][l, dc], wr=[r]))
        return th

    def rmsnorm_to(self, t0, n, gcol, hT, hres, sq, sqres, rstd, rres):
        k = self.k
        ts_ = slice(t0, t0 + n)
        bank = self.nb()
        ps = self.banks[bank]
        for c in range(8):
            s = c % 2
            self.act(sq[s], self.xT[:, c, ts_], AF.Square, rd=self.xr(c, t0, n), wr=[sqres[s]])
            self.mm(ps[:, :n], self.ones_b, sq[s], start=(c == 0), stop=(c == 7), rd=[sqres[s], self.rconst], wr=[self.bres[bank]])
        self.act(rstd, ps[:, :n], AF.Sqrt, rd=[self.bres[bank]], wr=[rres], bias=EPS, scale=1.0 / D)
        self.recip(rstd, rstd, rd=[rres], wr=[rres])
        for c in range(8):
            self.stt('dve', hT[:, c, :], self.xT[:, c, ts_], self.vec[:, gcol + c:gcol + c + 1], rstd, ALU.mult, ALU.mult,
                     rd=self.xr(c, t0, n) + [rres, self.rvec], wr=[hres[c]])

    def ffn_phase(self, l, j):
        k, nc, dr, ar = self.k, self.nc, self.dr, self.ar
        i = 2 * l + j
        k.barrier()
        m = ar.mark()
        self.rr = [0]
        hTs = [ar.bf16(8 * NT).rearrange("p (c t) -> p c t", c=8) for _ in range(2)]
        hress = [[Res() for _ in range(8)] for _ in range(2)]
        aT = ar.bf16(NFC * NT).rearrange("p (c t) -> p c t", c=NFC)
        ares = [Res() for _ in range(NFC)]
        sq = [ar.bf16(NT) for _ in range(2)]
        sqres = [Res(), Res()]
        rstd = ar.f32(NT)
        rres = Res()
        NW = 3
        wg = [ar.bf16(8 * 128).rearrange("p (c f) -> p c f", c=8) for _ in range(NW)]
        wu = [ar.bf16(8 * 128).rearrange("p (c f) -> p c f", c=8) for _ in range(NW)]
        wgres = [Res() for _ in range(NW)]
        wures = [Res() for _ in range(NW)]
        wd = [ar.bf16(NFC * 128).rearrange("p (c f) -> p c f", c=NFC) for _ in range(2)]
        wdres = [Res(), Res()]
        sg = [ar.bf16(NT) for _ in range(2)]
        sgres = [Res(), Res()]
        gcol = VLN * l + VL['ffn1' if j == 0 else 'ffn2']
        B_ = self.banks
        self.rmsnorm_to(0, NT, gcol, hTs[0], hress[0], sq, sqres, rstd, rres)
        for tb in range(NTB):
            t0 = tb * NT
            ts_ = slice(t0, t0 + NT)
            hT, hres = hTs[tb % 2], hress[tb % 2]
            for fc in range(NFC):
                s = fc % NW
                self.dma('sp', wg[s], dr['wgu_b'][i, 0, fc].rearrange("p (c f) -> p c f", c=8), rd=[self.rwgu[(i, 0, fc)]], wr=[wgres[s]])
                self.dma('sp', wu[s], dr['wgu_b'][i, 1, fc].rearrange("p (c f) -> p c f", c=8), rd=[self.rwgu[(i, 1, fc)]], wr=[wures[s]])
                gb = 1 + fc % 2
                ub = 3 + fc % 2
                for kc in range(8):
                    self.mm(B_[gb][:, :NT], wg[s][:, kc, :], hT[:, kc, :], start=(kc == 0), stop=(kc == 7),
                            rd=[wgres[s], hres[kc]], wr=[self.bres[gb]])
                for kc in range(8):
                    self.mm(B_[ub][:, :NT], wu[s][:, kc, :], hT[:, kc, :], start=(kc == 0), stop=(kc == 7),
                            rd=[wures[s], hres[kc]], wr=[self.bres[ub]])
                s2 = fc % 2
                self.act(sg[s2], B_[gb][:, :NT], AF.Silu, rd=[self.bres[gb]], wr=[sgres[s2]])
                self.tt('dve', aT[:, fc, :], B_[ub][:, :NT], sg[s2], ALU.mult, rd=[self.bres[ub], sgres[s2]], wr=[ares[fc]])
            if tb + 1 < NTB:
                self.rmsnorm_to(t0 + NT, NT, gcol, hTs[(tb + 1) % 2], hress[(tb + 1) % 2], sq, sqres, rstd, rres)
            for dc in range(8):
                s = dc % 2
                self.dma('sp', wd[s], dr['wd_b'][i, dc].rearrange("p (c f) -> p c f", c=NFC), rd=[self.rwd[(i, dc)]], wr=[wdres[s]])
                yb = 5 + dc % 2
                for fc in range(NFC):
                    self.mm(B_[yb][:, :NT], wd[s][:, fc, :], aT[:, fc, :], start=(fc == 0), stop=(fc == NFC - 1),
                            rd=[wdres[s], ares[fc]], wr=[self.bres[yb]])
                self.stt('dve', self.xT[:, dc, ts_], B_[yb][:, :NT], 0.5, self.xT[:, dc, ts_], ALU.mult, ALU.add,
                         rd=[self.bres[yb]] + self.xr(dc, t0, NT), wr=self.xr(dc, t0, NT))
        ar.release(m)

    def final_phase(self):
        k, nc, dr, ar = self.k, self.nc, self.dr, self.ar
        k.barrier()
        m = ar.mark()
        self.rr = [0, 1]
        sq = [ar.bf16(NT) for _ in range(2)]
        sqres = [Res(), Res()]
        rstd = ar.f32(NT)
        rres = Res()
        o = [ar.f32(8 * NT).rearrange("p (c t) -> p c t", c=8) for _ in range(2)]
        ores = [[Res() for _ in range(8)] for _ in range(2)]
        evs = []
        gcol = VLN * self.nlayers
        for tb in range(NTB):
            s = tb % 2
            t0 = tb * NT
            self.rmsnorm_to(t0, NT, gcol, o[s], ores[s], sq, sqres, rstd, rres)
            for c in range(8):
                evs.append(self.dma('sp', dr['outT'][c][:, t0:t0 + NT], o[s][:, c, :], rd=[ores[s][c]]))
        k.wait_all('sp', evs + self.dbg_evs)
        ar.release(m)

    def mix_phase(self, l):
        k, dr, ar = self.k, self.dr, self.ar
        k.barrier()
        m0 = ar.mark()
        self.rr = list(range(8))
        P = self.P = {}
        R = self.R = {}

        def alloc(name, kind, n):
            P[name] = ar.bf16(n) if kind == 'b' else ar.f32(n)
            R[name] = Res(name)
        alloc('pa', 'f', 11 * (NM + 1))
        alloc('Sf', 'f', 384)
        alloc('Sb', 'b', 384)
        alloc('rSf', 'f', 256)
        alloc('rSb', 'b', 256)
        alloc('cT', 'b', T)
        alloc('ctok', 'b', T)
        alloc('ik2', 'b', T)
        alloc('smw', 'b', 4 * 384)
        alloc('omka', 'f', 4)
        for nm in ('pa', 'Sf', 'Sb', 'rSf', 'rSb'):
            self.k.op('pool', lambda e, a=P[nm]: e.memset(a, 0.0), (), [R[nm]])
        self.dma('pool', P['smw'], dr['smw'][l], wr=[R['smw']])
        P['low'] = P['smw'][:, 0:384]
        P['v2'] = P['smw'][:, 384:768]
        P['wuk'] = P['smw'][:, 768:1152]
        P['wuv'] = P['smw'][:, 1152:1536]
        c = VLN * l + VL['ka']
        self.ts('dve', P['omka'][:, 0:3], self.vec[:, c:c + 3], -1.0, 1.0, ALU.mult, ALU.add, rd=[self.rvec], wr=[R['omka']])
        self.wring = [ar.bf16(1024) for _ in range(4)]
        self.wrres = [Res() for _ in range(4)]
        self.wri = 0
        for mb in range(NMB):
            self.mix_block(l, mb)
            if self.dbg.get('max_mb') is not None and mb >= self.dbg['max_mb']:
                break
        k.barrier()
        ar.release(m0)

    def sbar(self):
        if self.dbg.get('hardbar'):
            self.k.barrier()
        else:
            soft_barrier(self.k)

    def proj(self, l, name, M, hT, hres):
        ci = CIDX[name]
        s = self.wri % 4
        self.wri += 1
        w = self.wring[s]
        self.dma('sp', w, self.dr['win_b'][l, ci], rd=[self.rwin[(l, ci)]], wr=[self.wrres[s]])
        b = self.nb()
        for kc in range(8):
            self.mm(self.banks[b][:M, :NM], w[:, kc * 128:kc * 128 + M], hT[:, kc, :], start=(kc == 0), stop=(kc == 7),
                    rd=[self.wrres[s], hres[kc]], wr=[self.bres[b]])
        return b

    def mix_block(self, l, mb):
        k, dr, ar = self.k, self.dr, self.ar
        t0 = mb * NM
        m = ar.mark()
        hT = ar.bf16(8 * NM).rearrange("p (c t) -> p c t", c=8)
        hres = [Res() for _ in range(8)]
        sq = [ar.bf16(NM) for _ in range(2)]
        sqres = [Res(), Res()]
        rstd = ar.f32(NM)
        rres = Res()
        oT = ar.bf16(8 * NM)
        ores = [Res() for _ in range(8)]
        self.rr = list(range(8))
        self.k.op('pool', lambda e: e.memset(oT, 0.0), (), ores)
        self.rmsnorm_to(t0, NM, VLN * l + VL['mix'], hT, hres, sq, sqres, rstd, rres)
        self.flush_casts((len(getattr(self, 'pending', [])) + (NMB - mb) - 1) // (NMB - mb))
        if 'a' in self.mixers:
            m1 = ar.mark()
            self.rwkv_block(l, mb, hT, hres, oT, ores)
            self.sbar()
            ar.release(m1)
        if 'c' in self.mixers:
            m1 = ar.mark()
            self.ret_block(l, mb, hT, hres, oT, ores)
            self.sbar()
            ar.release(m1)
        if 'b' in self.mixers:
            m1 = ar.mark()
            self.dsa_block(l, mb, hT, hres, oT, ores)
            self.sbar()
            ar.release(m1)
        if l == 0:
            self.dump('oT%d' % mb, oT, ores)
        self.rr = list(range(8))
        wo = [ar.bf16(1024) for _ in range(2)]
        wores = [Res(), Res()]
        for dc in range(8):
            s = dc % 2
            self.dma('sp', wo[s], dr['wout_b'][l, dc], rd=[self.rwout[(l, dc)]], wr=[wores[s]])
            b = self.nb()
            for mc in range(8):
                self.mm(self.banks[b][:, :NM], wo[s][:, mc * 128:(mc + 1) * 128], oT[:, mc * NM:(mc + 1) * NM],
                        start=(mc == 0), stop=(mc == 7), rd=[wores[s], ores[mc]], wr=[self.bres[b]])
            self.tt('dve', self.xT[:, dc, t0:t0 + NM], self.banks[b][:, :NM], self.xT[:, dc, t0:t0 + NM], ALU.add,
                    rd=[self.bres[b], self.xres[dc][mb]], wr=[self.xres[dc][mb]])
        self.sbar()
        ar.release(m)

    def rwkv_block(self, l, mb, hT, hres, oT, ores):
        ar, P, R, dr = self.ar, self.P, self.R, self.dr
        t0 = mb * NM
        N = NM
        B_ = self.banks
        bres = self.bres
        cf = self.cf
        rc = self.rconst
        pa = P['pa'].rearrange("p (c t) -> p c t", c=11)
        rpa = R['pa']
        nA = 11 if l > 0 else 10
        for j in range(nA):
            name = 'a%d' % j if j < 10 else 'mv'
            M = 128 if j < 10 else 16
            b = self.proj(l, name, M, hT, hres)
            self.cp('act', pa[:M, j, 1:N + 1], B_[b][:M, :N], rd=[bres[b]], wr=[rpa])
        xx = ar.f32(10 * N)
        rxx = [Res() for _ in range(10)]
        dtmp = [ar.f32(N) for _ in range(2)]
        rdt = [Res(), Res()]
        for j in range(10):
            s = j % 2
            self.tt('pool', dtmp[s], pa[:, j, 0:N], pa[:, j, 1:N + 1], ALU.subtract, rd=[rpa], wr=[rdt[s]])
            self.stt('dve', xx[:, j * N:(j + 1) * N], dtmp[s], self.vc(l, 'mu', j), pa[:, j, 1:N + 1], ALU.mult, ALU.add,
                     rd=[rdt[s], rpa, self.rvec], wr=[rxx[j]])
        if self.dbg.get('stop', 99) <= 1:
            return
        mv_b = ar.bf16(N)
        rmv = Res()
        vfb = None
        if l > 0:
            self.tt('pool', dtmp[0][:16], pa[:16, 10, 0:N], pa[:16, 10, 1:N + 1], ALU.subtract, rd=[rpa], wr=[rdt[0]])
            self.stt('dve', mv_b[:16], dtmp[0][:16], self.vc(l, 'mumv')[:16], pa[:16, 10, 1:N + 1], ALU.mult, ALU.add,
                     rd=[rdt[0], rpa, self.rvec], wr=[rmv])
            vfb = ar.f32(3 * N)
            rvfb = Res()
            self.dma('act', vfb.rearrange("p (c t) -> p c t", c=3), dr['vf'][:, :, t0:t0 + N], rd=[self.vfres[mb]], wr=[rvfb])
        for j in range(nA):
            M = 128 if j < 10 else 16
            self.cp('pool', pa[:M, j, 0:1], pa[:M, j, N:N + 1], rd=[], wr=[rpa])
        if self.dbg.get('stop', 99) <= 2:
            return
        lo = xx[:, 9 * N:10 * N]
        lo_b = ar.bf16(N)
        rlo = Res()
        self.act(lo_b[0:32], lo[0:32], AF.Tanh, rd=[rxx[9]], wr=[rlo])
        self.act(lo_b[32:64], lo[32:64], AF.Copy, rd=[rxx[9]], wr=[rlo])
        self.act(lo_b[64:128], lo[64:128], AF.Sigmoid, rd=[rxx[9]], wr=[rlo])
        low = P['low']
        rsw = R['smw']
        at_b = ar.bf16(3 * N)
        bt_b = ar.bf16(3 * N)
        kt_b = ar.bf16(3 * N)
        rt_b = ar.bf16(3 * N)
        gT = ar.bf16(3 * N)
        bonus = ar.f32(3 * N)
        rat, rbt, rkt, rrt, rg, rbo = [Res() for _ in range(6)]
        tok = [ar.bf16(3 * 384) for _ in range(NQ)]
        rtok = [Res() for _ in range(NQ)]
        Ptot = ar.f32(3 * NQ)
        rPt = Res()
        names = ['sgw', 'cs', 'csx', 'Pinc', 'Pexc', 'Pinv', 'Pend', 'a', 'nrm', 'kkn', 't1', 'kp', 'bb', 'sgv']
        tf = {n: ar.f32(N) for n in names}
        rf = {n: Res() for n in names}
        tb16 = {n: ar.bf16(N) for n in ('sqk', 'rk', 'bhT', 'khT', 'vb')}
        rb16 = {n: Res() for n in tb16}
        nbv = ar.f32(4)
        rnb = Res()
        for j in range(3):
            cs_ = slice(j * 128, (j + 1) * 128)
            fs = slice(j * N, (j + 1) * N)
            r_j = xx[:, (0 + j) * N:(1 + j) * N]
            k_j = xx[:, (3 + j) * N:(4 + j) * N]
            v_j = xx[:, (6 + j) * N:(7 + j) * N]
            rr_, rk_, rv_ = rxx[j], rxx[3 + j], rxx[6 + j]
            b = self.nb()
            self.mm(B_[b][:, :N], low[0:32, cs_], lo_b[0:32], rd=[rsw, rlo], wr=[bres[b]])
            self.act(tf['sgw'], B_[b][:, :N], AF.Sigmoid, rd=[bres[b], self.rvec], wr=[rf['sgw']], bias=self.vc(l, 'w0', j))
            self.k.op('dve', lambda e, o=tf['cs'], d0=cf[:, CF['reset']:CF['reset'] + N], d1=tf['sgw']: e.tensor_tensor_scan(
                out=o, data0=d0, data1=d1, initial=0.0, op0=ALU.mult, op1=ALU.add), [rf['sgw'], rc], [rf['cs']])
            self.tt('pool', tf['csx'], tf['cs'], tf['sgw'], ALU.subtract, rd=[rf['cs'], rf['sgw']], wr=[rf['csx']])
            self.act(tf['Pinc'], tf['cs'], AF.Exp, rd=[rf['cs']], wr=[rf['Pinc']], scale=-C0)
            self.act(tf['Pexc'], tf['csx'], AF.Exp, rd=[rf['csx']], wr=[rf['Pexc']], scale=-C0)
            self.act(tf['Pinv'], tf['cs'], AF.Exp, rd=[rf['cs']], wr=[rf['Pinv']], scale=C0)
            for q in range(NQ):
                self.ts('dve', nbv[:, q:q + 1], tf['cs'][:, q * 128 + 127:q * 128 + 128], -C0, None, ALU.mult, rd=[rf['cs']], wr=[rnb])
            for q in range(NQ):
                tq = slice(q * 128, (q + 1) * 128)
                self.act(tf['Pend'][:, tq], tf['cs'][:, tq], AF.Exp, rd=[rf['cs'], rnb], wr=[rf['Pend']], scale=C0, bias=nbv[:, q:q + 1])
            self.act(Ptot[:, j * NQ:(j + 1) * NQ], nbv[:, 0:NQ], AF.Exp, rd=[rnb], wr=[rPt])
            if self.dbg.get('stop', 99) <= 3:
                continue
            b = self.nb()
            self.mm(B_[b][:, :N], low[32:64, cs_], lo_b[32:64], rd=[rsw, rlo], wr=[bres[b]])
            self.act(tf['a'], B_[b][:, :N], AF.Sigmoid, rd=[bres[b], self.rvec], wr=[rf['a']], bias=self.vc(l, 'a0', j))
            b = self.nb()
            self.mm(B_[b][:, :N], low[64:128, cs_], lo_b[64:128], rd=[rsw, rlo], wr=[bres[b]])
            self.cp('act', gT[:, fs], B_[b][:, :N], rd=[bres[b]], wr=[rg])
            self.act(tb16['sqk'], k_j, AF.Square, rd=[rk_, self.rvec], wr=[rb16['sqk']], scale=self.vc(l, 'kk', j))
            b = self.nb()
            self.mm(B_[b][:, :N], self.bones_b, tb16['sqk'], rd=[rc, rb16['sqk']], wr=[bres[b]])
            self.act(tf['nrm'], B_[b][:, :N], AF.Sqrt, rd=[bres[b]], wr=[rf['nrm']])
            self.ts('dve', tf['nrm'], tf['nrm'], 1e-12, None, ALU.max, rd=[rf['nrm']], wr=[rf['nrm']])
            self.recip(tf['nrm'], tf['nrm'], rd=[rf['nrm']], wr=[rf['nrm']])
            self.stt('dve', tf['kkn'], k_j, self.vc(l, 'kk', j), tf['nrm'], ALU.mult, ALU.mult, rd=[rk_, self.rvec, rf['nrm']], wr=[rf['kkn']])
            self.ts('dve', tf['t1'], tf['a'], self.vc(l, 'ka', j), P['omka'][:, j:j + 1], ALU.mult, ALU.add,
                    rd=[rf['a'], self.rvec, R['omka']], wr=[rf['t1']])
            self.tt('pool', tf['kp'], tf['t1'], k_j, ALU.mult, rd=[rf['t1'], rk_], wr=[rf['kp']])
            if self.dbg.get('stop', 99) <= 4:
                continue
            self.stt('dve', at_b[:, fs], tf['kkn'], -1.0, tf['Pexc'], ALU.mult, ALU.mult, rd=[rf['kkn'], rf['Pexc']], wr=[rat])
            self.tt('pool', tf['bb'], tf['kkn'], tf['a'], ALU.mult, rd=[rf['kkn'], rf['a']], wr=[rf['bb']])
            self.tt('dve', bt_b[:, fs], tf['bb'], tf['Pinv'], ALU.mult, rd=[rf['bb'], rf['Pinv']], wr=[rbt])
            self.tt('pool', tb16['bhT'], tf['bb'], tf['Pend'], ALU.mult, rd=[rf['bb'], rf['Pend']], wr=[rb16['bhT']])
            self.tt('dve', kt_b[:, fs], tf['kp'], tf['Pinv'], ALU.mult, rd=[rf['kp'], rf['Pinv']], wr=[rkt])
            self.tt('pool', tb16['khT'], tf['kp'], tf['Pend'], ALU.mult, rd=[rf['kp'], rf['Pend']], wr=[rb16['khT']])
            self.tt('dve', rt_b[:, fs], r_j, tf['Pinc'], ALU.mult, rd=[rr_, rf['Pinc']], wr=[rrt])
            if l == 0:
                self.dma('act', dr['vf'][:, j, t0:t0 + N], v_j, rd=[rv_], wr=[self.vfres[mb]])
            else:
                b = self.nb()
                self.mm(B_[b][:, :N], P['v2'][0:16, cs_], mv_b[0:16], rd=[rsw, rmv], wr=[bres[b]])
                self.act(tf['sgv'], B_[b][:, :N], AF.Sigmoid, rd=[bres[b], self.rvec], wr=[rf['sgv']], bias=self.vc(l, 'v0', j))
                self.tt('pool', tf['t1'], vfb[:, fs], v_j, ALU.subtract, rd=[rvfb, rv_, rf['t1']], wr=[rf['t1']])
                self.tt('dve', tf['t1'], tf['t1'], tf['sgv'], ALU.mult, rd=[rf['t1'], rf['sgv']], wr=[rf['t1']])
                self.tt('pool', v_j, v_j, tf['t1'], ALU.add, rd=[rv_, rf['t1']], wr=[rv_])
            self.cp('pool', tb16['vb'], v_j, rd=[rv_], wr=[rb16['vb']])
            self.stt('dve', tb16['rk'], r_j, self.vc(l, 'rk', j), tf['kp'], ALU.mult, ALU.mult, rd=[rr_, self.rvec, rf['kp']], wr=[rb16['rk']])
            b = self.nb()
            self.mm(B_[b][:, :N], self.bones_b, tb16['rk'], rd=[rc, rb16['rk']], wr=[bres[b]])
            self.tt('dve', bonus[:, fs], B_[b][:, :N], v_j, ALU.mult, rd=[bres[b], rv_], wr=[rbo])
            if self.dbg.get('stop', 99) <= 5:
                continue
            for q in range(NQ):
                tq = slice(q * 128, (q + 1) * 128)
                b = self.nb()
                for x, nm in enumerate(('bhT', 'khT', 'vb')):
                    self.mm(B_[b][:, x * 128:(x + 1) * 128], tb16[nm][:, tq], self.ident_b, rd=[rb16[nm], rc], wr=[bres[b]])
                self.cp('act', tok[q].rearrange("p (x f) -> p x f", x=3)[:, :, cs_],
                        B_[b][:, 0:384].rearrange("p (x f) -> p x f", x=3), rd=[bres[b]], wr=[rtok[q]])
        if self.dbg.get('stop', 99) <= 6:
            return
        y_sb = ar.f32(3 * N)
        ry = Res()
        kinds = [('N', at_b, rat, bt_b, rbt, 'maskL'), ('Nt', bt_b, rbt, at_b, rat, 'maskU'), ('Aak', kt_b, rkt, at_b, rat, 'maskU'),
                 ('Arb', bt_b, rbt, rt_b, rrt, 'maskUi'), ('Ark', kt_b, rkt, rt_b, rrt, 'maskUi')]
        Am = {kd[0]: ar.bf16(768) for kd in kinds}
        rAm = {kd[0]: Res() for kd in kinds}
        Mx = [ar.bf16(768) for _ in range(2)]
        Mtx = [ar.bf16(768) for _ in range(2)]
        Qx = [ar.bf16(768) for _ in range(2)]
        rMx = [Res(), Res()]
        rMtx = [Res(), Res()]
        rQx = [Res(), Res()]
        W_sb = ar.bf16(384)
        U_sb = ar.bf16(384)
        rW, rU = Res(), Res()
        Sf, Sb = P['Sf'], P['Sb']
        rSf, rSb = R['Sf'], R['Sb']
        ident6 = self.cb[:, CB['ident']:CB['ident'] + 768]
        for q in range(NQ):
            tq = slice(q * 128, (q + 1) * 128)
            tokq = tok[q]
            for (nm, Lt, rL, Rt, rR, mk) in kinds:
                for e in range(2):
                    pr = slice(e * 64, (e + 1) * 64)
                    b = self.nb()
                    for j in range(3):
                        cols = slice(j * N + q * 128, j * N + (q + 1) * 128)
                        self.mm(B_[b][:, j * 128:(j + 1) * 128], Lt[pr, cols], Rt[pr, cols], rd=[rL, rR], wr=[bres[b]])
                    self.tt('dve', Am[nm].rearrange("p (j e t) -> p j e t", j=3, e=2)[:, :, e, :],
                            B_[b][:, 0:384].rearrange("p (j t) -> p j t", j=3),
                            cf[:, CF[mk]:CF[mk] + 384].rearrange("p (j t) -> p j t", j=3), ALU.mult,
                            rd=[bres[b], rc], wr=[rAm[nm]])
            if self.dbg.get('stop', 99) <= 7:
                continue
            Mc, Mtc, rMc, rMtc = Am['N'], Am['Nt'], rAm['N'], rAm['Nt']
            qi = 0
            self.tt('pool', Qx[qi], Am['Nt'], ident6, ALU.add, rd=[rAm['Nt'], rc], wr=[rQx[qi]])
            for lev in range(1, 7):
                mi = lev % 2
                for half in range(2):
                    hs = slice(half * 384, (half + 1) * 384)
                    b = self.nb()
                    for hh in range(3):
                        c_ = slice((half * 3 + hh) * 128, (half * 3 + hh + 1) * 128)
                        self.mm(B_[b][:, hh * 128:(hh + 1) * 128], Mtc[:, c_], Mc[:, c_], rd=[rMc, rMtc], wr=[bres[b]])
                    self.cp('act', Mx[mi][:, hs], B_[b][:, 0:384], rd=[bres[b]], wr=[rMx[mi]])
                    if lev < 6:
                        b = self.nb()
                        for hh in range(3):
                            c_ = slice((half * 3 + hh) * 128, (half * 3 + hh + 1) * 128)
                            self.mm(B_[b][:, hh * 128:(hh + 1) * 128], Mc[:, c_], Mtc[:, c_], rd=[rMc, rMtc], wr=[bres[b]])
                        self.cp('act', Mtx[mi][:, hs], B_[b][:, 0:384], rd=[bres[b]], wr=[rMtx[mi]])
                Mc, rMc = Mx[mi], rMx[mi]
                if lev < 6:
                    Mtc, rMtc = Mtx[mi], rMtx[mi]
                qn = 1 - qi
                for half in range(2):
                    hs = slice(half * 384, (half + 1) * 384)
                    b = self.nb()
                    for hh in range(3):
                        c_ = slice((half * 3 + hh) * 128, (half * 3 + hh + 1) * 128)
                        o_ = B_[b][:, hh * 128:(hh + 1) * 128]
                        self.mm(o_, self.ident_b, Qx[qi][:, c_], start=True, stop=False, rd=[rc, rQx[qi]], wr=[bres[b]])
                        self.mm(o_, Mc[:, c_], Qx[qi][:, c_], start=False, stop=True, rd=[rMc, rQx[qi]], wr=[bres[b]])
                    self.cp('dve', Qx[qn][:, hs], B_[b][:, 0:384], rd=[bres[b]], wr=[rQx[qn]])
                qi = qn
            Tt, rTt = Qx[qi], rQx[qi]
            if self.dbg.get('stop', 99) <= 8:
                continue
            b = self.nb()
            for j in range(3):
                cols = slice(j * N + q * 128, j * N + (q + 1) * 128)
                self.mm(B_[b][:, j * 128:(j + 1) * 128], at_b[:, cols], Sb[:, j * 128:(j + 1) * 128], start=True, stop=False,
                        rd=[rat, rSb], wr=[bres[b]])
                for e in range(2):
                    h = 2 * j + e
                    self.mm(B_[b][:, h * 64:(h + 1) * 64], Am['Aak'][:, h * 128:(h + 1) * 128], tokq[:, 768 + h * 64:768 + (h + 1) * 64],
                            start=False, stop=(e == 1), rd=[rAm['Aak'], rtok[q]], wr=[bres[b]])
            self.cp('act', W_sb, B_[b][:, 0:384], rd=[bres[b]], wr=[rW])
            b = self.nb()
            for h in range(6):
                self.mm(B_[b][:, h * 64:(h + 1) * 64], Tt[:, h * 128:(h + 1) * 128], W_sb[:, h * 64:(h + 1) * 64], rd=[rTt, rW], wr=[bres[b]])
            self.cp('dve', U_sb, B_[b][:, 0:384], rd=[bres[b]], wr=[rU])
            if self.dbg.get('stop', 99) <= 9:
                continue
            b = self.nb()
            for j in range(3):
                cols = slice(j * N + q * 128, j * N + (q + 1) * 128)
                self.mm(B_[b][:, j * 128:(j + 1) * 128], Sb[:, j * 128:(j + 1) * 128], rt_b[:, cols], start=True, stop=False,
                        rd=[rSb, rrt], wr=[bres[b]])
                for e in range(2):
                    h = 2 * j + e
                    pr = slice(e * 64, (e + 1) * 64)
                    o_ = B_[b][pr, j * 128:(j + 1) * 128]
                    self.mm(o_, U_sb[:, h * 64:(h + 1) * 64], Am['Arb'][:, h * 128:(h + 1) * 128], start=False, stop=False,
                            rd=[rU, rAm['Arb']], wr=[bres[b]])
                    self.mm(o_, tokq[:, 768 + h * 64:768 + (h + 1) * 64], Am['Ark'][:, h * 128:(h + 1) * 128], start=False, stop=True,
                            rd=[rtok[q], rAm['Ark']], wr=[bres[b]])
            self.cp('act', y_sb.rearrange("p (j t) -> p j t", j=3)[:, :, tq], B_[b][:, 0:384].rearrange("p (j t) -> p j t", j=3),
                    rd=[bres[b]], wr=[ry])
            if self.dbg.get('stop', 99) <= 10:
                continue
            b = self.nb()
            for h in range(6):
                j, e = h // 2, h % 2
                pr = slice(e * 64, (e + 1) * 64)
                o_ = B_[b][pr, j * 64:(j + 1) * 64]
                self.mm(o_, tokq[:, 0 + h * 64:0 + (h + 1) * 64], U_sb[:, h * 64:(h + 1) * 64], start=True, stop=False,
                        rd=[rtok[q], rU], wr=[bres[b]])
                self.mm(o_, tokq[:, 384 + h * 64:384 + (h + 1) * 64], tokq[:, 768 + h * 64:768 + (h + 1) * 64], start=False, stop=True,
                        rd=[rtok[q]], wr=[bres[b]])
            for j in range(3):
                for e in range(2):
                    pr = slice(e * 64, (e + 1) * 64)
                    sc = slice(j * 128 + e * 64, j * 128 + (e + 1) * 64)
                    self.stt('dve', Sf[pr, sc], Sf[pr, sc], Ptot[pr, j * NQ + q:j * NQ + q + 1],
                             B_[b][pr, j * 64:(j + 1) * 64], ALU.mult, ALU.add, rd=[rSf, rPt, bres[b]], wr=[rSf])
            self.cp('dve', Sb, Sf, rd=[rSf], wr=[rSb])
        if self.dbg.get('stop', 99) <= 11:
            return
        yb = ar.bf16(N)
        ryb = Res()
        yc = ar.f32(N)
        ryc = Res()
        sd = ar.f32(N)
        rsd = Res()
        for j in range(3):
            fs = slice(j * N, (j + 1) * N)
            yj = y_sb[:, fs]
            self.cp('act', yb, yj, rd=[ry], wr=[ryb])
            b = self.nb()
            self.mm(B_[b][:, :N], self.bmean_b, yb, rd=[rc, ryb], wr=[bres[b]])
            self.tt('dve', yc, yj, B_[b][:, :N], ALU.subtract, rd=[ry, bres[b]], wr=[ryc])
            self.act(yb, yc, AF.Square, rd=[ryc], wr=[ryb])
            b = self.nb()
            self.mm(B_[b][:, :N], self.bmean_b, yb, rd=[rc, ryb], wr=[bres[b]])
            self.act(sd, B_[b][:, :N], AF.Sqrt, rd=[bres[b]], wr=[rsd], bias=GN_EPS)
            self.recip(sd, sd, rd=[rsd], wr=[rsd])
            self.tt('dve', yc, yc, sd, ALU.mult, rd=[ryc, rsd], wr=[ryc])
            self.ts('dve', yc, yc, self.vc(l, 'lnw', j), self.vc(l, 'lnb', j), ALU.mult, ALU.add, rd=[ryc, self.rvec], wr=[ryc])
            self.tt('pool', yc, yc, bonus[:, fs], ALU.add, rd=[ryc, rbo], wr=[ryc])
            self.tt('dve', oT[:, j * N:(j + 1) * N], yc, gT[:, fs], ALU.mult, rd=[ryc, rg], wr=[ores[j]])

    def ret_block(self, l, mb, hT, hres, oT, ores):
        ar, P, R, dr = self.ar, self.P, self.R, self.dr
        t0 = mb * NM
        N = NM
        B_ = self.banks
        bres = self.bres
        cf = self.cf
        rc = self.rconst
        z = {}
        rz = {}
        for nm in ('cq', 'ck', 'cv', 'cg'):
            z[nm] = ar.f32(2 * N)
            rz[nm] = Res()
            for j in range(2):
                b = self.proj(l, nm + str(j), 128, hT, hres)
                self.cp('act', z[nm][:, j * N:(j + 1) * N], B_[b][:, :N], rd=[bres[b]], wr=[rz[nm]])
        rot = ar.f32(4 * N)
        rrot = Res()
        self.dma('act', rot.rearrange("p (c t) -> p c t", c=4), dr['rot'][:, :, t0:t0 + N], wr=[rrot])
        qr_b = ar.bf16(2 * N)
        qd_b = ar.bf16(2 * N)
        kr_b = ar.bf16(2 * N)
        kdT = ar.bf16(2 * N)
        cvb = ar.bf16(2 * N)
        rqr, rqd, rkr, rkd, rcvb = [Res() for _ in range(5)]
        t1 = ar.f32(N)
        t2 = ar.f32(N)
        zr = ar.f32(N)
        rt1, rt2, rzr = Res(), Res(), Res()
        prot = cf[:, CF['prot']:CF['prot'] + 128]
        for (nm, ci, si) in (('cq', 0, 1), ('ck', 2, 3)):
            for j in range(2):
                fs = slice(j * N, (j + 1) * N)
                zj = z[nm][:, fs]
                b = self.nb()
                self.mm(B_[b][:, :N], prot, zj, rd=[rc, rz[nm]], wr=[bres[b]])
                self.tt('pool', t1, zj, rot[:, ci * N:(ci + 1) * N], ALU.mult, rd=[rz[nm], rrot], wr=[rt1])
                self.tt('dve', t2, B_[b][:, :N], rot[:, si * N:(si + 1) * N], ALU.mult, rd=[bres[b], rrot], wr=[rt2])
                self.tt('dve', zr, t1, t2, ALU.add, rd=[rt1, rt2], wr=[rzr])
                if nm == 'cq':
                    self.cp('act', qr_b[:, fs], zr, rd=[rzr], wr=[rqr])
                    self.tt('pool', qd_b[:, fs], zr, cf[:, CF['qdec'] + j * N:CF['qdec'] + (j + 1) * N], ALU.mult, rd=[rzr, rc], wr=[rqd])
                else:
                    self.cp('act', kr_b[:, fs], zr, rd=[rzr], wr=[rkr])
                    self.tt('pool', kdT[:, fs], zr, cf[:, CF['kdec'] + j * N:CF['kdec'] + (j + 1) * N], ALU.mult, rd=[rzr, rc], wr=[rkd])
        self.cp('pool', cvb, z['cv'], rd=[rz['cv']], wr=[rcvb])
        tokr = [ar.bf16(512) for _ in range(NQ)]
        rtokr = [Res() for _ in range(NQ)]
        for q in range(NQ):
            b = self.nb()
            for x, (src, rs) in enumerate(((kdT, rkd), (cvb, rcvb))):
                for j in range(2):
                    self.mm(B_[b][:, (x * 2 + j) * 128:(x * 2 + j + 1) * 128], src[:, j * N + q * 128:j * N + (q + 1) * 128], self.ident_b,
                            rd=[rs, rc], wr=[bres[b]])
            self.cp('act', tokr[q], B_[b][:, 0:512], rd=[bres[b]], wr=[rtokr[q]])
        yret = ar.f32(2 * N)
        ryr = Res()
        sm = ar.bf16(512)
        rsm = Res()
        Sf, Sb = P['rSf'], P['rSb']
        rSf, rSb = R['rSf'], R['rSb']
        for q in range(NQ):
            tq = slice(q * 128, (q + 1) * 128)
            for e in range(2):
                pr = slice(e * 64, (e + 1) * 64)
                b = self.nb()
                for j in range(2):
                    cols = slice(j * N + q * 128, j * N + (q + 1) * 128)
                    self.mm(B_[b][:, j * 128:(j + 1) * 128], kr_b[pr, cols], qr_b[pr, cols], rd=[rkr, rqr], wr=[bres[b]])
                self.tt('dve', sm.rearrange("p (j e t) -> p j e t", j=2, e=2)[:, :, e, :],
                        B_[b][:, 0:256].rearrange("p (j t) -> p j t", j=2),
                        cf[:, CF['intraT']:CF['intraT'] + 512].rearrange("p (j e t) -> p j e t", j=2, e=2)[:, :, e, :], ALU.mult,
                        rd=[bres[b], rc], wr=[rsm])
            b = self.nb()
            for j in range(2):
                cols = slice(j * N + q * 128, j * N + (q + 1) * 128)
                self.mm(B_[b][:, j * 128:(j + 1) * 128], Sb[:, j * 128:(j + 1) * 128], qd_b[:, cols], start=True, stop=False,
                        rd=[rSb, rqd], wr=[bres[b]])
                for e in range(2):
                    h = 2 * j + e
                    pr = slice(e * 64, (e + 1) * 64)
                    self.mm(B_[b][pr, j * 128:(j + 1) * 128], tokr[q][:, 256 + h * 64:256 + (h + 1) * 64], sm[:, h * 128:(h + 1) * 128],
                            start=False, stop=True, rd=[rtokr[q], rsm], wr=[bres[b]])
            self.cp('act', yret.rearrange("p (j t) -> p j t", j=2)[:, :, tq], B_[b][:, 0:256].rearrange("p (j t) -> p j t", j=2),
                    rd=[bres[b]], wr=[ryr])
            b = self.nb()
            for h in range(4):
                j, e = h // 2, h % 2
                pr = slice(e * 64, (e + 1) * 64)
                self.mm(B_[b][pr, j * 64:(j + 1) * 64], tokr[q][:, h * 64:(h + 1) * 64], tokr[q][:, 256 + h * 64:256 + (h + 1) * 64],
                        rd=[rtokr[q]], wr=[bres[b]])
            for j in range(2):
                for e in range(2):
                    pr = slice(e * 64, (e + 1) * 64)
                    sc = slice(j * 128 + e * 64, j * 128 + (e + 1) * 64)
                    self.stt('dve', Sf[pr, sc], Sf[pr, sc], cf[pr, CF['cdec'] + j:CF['cdec'] + j + 1],
                             B_[b][pr, j * 64:(j + 1) * 64], ALU.mult, ALU.add, rd=[rSf, rc, bres[b]], wr=[rSf])
            self.cp('dve', Sb, Sf, rd=[rSf], wr=[rSb])
        sqb = ar.bf16(N)
        rsq = Res()
        sd = ar.f32(N)
        rsd = Res()
        sg = ar.f32(N)
        rsg = Res()
        for j in range(2):
            fs = slice(j * N, (j + 1) * N)
            self.act(sqb, yret[:, fs], AF.Square, rd=[ryr], wr=[rsq])
            b = self.nb()
            self.mm(B_[b][:, :N], self.bmean_b, sqb, rd=[rc, rsq], wr=[bres[b]])
            self.act(sd, B_[b][:, :N], AF.Sqrt, rd=[bres[b]], wr=[rsd], bias=EPS)
            self.recip(sd, sd, rd=[rsd], wr=[rsd])
            self.tt('dve', sd, sd, yret[:, fs], ALU.mult, rd=[rsd, ryr], wr=[rsd])
            self.act(sg, z['cg'][:, fs], AF.Silu, rd=[rz['cg']], wr=[rsg])
            self.tt('pool', oT[:, (6 + j) * N:(7 + j) * N], sd, sg, ALU.mult, rd=[rsd, rsg], wr=[ores[6 + j]])

    def dsa_block(self, l, mb, hT, hres, oT, ores):
        ar, P, R, dr = self.ar, self.P, self.R, self.dr
        t0 = mb * NM
        N = NM
        B_ = self.banks
        bres = self.bres
        cf = self.cf
        rc = self.rconst
        cT, ctok, ik2 = P['cT'], P['ctok'], P['ik2']
        rcT, rctok, rik2 = R['cT'], R['ctok'], R['ik2']
        self.rr = [0, 1, 2, 3]
        qT_b = ar.bf16(3 * N)
        rq = Res()
        for j in range(3):
            b = self.proj(l, 'bq%d' % j, 128, hT, hres)
            self.cp('act', qT_b[:, j * N:(j + 1) * N], B_[b][:, :N], rd=[bres[b]], wr=[rq])
        ckv = ar.f32(N)
        rckv = Res()
        b = self.proj(l, 'bc', 128, hT, hres)
        self.cp('act', ckv, B_[b][:, :N], rd=[bres[b]], wr=[rckv])
        sqb = ar.bf16(N)
        rsq = Res()
        sd = ar.f32(N)
        rsd = Res()
        self.act(sqb, ckv, AF.Square, rd=[rckv], wr=[rsq])
        b = self.nb()
        self.mm(B_[b][:, :N], self.ones_b, sqb, rd=[rc, rsq], wr=[bres[b]])
        self.act(sd, B_[b][:, :N], AF.Sqrt, rd=[bres[b]], wr=[rsd], bias=EPS, scale=1.0 / 128)
        self.recip(sd, sd, rd=[rsd], wr=[rsd])
        self.stt('dve', cT[:, t0:t0 + N], ckv, self.vc(l, 'kvn'), sd, ALU.mult, ALU.mult, rd=[rckv, self.rvec, rsd], wr=[rcT])
        for q in range(NQ):
            gq = t0 // 128 + q
            b = self.nb()
            self.mm(B_[b][:, :128], cT[:, gq * 128:(gq + 1) * 128], self.ident_b, rd=[rcT, rc], wr=[bres[b]])
            self.cp('act', ctok[:, gq * 128:(gq + 1) * 128], B_[b][:, :128], rd=[bres[b]], wr=[rctok])
        iq_b = ar.bf16(4 * N)
        riq = Res()
        for j in range(4):
            b = self.proj(l, 'biq%d' % j, 128, hT, hres)
            self.cp('act', iq_b[:, j * N:(j + 1) * N], B_[b][:, :N], rd=[bres[b]], wr=[riq])
        b = self.proj(l, 'bik2', 128, hT, hres)
        self.cp('act', ik2[:, t0:t0 + N], B_[b][:, :N], rd=[bres[b]], wr=[rik2])
        iw_f = ar.f32(N)
        riw = Res()
        b = self.proj(l, 'biw', 8, hT, hres)
        self.cp('act', iw_f[:8], B_[b][:8, :N], rd=[bres[b]], wr=[riw])
        iwbc = ar.f32(8 * N)
        riwbc = Res()
        SC = (8.0 ** -0.5) * (64.0 ** -0.5)
        for h8 in range(8):
            b = self.nb()
            self.mm(B_[b][:, :N], cf[0:8, CF['sel'] + h8 * 128:CF['sel'] + (h8 + 1) * 128], iw_f[:8], rd=[rc, riw], wr=[bres[b]])
            self.act(iwbc[:, h8 * N:(h8 + 1) * N], B_[b][:, :N], AF.Copy, rd=[bres[b]], wr=[riwbc], scale=SC)
        qlT = ar.bf16(NQ * 768)
        rql = Res()
        for h in range(6):
            j, e = h // 2, h % 2
            pr = slice(e * 64, (e + 1) * 64)
            b = self.nb()
            self.mm(B_[b][:, :N], P['wuk'][pr, j * 128:(j + 1) * 128], qT_b[pr, j * N:(j + 1) * N], rd=[R['smw'], rq], wr=[bres[b]])
            for q in range(NQ):
                self.act(qlT[:, (q * 6 + h) * 128:(q * 6 + h + 1) * 128], B_[b][:, q * 128:(q + 1) * 128], AF.Copy,
                         rd=[bres[b]], wr=[rql], scale=0.125)
        score = [ar.f32(T) for _ in range(NQ)]
        rsc = [Res() for _ in range(NQ)]
        junk = ar.bf16(T)
        rjk = Res()
        bsq = [ar.f32(8) for _ in range(NQ)]
        rbsq = [Res() for _ in range(NQ)]
        negmq = [ar.bf16(T) for _ in range(NQ)]
        rngq = [Res() for _ in range(NQ)]
        tmp = [ar.bf16(1024) for _ in range(2)]
        rtmp = [Res(), Res()]
        ex = [ar.bf16(768) for _ in range(2)]
        rex = [Res(), Res()]
        rden = ar.f32(768)
        rrd = Res()
        oln = ar.bf16(768)
        roln = Res()
        ident4 = self.cb[:, CB['ident']:CB['ident'] + 512]
        iq3 = iq_b.rearrange("p (j t) -> p j t", j=4)

        def idx(q):
            gq = t0 // 128 + q
            nS = gq + 1
            tq = slice(q * 128, (q + 1) * 128)
            self.rr = [0, 1, 2, 3]
            sb = None
            for si in range(nS):
                zb = [self.nb(), self.nb()]
                for e in range(2):
                    pr = slice(e * 64, (e + 1) * 64)
                    self.mm(B_[zb[e]][:, 0:512], ik2[pr, si * 128:(si + 1) * 128], iq3[pr, :, tq], rd=[rik2, riq], wr=[bres[zb[e]]])
                ts_ = si % 2
                for half in range(2):
                    self.stt('dve', tmp[ts_].rearrange("p (j e t) -> p j e t", j=4, e=2)[:, :, half, :],
                             B_[zb[half]][:, 0:512].rearrange("p (h t) -> p h t", h=4), 0.0,
                             iwbc.rearrange("p (j e t) -> p j e t", j=4, e=2)[:, :, half, tq], ALU.max, ALU.mult,
                             rd=[bres[zb[half]], riwbc], wr=[rtmp[ts_]])
                if si % 4 == 0:
                    sb = 4 + (si // 4) % 2
                for h8 in range(8):
                    self.mm(B_[sb][:, (si % 4) * 128:(si % 4 + 1) * 128], tmp[ts_][:, h8 * 128:(h8 + 1) * 128], self.ident_b,
                            start=(h8 == 0), stop=(h8 == 7), rd=[rtmp[ts_], rc], wr=[bres[sb]])
                if si % 4 == 3 or si == nS - 1:
                    c0 = (si // 4) * 512
                    nc_ = (si % 4 + 1) * 128
                    self.cp('act', score[q][:, c0:c0 + nc_], B_[sb][:, 0:nc_], rd=[bres[sb]], wr=[rsc[q]])
            if gq >= 2 and not self.dbg.get('notopk'):
                ncols_ = nS * 128
                bs_, rbs_ = bsq[q], rbsq[q]
                self.k.op('dve', lambda e, o=bs_[:, 1:2], i=score[q][:, :ncols_]: e.tensor_reduce(out=o, in_=i, axis=AX.X, op=ALU.max),
                          [rsc[q]], [rbs_])
                self.k.op('dve', lambda e, o=bs_[:, 0:1], i=score[q][:, :ncols_]: e.tensor_reduce(out=o, in_=i, axis=AX.X, op=ALU.min),
                          [rsc[q]], [rbs_])
            self.tt('pool', score[q][:, gq * 128:(gq + 1) * 128], score[q][:, gq * 128:(gq + 1) * 128],
                    cf[:, CF['cmask']:CF['cmask'] + 128], ALU.add, rd=[rsc[q], rc], wr=[rsc[q]])

        def topk(q):
            gq = t0 // 128 + q
            ncols = (gq + 1) * 128
            sc_ = score[q][:, :ncols]
            bs, rbs = bsq[q], rbsq[q]
            negm, rng = negmq[q], rngq[q]
            if gq >= 2 and not self.dbg.get('notopk'):
                self.ts('dve', bs[:, 0:1], bs[:, 0:1], -1.0, None, ALU.add, rd=[rbs], wr=[rbs])
                self.ts('dve', bs[:, 1:2], bs[:, 1:2], 1.0, None, ALU.add, rd=[rbs], wr=[rbs])
                self.tt('dve', bs[:, 1:2], bs[:, 1:2], bs[:, 0:1], ALU.subtract, rd=[rbs], wr=[rbs])
                for it in range(NIT_TOPK):
                    c_ = 2.0 ** -(it + 1)
                    self.stt('dve', bs[:, 2:3], bs[:, 1:2], c_, bs[:, 0:1], ALU.mult, ALU.add, rd=[rbs], wr=[rbs])
                    self.k.op('dve', lambda e, o=junk[:, :ncols], i=sc_, m_=bs[:, 2:3], a_=bs[:, 3:4]: e.tensor_scalar(
                        out=o, in0=i, scalar1=m_, scalar2=0.0, op0=ALU.is_ge, op1=ALU.add, accum_out=a_), [rsc[q], rbs, rjk], [rjk, rbs])
                    self.ts('dve', bs[:, 4:5], bs[:, 3:4], 255.5, c_, ALU.is_ge, ALU.mult, rd=[rbs], wr=[rbs])
                    self.stt('dve', bs[:, 0:1], bs[:, 4:5], bs[:, 1:2], bs[:, 0:1], ALU.mult, ALU.add, rd=[rbs], wr=[rbs])
                self.ts('dve', negm[:, :ncols], sc_, bs[:, 0:1], -30000.0, ALU.is_lt, ALU.mult, rd=[rsc[q], rbs], wr=[rng])
            else:
                self.ts('dve', negm[:, :ncols], sc_, -1e29, -30000.0, ALU.is_lt, ALU.mult, rd=[rsc[q]], wr=[rng])

        def attm(q):
            gq = t0 // 128 + q
            nS = gq + 1
            negm, rng = negmq[q], rngq[q]
            self.rr = [0, 1, 2, 3]
            for si in range(nS):
                s_ = slice(si * 128, (si + 1) * 128)
                la, lb = self.nb(), self.nb()
                xs = si % 2
                self.mm(B_[la][:, 0:512], cT[:, s_], qlT[:, q * 768:q * 768 + 512], start=True, stop=False, rd=[rcT, rql], wr=[bres[la]])
                self.mm(B_[la][:, 0:512], negm[:, s_], ident4, start=False, stop=True, rd=[rng, rc], wr=[bres[la]])
                self.mm(B_[lb][:, 0:256], cT[:, s_], qlT[:, q * 768 + 512:q * 768 + 768], start=True, stop=False, rd=[rcT, rql], wr=[bres[lb]])
                self.mm(B_[lb][:, 0:256], negm[:, s_], ident4[:, 0:256], start=False, stop=True, rd=[rng, rc], wr=[bres[lb]])
                self.act(ex[xs][:, 0:512], B_[la][:, 0:512], AF.Exp, rd=[bres[la]], wr=[rex[xs]])
                self.act(ex[xs][:, 512:768], B_[lb][:, 0:256], AF.Exp, rd=[bres[lb]], wr=[rex[xs]])
                st_, sp_ = (si == 0), (si == nS - 1)
                self.mm(B_[4][:, 0:512], ctok[:, s_], ex[xs][:, 0:512], start=st_, stop=sp_, rd=[rctok, rex[xs]], wr=[bres[4]])
                self.mm(B_[5][:, 0:256], ctok[:, s_], ex[xs][:, 512:768], start=st_, stop=sp_, rd=[rctok, rex[xs]], wr=[bres[5]])
                self.mm(B_[6][:, 0:512], self.ones_b, ex[xs][:, 0:512], start=st_, stop=sp_, rd=[rc, rex[xs]], wr=[bres[6]])
                self.mm(B_[7][:, 0:256], self.ones_b, ex[xs][:, 512:768], start=st_, stop=sp_, rd=[rc, rex[xs]], wr=[bres[7]])

        def attf(q):
            self.rr = [0, 1, 2, 3]
            self.recip(rden[:, 0:512], B_[6][:, 0:512], rd=[bres[6]], wr=[rrd])
            self.recip(rden[:, 512:768], B_[7][:, 0:256], rd=[bres[7]], wr=[rrd])
            self.tt('dve', oln[:, 0:512], B_[4][:, 0:512], rden[:, 0:512], ALU.mult, rd=[bres[4], rrd], wr=[roln])
            self.tt('dve', oln[:, 512:768], B_[5][:, 0:256], rden[:, 512:768], ALU.mult, rd=[bres[5], rrd], wr=[roln])
            b = self.nb()
            for h in range(6):
                j, e = h // 2, h % 2
                pr = slice(e * 64, (e + 1) * 64)
                self.mm(B_[b][pr, j * 128:(j + 1) * 128], P['wuv'][:, h * 64:(h + 1) * 64], oln[:, h * 128:(h + 1) * 128],
                        rd=[R['smw'], roln], wr=[bres[b]])
            for j in range(3):
                self.cp('act', oT[:, (3 + j) * N + q * 128:(3 + j) * N + (q + 1) * 128], B_[b][:, j * 128:(j + 1) * 128],
                        rd=[bres[b]], wr=[ores[3 + j]])

        idx(0)
        idx(1)
        topk(0)
        attm(0)
        topk(1)
        attf(0)
        attm(1)
        attf(1)
        self.rr = list(range(8))


def _prep_ffn(wg, wu, wd):
    def gu(w):
        wp = np.zeros((D, DFFP), np.float32)
        wp[:, :DFF] = w
        return np.ascontiguousarray(wp.reshape(8, 128, NFC, 128).transpose(2, 1, 0, 3)).reshape(NFC, 128, 8 * 128)
    wdp = np.zeros((DFFP, D), np.float32)
    wdp[:DFF] = wd
    wdt = np.ascontiguousarray(wdp.reshape(NFC, 128, 8, 128).transpose(2, 1, 0, 3)).reshape(8, 128, NFC * 128)
    return np.stack([gu(wg), gu(wu)]), wdt


def _col(v, n):
    return np.ascontiguousarray(np.asarray(v, np.float32).reshape(n, 128).T)


def _consts():
    cf = np.zeros((128, CFN), np.float32)
    cb = np.zeros((128, CBN), np.float32)
    r = np.arange(128)[:, None]
    c = np.arange(128)[None, :]
    cf[:, CF['maskL']:CF['maskL'] + 384] = np.tile((c < r).astype(np.float32), (1, 3))
    cf[:, CF['maskU']:CF['maskU'] + 384] = np.tile((r < c).astype(np.float32), (1, 3))
    cf[:, CF['maskUi']:CF['maskUi'] + 384] = np.tile((r <= c).astype(np.float32), (1, 3))
    gam = 1.0 - 2.0 ** (-5.0 - np.arange(4, dtype=np.float64))
    lg = np.log(gam)
    for h in range(4):
        dm = (c - r).astype(np.float64)
        cf[:, CF['intraT'] + h * 128:CF['intraT'] + (h + 1) * 128] = np.where(dm >= 0, np.exp(np.maximum(dm, 0) * lg[h]), 0.0)
    p = np.arange(128)
    partner = np.where((p % 64) < 32, p + 32, p - 32)
    prot = np.zeros((128, 128), np.float32)
    prot[partner, p] = 1.0
    cf[:, CF['prot']:CF['prot'] + 128] = prot
    rs = np.ones((128, NM), np.float32)
    rs[:, 0::128] = 0.0
    cf[:, CF['reset']:CF['reset'] + NM] = rs
    cf[:, CF['cmask']:CF['cmask'] + 128] = np.where(c <= r, 0.0, -1e30)
    sel = np.zeros((128, 1024), np.float32)
    for h in range(8):
        sel[h, h * 128:(h + 1) * 128] = 1.0
    cf[:, CF['sel']:CF['sel'] + 1024] = sel
    n = (np.arange(NM) % 128).astype(np.float64)
    for j in range(2):
        hh = 2 * j + (p // 64)
        cf[:, CF['qdec'] + j * NM:CF['qdec'] + (j + 1) * NM] = np.exp((n[None, :] + 1.0) * lg[hh][:, None])
        cf[:, CF['kdec'] + j * NM:CF['kdec'] + (j + 1) * NM] = np.exp((127.0 - n[None, :]) * lg[hh][:, None])
        cf[:, CF['cdec'] + j] = np.exp(128.0 * lg[hh])
    cf[:, CF['negbig']] = -1e29
    cb[:, CB['ident']:CB['ident'] + 768] = np.tile(np.eye(128, dtype=np.float32), (1, 6))
    bo = np.zeros((128, 128), np.float32)
    bo[:64, :64] = 1.0
    bo[64:, 64:] = 1.0
    cb[:, CB['bones']:CB['bones'] + 128] = bo
    cb[:, CB['bmean']:CB['bmean'] + 128] = bo / 64.0
    cb[:, CB['ones']:CB['ones'] + 128] = 1.0
    half = 32
    theta = 10000.0 ** (-np.linspace(0.0, 1.0, half))
    i = p % 32
    ang = np.arange(T, dtype=np.float64)[None, :] * theta[i][:, None]
    ang32 = (np.arange(T, dtype=np.float32)[None, :] * theta.astype(np.float32)[i][:, None]).astype(np.float64)
    sign = np.where((p % 64) < 32, -1.0, 1.0)[:, None]
    rot = np.zeros((128, 4, T), np.float32)
    rot[:, 0] = np.cos(ang32)
    rot[:, 1] = np.sin(ang32) * sign
    rot[:, 2] = np.cos(ang32) * 0.125
    rot[:, 3] = np.sin(ang32) * sign * 0.125
    return cf, cb, rot


def make_inputs(bld, inp):
    nl = bld.nlayers
    wgu = np.zeros((nl * 2, 2, NFC, 128, 8 * 128), np.float32)
    wd = np.zeros((nl * 2, 8, 128, NFC * 128), np.float32)
    win = np.zeros((nl, NCH, 128, 8 * 128), np.float32)
    wout = np.zeros((nl, 8, 128, 8 * 128), np.float32)
    smw = np.zeros((nl, 128, 4 * 384), np.float32)
    vec = np.zeros((128, VLN * nl + 8), np.float32)
    for l in range(nl):
        for j, nm in enumerate(('ffn1', 'ffn2')):
            a, b = _prep_ffn(inp[nm + '_w_gate'][l], inp[nm + '_w_up'][l], inp[nm + '_w_down'][l])
            wgu[2 * l + j] = a
            wd[2 * l + j] = b
        w_in = inp['w_in'][l]
        for ci, (name, cols) in enumerate(CHUNKS):
            if cols == 'mv':
                if l == 0:
                    continue
                src = inp['rwkv_vres_w_in'][l - 1]
            else:
                src = w_in[:, cols]
            M = src.shape[1]
            img = np.zeros((8, 128, 128), np.float32)
            img[:, :, :M] = src.reshape(8, 128, M)
            win[l, ci] = img.transpose(1, 0, 2).reshape(128, 8 * 128)
        wo = inp['w_out'][l]
        wout[l] = wo.reshape(8, 128, 8, 128).transpose(2, 1, 0, 3).reshape(8, 128, 8 * 128)
        smw[l, 0:32, 0:384] = inp['rwkv_w2'][l]
        smw[l, 32:64, 0:384] = inp['rwkv_a2'][l]
        smw[l, 64:128, 0:384] = inp['rwkv_g2'][l]
        if l > 0:
            smw[l, 0:16, 384:768] = inp['rwkv_v2'][l - 1]
        wuk = inp['dsa_w_uk'][l]
        for h in range(6):
            j, e = h // 2, h % 2
            smw[l, e * 64:(e + 1) * 64, 768 + j * 128:768 + (j + 1) * 128] = wuk[h]
        wuv = inp['dsa_w_uv'][l]
        for h in range(6):
            smw[l, :, 1152 + h * 64:1152 + (h + 1) * 64] = wuv[h]
        o = VLN * l
        vec[:, o + VL['ffn1']:o + VL['ffn1'] + 8] = _col(inp['ffn1_norm'][l], 8)
        vec[:, o + VL['ffn2']:o + VL['ffn2'] + 8] = _col(inp['ffn2_norm'][l], 8)
        vec[:, o + VL['mix']:o + VL['mix'] + 8] = _col(inp['mix_norm'][l], 8)
        vec[:, o + VL['mu']:o + VL['mu'] + 10] = _col(inp['rwkv_mu'][l], 10)
        if l > 0:
            vec[:16, o + VL['mumv']] = inp['rwkv_vres_mu'][l - 1]
            vec[:, o + VL['v0']:o + VL['v0'] + 3] = _col(inp['rwkv_v0'][l - 1], 3)
        vec[:, o + VL['w0']:o + VL['w0'] + 3] = _col(inp['rwkv_w0'][l], 3)
        vec[:, o + VL['a0']:o + VL['a0'] + 3] = _col(inp['rwkv_a0'][l], 3)
        vec[:, o + VL['kk']:o + VL['kk'] + 3] = _col(inp['rwkv_k_k'][l], 3)
        vec[:, o + VL['ka']:o + VL['ka'] + 3] = _col(inp['rwkv_k_a'][l], 3)
        vec[:, o + VL['rk']:o + VL['rk'] + 3] = _col(inp['rwkv_r_k'][l].reshape(-1), 3)
        vec[:, o + VL['lnw']:o + VL['lnw'] + 3] = _col(inp['rwkv_ln_w'][l], 3)
        vec[:, o + VL['lnb']:o + VL['lnb'] + 3] = _col(inp['rwkv_ln_b'][l], 3)
        vec[:, o + VL['kvn']] = inp['dsa_kv_norm'][l]
    vec[:, VLN * nl:VLN * nl + 8] = _col(inp['final_norm'], 8)
    cf, cb, rot = _consts()
    return {'wgu': wgu, 'wd': wd, 'win': win, 'wout': wout, 'smw': smw, 'vec': vec, 'cf': cf, 'cb': cb, 'rot': rot}


def kernel(**inputs):
    inp = {k_: np.asarray(v) for k_, v in inputs.items()}
    bld = B()
    nc = bld.build()
    shared = make_inputs(bld, inp)
    x = inp['x']
    in_maps = []
    for b in range(8):
        m = dict(shared)
        m['xT'] = np.ascontiguousarray(x[b].T).reshape(8, 128, T)
        in_maps.append(m)
    res = run_bass_kernel_spmd(nc, in_maps, core_ids=list(range(8)))
    out = np.stack([np.ascontiguousarray(np.asarray(r['outT']).reshape(D, T).T) for r in res.results])
    return out.astype(np.float32)
```

```python
import math
from contextlib import ExitStack
import numpy as np
import concourse.bass as bass
import concourse.mybir as mybir
from concourse.bass_utils import run_bass_kernel_spmd

F32 = mybir.dt.float32
BF16 = mybir.dt.bfloat16
ALU = mybir.AluOpType
AF = mybir.ActivationFunctionType
AX = mybir.AxisListType

ENG = ('pe', 'act', 'dve', 'pool', 'sp')

D = 1024
T = 2048
L = 2
DFF = 2752
DFFP = 2816
NFC = 22
NT = 512
NTB = T // NT
NM = 256
NMB = T // NM
NQ = NM // 128
EPS = 1e-6
GN_EPS = 64e-5
C0 = math.exp(-0.5)
NCH = 29
NIT_TOPK = 12


class Res:
    __slots__ = ('name', 'w', 'r')
    REG = []
    INIT = []

    def __init__(self, name=''):
        self.name = name
        self.w = None
        self.r = list(Res.INIT)


def soft_barrier(k):
    evs = [(('e', e), k.cnt[e]) for e in ENG if k.cnt[e] > 0]
    evs += list(k.last_dma.values())
    Res.INIT = evs
    Res.REG = []


class KB:
    def __init__(self, nc, same_engine_sync=True, dma_ring=8):
        self.nc = nc
        self.ops = {e: [] for e in ENG}
        self.cnt = {e: 0 for e in ENG}
        self.waited = {e: {} for e in ENG}
        self.same = same_engine_sync
        self.same_only = None
        self.ring = dma_ring
        self.dma_n = {e: 0 for e in ENG}
        self.semh = {}
        self.semkeys = [('e', e) for e in ENG]
        for e in ('sp', 'pool', 'act'):
            for i in range(dma_ring):
                self.semkeys.append(('d', e, i))
        self.last_dma = {}
        self.n_wait = 0

    def _wait(self, eng, ev):
        if ev is None:
            return
        key, val = ev
        if key == ('e', eng) and (eng == 'pe' or not self.same or (self.same_only is not None and eng not in self.same_only)):
            return
        cur = self.waited[eng].get(key, 0)
        if cur >= val:
            return
        self.waited[eng][key] = val
        self.n_wait += 1
        self.ops[eng].append(('wait', key, val))

    def _deps(self, eng, reads, writes):
        for r in reads:
            self._wait(eng, r.w)
        for w in writes:
            self._wait(eng, w.w)
            for ev in w.r:
                self._wait(eng, ev)

    def _commit(self, ev, reads, writes):
        for w in writes:
            w.w = ev
            w.r = []
        for r in reads:
            if r not in writes:
                r.r.append(ev)
                if len(r.r) > 64:
                    r.r = r.r[-64:] if False else r.r

    def op(self, eng, fn, reads=(), writes=()):
        reads = list(reads)
        writes = list(writes)
        self._deps(eng, reads, writes)
        self.cnt[eng] += 1
        ev = (('e', eng), self.cnt[eng])
        self.ops[eng].append(('op', fn, ('e', eng), 1))
        self._commit(ev, reads, writes)
        return ev

    def dma(self, q, fn, reads=(), writes=()):
        reads = list(reads)
        writes = list(writes)
        self._deps(q, reads, writes)
        j = self.dma_n[q]
        self.dma_n[q] += 1
        slot = j % self.ring
        key = ('d', q, slot)
        tgt = 16 * (j // self.ring + 1)
        if j >= self.ring:
            self._wait(q, (key, tgt - 16))
        ev = (key, tgt)
        self.last_dma[key] = ev
        self.ops[q].append(('op', fn, key, 16))
        self._commit(ev, reads, writes)
        return ev

    def wait_all(self, eng, evs):
        for ev in evs:
            self._wait(eng, ev)

    def barrier(self):
        Res.INIT = []
        Res.REG = []
        evs = [(('e', e), self.cnt[e]) for e in ENG if self.cnt[e] > 0]
        evs += list(self.last_dma.values())
        for e in ENG:
            for ev in evs:
                self._wait(e, ev)

    def finalize(self, stack):
        nc = self.nc
        for kk in self.semkeys:
            self.semh[kk] = stack.enter_context(nc.semaphore('s_' + '_'.join(str(x) for x in kk)))
        block = stack.enter_context(nc.Block())
        semh = self.semh

        def run(eng_name):
            def body(eng):
                for it in self.ops[eng_name]:
                    if it[0] == 'wait':
                        eng.wait_ge(semh[it[1]], it[2])
                    else:
                        ins = it[1](eng)
                        ins.then_inc(semh[it[2]], it[3])
            return body

        block.tensor(run('pe'))
        block.scalar(run('act'))
        block.vector(run('dve'))
        block.gpsimd(run('pool'))
        block.sync(run('sp'))


class Arena:
    def __init__(self, ap, nwords):
        self.ap = ap
        self.n = nwords
        self.top = 0
        self.peak = 0

    def mark(self):
        return self.top

    def release(self, m):
        self.top = m

    def f32(self, n):
        a = self.ap[:, self.top:self.top + n]
        self.top += n
        self.peak = max(self.peak, self.top)
        assert self.top <= self.n, ("arena overflow", self.top, self.n)
        return a

    def bf16(self, n):
        w = (n + 1) // 2
        return self.f32(w).bitcast(BF16)[:, 0:n]


def win_chunks():
    ch = []
    for j in range(10):
        ch.append(('a%d' % j, list(range(j * 128, (j + 1) * 128))))
    ch.append(('mv', 'mv'))
    for j in range(3):
        ch.append(('bq%d' % j, list(range(1280 + j * 128, 1280 + (j + 1) * 128))))
    ch.append(('bc', list(range(1664, 1792))))
    for j in range(4):
        ch.append(('biq%d' % j, list(range(1792 + j * 128, 1792 + (j + 1) * 128))))
    ch.append(('bik2', list(range(2304, 2368)) * 2))
    ch.append(('biw', list(range(2368, 2376))))
    for n, base in (('cq', 2376), ('ck', 2632), ('cv', 2888), ('cg', 3144)):
        for j in range(2):
            ch.append((n + str(j), list(range(base + j * 128, base + (j + 1) * 128))))
    return ch


CHUNKS = win_chunks()
CIDX = {c[0]: i for i, c in enumerate(CHUNKS)}

VL = {}
_o = 0
for _n, _w in (('ffn1', 8), ('ffn2', 8), ('mix', 8), ('mu', 10), ('mumv', 1), ('w0', 3), ('a0', 3), ('kk', 3), ('ka', 3),
               ('rk', 3), ('lnw', 3), ('lnb', 3), ('v0', 3), ('kvn', 1)):
    VL[_n] = _o
    _o += _w
VLN = _o

CF = {}
_o = 0
for _n, _w in (('maskL', 384), ('maskU', 384), ('maskUi', 384), ('intraT', 512), ('prot', 128), ('reset', NM), ('cmask', 128),
               ('sel', 1024), ('qdec', 2 * NM), ('kdec', 2 * NM), ('cdec', 2), ('negbig', 1)):
    CF[_n] = _o
    _o += _w
CFN = _o
CB = {}
_o = 0
for _n, _w in (('ident', 768), ('bones', 128), ('bmean', 128), ('ones', 128)):
    CB[_n] = _o
    _o += _w
CBN = _o


class B:
    def __init__(self, nlayers=L, do_ffn=True, do_mix=True, dbg=None, mixers=('a', 'b', 'c')):
        self.nlayers = nlayers
        self.do_ffn = do_ffn
        self.do_mix = do_mix
        self.dbg = dbg or {}
        self.mixers = mixers

    def mm(self, out, lhsT, rhs, start=True, stop=True, rd=(), wr=()):
        return self.k.op('pe', lambda e: e.matmul(out, lhsT=lhsT, rhs=rhs, start=start, stop=stop), rd, wr)

    def act(self, out, in_, func, rd=(), wr=(), **kw):
        return self.k.op('act', lambda e: e.activation(out=out, in_=in_, func=func, **kw), rd, wr)

    def tt(self, eng, out, in0, in1, op, rd=(), wr=()):
        return self.k.op(eng, lambda e: e.tensor_tensor(out=out, in0=in0, in1=in1, op=op), rd, wr)

    def stt(self, eng, out, in0, scalar, in1, op0, op1, rd=(), wr=()):
        return self.k.op(eng, lambda e: e.scalar_tensor_tensor(out=out, in0=in0, scalar=scalar, in1=in1, op0=op0, op1=op1), rd, wr)

    def ts(self, eng, out, in0, s1, s2, op0, op1=None, rd=(), wr=()):
        if op1 is None:
            return self.k.op(eng, lambda e: e.tensor_scalar(out=out, in0=in0, scalar1=s1, scalar2=None, op0=op0), rd, wr)
        return self.k.op(eng, lambda e: e.tensor_scalar(out=out, in0=in0, scalar1=s1, scalar2=s2, op0=op0, op1=op1), rd, wr)

    def cp(self, eng, out, in_, rd=(), wr=()):
        if eng == 'act':
            return self.k.op('act', lambda e: e.activation(out=out, in_=in_, func=AF.Copy), rd, wr)
        return self.k.op(eng, lambda e: e.tensor_copy(out=out, in_=in_), rd, wr)

    def recip(self, out, in_, rd=(), wr=()):
        return self.k.op('dve', lambda e: e.reciprocal(out=out, in_=in_), rd, wr)

    def dma(self, q, out, in_, rd=(), wr=()):
        return self.k.dma(q, lambda e: e.dma_start(out=out, in_=in_), rd, wr)

    def nb(self):
        b = self.rr[self.rri % len(self.rr)]
        self.rri += 1
        return b

    def dump(self, name, ap, reads):
        if name not in self.dbg:
            return
        shape = list(ap.shape)
        d = self.nc.dram_tensor("dbg_" + name, shape, ap.dtype, kind="ExternalOutput").ap()
        ev = self.k.dma('sp', lambda e: e.dma_start(out=d, in_=ap), reads=reads)
        self.dbg_evs.append(ev)

    def build(self):
        nc = bass.Bass("TRN2", target_bir_lowering=False)
        self.nc = nc
        nl = self.nlayers
        Res.REG = []
        Res.INIT = []
        dr = {}
        dr['xT'] = nc.dram_tensor("xT", [8, 128, T], F32, kind="ExternalInput").ap()
        dr['outT'] = nc.dram_tensor("outT", [8, 128, T], F32, kind="ExternalOutput").ap()
        dr['wgu'] = nc.dram_tensor("wgu", [nl * 2, 2, NFC, 128, 8 * 128], F32, kind="ExternalInput").ap()
        dr['wd'] = nc.dram_tensor("wd", [nl * 2, 8, 128, NFC * 128], F32, kind="ExternalInput").ap()
        dr['wgu_b'] = nc.dram_tensor("wgu_b", [nl * 2, 2, NFC, 128, 8 * 128], BF16, kind="Internal").ap()
        dr['wd_b'] = nc.dram_tensor("wd_b", [nl * 2, 8, 128, NFC * 128], BF16, kind="Internal").ap()
        dr['win'] = nc.dram_tensor("win", [nl, NCH, 128, 8 * 128], F32, kind="ExternalInput").ap()
        dr['win_b'] = nc.dram_tensor("win_b", [nl, NCH, 128, 8 * 128], BF16, kind="Internal").ap()
        dr['wout'] = nc.dram_tensor("wout", [nl, 8, 128, 8 * 128], F32, kind="ExternalInput").ap()
        dr['wout_b'] = nc.dram_tensor("wout_b", [nl, 8, 128, 8 * 128], BF16, kind="Internal").ap()
        dr['smw'] = nc.dram_tensor("smw", [nl, 128, 384 + 384 + 384 + 384], F32, kind="ExternalInput").ap()
        dr['vec'] = nc.dram_tensor("vec", [128, VLN * nl + 8], F32, kind="ExternalInput").ap()
        dr['cf'] = nc.dram_tensor("cf", [128, CFN], F32, kind="ExternalInput").ap()
        dr['cb'] = nc.dram_tensor("cb", [128, CBN], F32, kind="ExternalInput").ap()
        dr['rot'] = nc.dram_tensor("rot", [128, 4, T], F32, kind="ExternalInput").ap()
        dr['vf'] = nc.dram_tensor("vf", [128, 3, T], F32, kind="Internal").ap()
        self.dr = dr

        with ExitStack() as st:
            self.st = st
            k = KB(nc, same_engine_sync=not self.dbg.get('nosame'))
            k.same_only = self.dbg.get('same_only')
            self.k = k
            self.xT = st.enter_context(nc.sbuf_tensor("xT_sb", [128, 8, T], F32))
            self.xres = [[Res() for _ in range(NMB)] for _ in range(8)]
            self.vec = st.enter_context(nc.sbuf_tensor("vec_sb", [128, VLN * nl + 8], F32))
            self.rvec = Res()
            self.cf = st.enter_context(nc.sbuf_tensor("cf_sb", [128, CFN], F32))
            self.cb = st.enter_context(nc.sbuf_tensor("cb_sb", [128, CBN], BF16))
            self.rconst = Res()
            self.ones_b = self.cb[:, CB['ones']:CB['ones'] + 128]
            self.ident_b = self.cb[:, CB['ident']:CB['ident'] + 128]
            self.bones_b = self.cb[:, CB['bones']:CB['bones'] + 128]
            self.bmean_b = self.cb[:, CB['bmean']:CB['bmean'] + 128]
            rem = nc.sbuf_bytes_remaining
            AW = (rem - 2048) // 4
            arena_t = st.enter_context(nc.sbuf_tensor("arena", [128, AW], F32))
            self.ar = Arena(arena_t, AW)
            self.banks = [st.enter_context(nc.psum_tensor("bank%d" % i, [128, 512], F32)) for i in range(8)]
            self.bres = [Res() for _ in range(8)]
            self.rr = list(range(8))
            self.rri = 0
            self.dbg_evs = []
            self.vfres = [Res() for _ in range(NMB)]

            self.prologue()
            for l in range(nl):
                if self.do_ffn:
                    self.ffn_phase(l, 0)
                self.cast_layer(l + 1, defer=True)
                if self.do_mix:
                    self.mix_phase(l)
                self.flush_casts()
                if self.do_ffn and not self.dbg.get('skip_ffn2'):
                    self.ffn_phase(l, 1)
            self.final_phase()
            k.finalize(st)
        return nc

    def vc(self, l, name, j=0):
        c = VLN * l + VL[name] + j
        return self.vec[:, c:c + 1]

    def xr(self, c, t0, n):
        return [self.xres[c][b] for b in range(t0 // NM, (t0 + n) // NM)]

    def prologue(self):
        k, nc, dr = self.k, self.nc, self.dr
        for c in range(8):
            self.dma('sp', self.xT[:, c, :], dr['xT'][c], wr=[self.xres[c][b] for b in range(NMB)])
        self.dma('act', self.vec[:], dr['vec'][:, :], wr=[self.rvec])
        self.dma('act', self.cf[:], dr['cf'][:, :], wr=[self.rconst])
        self.dma('pool', self.cb[:], dr['cb'][:, :], wr=[self.rconst])
        self.rwgu = {}
        self.rwd = {}
        self.rwin = {}
        self.rwout = {}
        self.cast_layer(0)

    def cast_layer(self, l, defer=False):
        if l >= self.nlayers:
            return
        th = []
        if self.do_ffn:
            th += self.cast_ffn(2 * l)
        if self.do_mix:
            th += self.cast_mix(l)
        if self.do_ffn and not self.dbg.get('skip_ffn2'):
            th += self.cast_ffn(2 * l + 1)
        if defer:
            self.pending = th
        else:
            for f in th:
                f()

    def flush_casts(self, n=None):
        p = getattr(self, 'pending', [])
        n = len(p) if n is None else min(n, len(p))
        for f in p[:n]:
            f()
        self.pending = p[n:]

    def cast_ffn(self, i):
        dr = self.dr
        th = []
        for gu in range(2):
            for fc in range(NFC):
                r = Res()
                self.rwgu[(i, gu, fc)] = r
                th.append(lambda i=i, gu=gu, fc=fc, r=r: self.dma('pool', dr['wgu_b'][i, gu, fc], dr['wgu'][i, gu, fc], wr=[r]))
        for dc in range(8):
            r = Res()
            self.rwd[(i, dc)] = r
            th.append(lambda i=i, dc=dc, r=r: self.dma('pool', dr['wd_b'][i, dc].rearrange("p (a f) -> (p a) f", a=2),
                                                      dr['wd'][i, dc].rearrange("p (a f) -> (p a) f", a=2), wr=[r]))
        return th

    def cast_mix(self, l):
        dr = self.dr
        th = []
        for ci in range(NCH):
            r = Res()
            self.rwin[(l, ci)] = r
            th.append(lambda l=l, ci=ci, r=r: self.dma('pool', dr['win_b'][l, ci], dr['win'][l, ci], wr=[r]))
        for dc in range(8):
            r = Res()
            self.rwout[(l, dc)] = r
            th.append(lambda l=l, dc=dc, r=r: self.dma('pool', dr['wout_b'][l, dc], dr['wout'][l, dc], wr=[r]))
        return th

    def rmsnorm_to(self, t0, n, gcol, hT, hres, sq, sqres, rstd, rres):
        k = self.k
        ts_ = slice(t0, t0 + n)
        bank = self.nb()
        ps = self.banks[bank]
        for c in range(8):
            s = c % 2
            self.act(sq[s], self.xT[:, c, ts_], AF.Square, rd=self.xr(c, t0, n), wr=[sqres[s]])
            self.mm(ps[:, :n], self.ones_b, sq[s], start=(c == 0), stop=(c == 7), rd=[sqres[s], self.rconst], wr=[self.bres[bank]])
        self.act(rstd, ps[:, :n], AF.Sqrt, rd=[self.bres[bank]], wr=[rres], bias=EPS, scale=1.0 / D)
        self.recip(rstd, rstd, rd=[rres], wr=[rres])
        for c in range(8):
            self.stt('dve', hT[:, c, :], self.xT[:, c, ts_], self.vec[:, gcol + c:gcol + c + 1], rstd, ALU.mult, ALU.mult,
                     rd=self.xr(c, t0, n) + [rres, self.rvec], wr=[hres[c]])

    def ffn_phase(self, l, j):
        k, nc, dr, ar = self.k, self.nc, self.dr, self.ar
        i = 2 * l + j
        k.barrier()
        m = ar.mark()
        self.rr = [0]
        hTs = [ar.bf16(8 * NT).rearrange("p (c t) -> p c t", c=8) for _ in range(2)]
        hress = [[Res() for _ in range(8)] for _ in range(2)]
        aT = ar.bf16(NFC * NT).rearrange("p (c t) -> p c t", c=NFC)
        ares = [Res() for _ in range(NFC)]
        sq = [ar.bf16(NT) for _ in range(2)]
        sqres = [Res(), Res()]
        rstd = ar.f32(NT)
        rres = Res()
        NW = 3
        wg = [ar.bf16(8 * 128).rearrange("p (c f) -> p c f", c=8) for _ in range(NW)]
        wu = [ar.bf16(8 * 128).rearrange("p (c f) -> p c f", c=8) for _ in range(NW)]
        wgres = [Res() for _ in range(NW)]
        wures = [Res() for _ in range(NW)]
        wd = [ar.bf16(NFC * 128).rearrange("p (c f) -> p c f", c=NFC) for _ in range(2)]
        wdres = [Res(), Res()]
        sg = [ar.bf16(NT) for _ in range(2)]
        sgres = [Res(), Res()]
        gcol = VLN * l + VL['ffn1' if j == 0 else 'ffn2']
        B_ = self.banks
        self.rmsnorm_to(0, NT, gcol, hTs[0], hress[0], sq, sqres, rstd, rres)
        for tb in range(NTB):
            t0 = tb * NT
            ts_ = slice(t0, t0 + NT)
            hT, hres = hTs[tb % 2], hress[tb % 2]
            for fc in range(NFC):
                s = fc % NW
                self.dma('sp', wg[s], dr['wgu_b'][i, 0, fc].rearrange("p (c f) -> p c f", c=8), rd=[self.rwgu[(i, 0, fc)]], wr=[wgres[s]])
                self.dma('sp', wu[s], dr['wgu_b'][i, 1, fc].rearrange("p (c f) -> p c f", c=8), rd=[self.rwgu[(i, 1, fc)]], wr=[wures[s]])
                gb = 1 + fc % 2
                ub = 3 + fc % 2
                for kc in range(8):
                    self.mm(B_[gb][:, :NT], wg[s][:, kc, :], hT[:, kc, :], start=(kc == 0), stop=(kc == 7),
                            rd=[wgres[s], hres[kc]], wr=[self.bres[gb]])
                for kc in range(8):
                    self.mm(B_[ub][:, :NT], wu[s][:, kc, :], hT[:, kc, :], start=(kc == 0), stop=(kc == 7),
                            rd=[wures[s], hres[kc]], wr=[self.bres[ub]])
                s2 = fc % 2
                self.act(sg[s2], B_[gb][:, :NT], AF.Silu, rd=[self.bres[gb]], wr=[sgres[s2]])
                self.tt('dve', aT[:, fc, :], B_[ub][:, :NT], sg[s2], ALU.mult, rd=[self.bres[ub], sgres[s2]], wr=[ares[fc]])
            if tb + 1 < NTB:
                self.rmsnorm_to(t0 + NT, NT, gcol, hTs[(tb + 1) % 2], hress[(tb + 1) % 2], sq, sqres, rstd, rres)
            for dc in range(8):
                s = dc % 2
                self.dma('sp', wd[s], dr['wd_b'][i, dc].rearrange("p (c f) -> p c f", c=NFC), rd=[self.rwd[(i, dc)]], wr=[wdres[s]])
                yb = 5 + dc % 2
                for fc in range(NFC):
                    self.mm(B_[yb][:, :NT], wd[s][:, fc, :], aT[:, fc, :], start=(fc == 0), stop=(fc == NFC - 1),
                            rd=[wdres[s], ares[fc]], wr=[self.bres[yb]])
                self.stt('dve', self.xT[:, dc, ts_], B_[yb][:, :NT], 0.5, self.xT[:, dc, ts_], ALU.mult, ALU.add,
                         rd=[self.bres[yb]] + self.xr(dc, t0, NT), wr=self.xr(dc, t0, NT))
        ar.release(m)

    def final_phase(self):
        k, nc, dr, ar = self.k, self.nc, self.dr, self.ar
        k.barrier()
        m = ar.mark()
        self.rr = [0, 1]
        sq = [ar.bf16(NT) for _ in range(2)]
        sqres = [Res(), Res()]
        rstd = ar.f32(NT)
        rres = Res()
        o = [ar.f32(8 * NT).rearrange("p (c t) -> p c t", c=8) for _ in range(2)]
        ores = [[Res() for _ in range(8)] for _ in range(2)]
        evs = []
        gcol = VLN * self.nlayers
        for tb in range(NTB):
            s = tb % 2
            t0 = tb * NT
            self.rmsnorm_to(t0, NT, gcol, o[s], ores[s], sq, sqres, rstd, rres)
            for c in range(8):
                evs.append(self.dma('sp', dr['outT'][c][:, t0:t0 + NT], o[s][:, c, :], rd=[ores[s][c]]))
        k.wait_all('sp', evs + self.dbg_evs)
        ar.release(m)

    def mix_phase(self, l):
        k, dr, ar = self.k, self.dr, self.ar
        k.barrier()
        m0 = ar.mark()
        self.rr = list(range(8))
        P = self.P = {}
        R = self.R = {}

        def alloc(name, kind, n):
            P[name] = ar.bf16(n) if kind == 'b' else ar.f32(n)
            R[name] = Res(name)
        alloc('pa', 'f', 11 * (NM + 1))
        alloc('Sf', 'f', 384)
        alloc('Sb', 'b', 384)
        alloc('rSf', 'f', 256)
        alloc('rSb', 'b', 256)
        alloc('cT', 'b', T)
        alloc('ctok', 'b', T)
        alloc('ik2', 'b', T)
        alloc('smw', 'b', 4 * 384)
        alloc('omka', 'f', 4)
        for nm in ('pa', 'Sf', 'Sb', 'rSf', 'rSb'):
            self.k.op('pool', lambda e, a=P[nm]: e.memset(a, 0.0), (), [R[nm]])
        self.dma('pool', P['smw'], dr['smw'][l], wr=[R['smw']])
        P['low'] = P['smw'][:, 0:384]
        P['v2'] = P['smw'][:, 384:768]
        P['wuk'] = P['smw'][:, 768:1152]
        P['wuv'] = P['smw'][:, 1152:1536]
        c = VLN * l + VL['ka']
        self.ts('dve', P['omka'][:, 0:3], self.vec[:, c:c + 3], -1.0, 1.0, ALU.mult, ALU.add, rd=[self.rvec], wr=[R['omka']])
        self.wring = [ar.bf16(1024) for _ in range(4)]
        self.wrres = [Res() for _ in range(4)]
        self.wri = 0
        for mb in range(NMB):
            self.mix_block(l, mb)
            if self.dbg.get('max_mb') is not None and mb >= self.dbg['max_mb']:
                break
        k.barrier()
        ar.release(m0)

    def sbar(self):
        if self.dbg.get('hardbar'):
            self.k.barrier()
        else:
            soft_barrier(self.k)

    def proj(self, l, name, M, hT, hres):
        ci = CIDX[name]
        s = self.wri % 4
        self.wri += 1
        w = self.wring[s]
        self.dma('sp', w, self.dr['win_b'][l, ci], rd=[self.rwin[(l, ci)]], wr=[self.wrres[s]])
        b = self.nb()
        for kc in range(8):
            self.mm(self.banks[b][:M, :NM], w[:, kc * 128:kc * 128 + M], hT[:, kc, :], start=(kc == 0), stop=(kc == 7),
                    rd=[self.wrres[s], hres[kc]], wr=[self.bres[b]])
        return b

    def mix_block(self, l, mb):
        k, dr, ar = self.k, self.dr, self.ar
        t0 = mb * NM
        m = ar.mark()
        hT = ar.bf16(8 * NM).rearrange("p (c t) -> p c t", c=8)
        hres = [Res() for _ in range(8)]
        sq = [ar.bf16(NM) for _ in range(2)]
        sqres = [Res(), Res()]
        rstd = ar.f32(NM)
        rres = Res()
        oT = ar.bf16(8 * NM)
        ores = [Res() for _ in range(8)]
        self.rr = list(range(8))
        self.k.op('pool', lambda e: e.memset(oT, 0.0), (), ores)
        self.rmsnorm_to(t0, NM, VLN * l + VL['mix'], hT, hres, sq, sqres, rstd, rres)
        self.flush_casts((len(getattr(self, 'pending', [])) + (NMB - mb) - 1) // (NMB - mb))
        if 'a' in self.mixers:
            m1 = ar.mark()
            self.rwkv_block(l, mb, hT, hres, oT, ores)
            self.sbar()
            ar.release(m1)
        if 'c' in self.mixers:
            m1 = ar.mark()
            self.ret_block(l, mb, hT, hres, oT, ores)
            self.sbar()
            ar.release(m1)
        if 'b' in self.mixers:
            m1 = ar.mark()
            self.dsa_block(l, mb, hT, hres, oT, ores)
            self.sbar()
            ar.release(m1)
        if l == 0:
            self.dump('oT%d' % mb, oT, ores)
        self.rr = list(range(8))
        wo = [ar.bf16(1024) for _ in range(2)]
        wores = [Res(), Res()]
        for dc in range(8):
            s = dc % 2
            self.dma('sp', wo[s], dr['wout_b'][l, dc], rd=[self.rwout[(l, dc)]], wr=[wores[s]])
            b = self.nb()
            for mc in range(8):
                self.mm(self.banks[b][:, :NM], wo[s][:, mc * 128:(mc + 1) * 128], oT[:, mc * NM:(mc + 1) * NM],
                        start=(mc == 0), stop=(mc == 7), rd=[wores[s], ores[mc]], wr=[self.bres[b]])
            self.tt('dve', self.xT[:, dc, t0:t0 + NM], self.banks[b][:, :NM], self.xT[:, dc, t0:t0 + NM], ALU.add,
                    rd=[self.bres[b], self.xres[dc][mb]], wr=[self.xres[dc][mb]])
        self.sbar()
        ar.release(m)

    def rwkv_block(self, l, mb, hT, hres, oT, ores):
        ar, P, R, dr = self.ar, self.P, self.R, self.dr
        t0 = mb * NM
        N = NM
        B_ = self.banks
        bres = self.bres
        cf = self.cf
        rc = self.rconst
        pa = P['pa'].rearrange("p (c t) -> p c t", c=11)
        rpa = R['pa']
        nA = 11 if l > 0 else 10
        for j in range(nA):
            name = 'a%d' % j if j < 10 else 'mv'
            M = 128 if j < 10 else 16
            b = self.proj(l, name, M, hT, hres)
            self.cp('act', pa[:M, j, 1:N + 1], B_[b][:M, :N], rd=[bres[b]], wr=[rpa])
        xx = ar.f32(10 * N)
        rxx = [Res() for _ in range(10)]
        dtmp = [ar.f32(N) for _ in range(2)]
        rdt = [Res(), Res()]
        for j in range(10):
            s = j % 2
            self.tt('pool', dtmp[s], pa[:, j, 0:N], pa[:, j, 1:N + 1], ALU.subtract, rd=[rpa], wr=[rdt[s]])
            self.stt('dve', xx[:, j * N:(j + 1) * N], dtmp[s], self.vc(l, 'mu', j), pa[:, j, 1:N + 1], ALU.mult, ALU.add,
                     rd=[rdt[s], rpa, self.rvec], wr=[rxx[j]])
        if self.dbg.get('stop', 99) <= 1:
            return
        mv_b = ar.bf16(N)
        rmv = Res()
        vfb = None
        if l > 0:
            self.tt('pool', dtmp[0][:16], pa[:16, 10, 0:N], pa[:16, 10, 1:N + 1], ALU.subtract, rd=[rpa], wr=[rdt[0]])
            self.stt('dve', mv_b[:16], dtmp[0][:16], self.vc(l, 'mumv')[:16], pa[:16, 10, 1:N + 1], ALU.mult, ALU.add,
                     rd=[rdt[0], rpa, self.rvec], wr=[rmv])
            vfb = ar.f32(3 * N)
            rvfb = Res()
            self.dma('act', vfb.rearrange("p (c t) -> p c t", c=3), dr['vf'][:, :, t0:t0 + N], rd=[self.vfres[mb]], wr=[rvfb])
        for j in range(nA):
            M = 128 if j < 10 else 16
            self.cp('pool', pa[:M, j, 0:1], pa[:M, j, N:N + 1], rd=[], wr=[rpa])
        if self.dbg.get('stop', 99) <= 2:
            return
        lo = xx[:, 9 * N:10 * N]
        lo_b = ar.bf16(N)
        rlo = Res()
        self.act(lo_b[0:32], lo[0:32], AF.Tanh, rd=[rxx[9]], wr=[rlo])
        self.act(lo_b[32:64], lo[32:64], AF.Copy, rd=[rxx[9]], wr=[rlo])
        self.act(lo_b[64:128], lo[64:128], AF.Sigmoid, rd=[rxx[9]], wr=[rlo])
        low = P['low']
        rsw = R['smw']
        at_b = ar.bf16(3 * N)
        bt_b = ar.bf16(3 * N)
        kt_b = ar.bf16(3 * N)
        rt_b = ar.bf16(3 * N)
        gT = ar.bf16(3 * N)
        bonus = ar.f32(3 * N)
        rat, rbt, rkt, rrt, rg, rbo = [Res() for _ in range(6)]
        tok = [ar.bf16(3 * 384) for _ in range(NQ)]
        rtok = [Res() for _ in range(NQ)]
        Ptot = ar.f32(3 * NQ)
        rPt = Res()
        names = ['sgw', 'cs', 'csx', 'Pinc', 'Pexc', 'Pinv', 'Pend', 'a', 'nrm', 'kkn', 't1', 'kp', 'bb', 'sgv']
        tf = {n: ar.f32(N) for n in names}
        rf = {n: Res() for n in names}
        tb16 = {n: ar.bf16(N) for n in ('sqk', 'rk', 'bhT', 'khT', 'vb')}
        rb16 = {n: Res() for n in tb16}
        nbv = ar.f32(4)
        rnb = Res()
        for j in range(3):
            cs_ = slice(j * 128, (j + 1) * 128)
            fs = slice(j * N, (j + 1) * N)
            r_j = xx[:, (0 + j) * N:(1 + j) * N]
            k_j = xx[:, (3 + j) * N:(4 + j) * N]
            v_j = xx[:, (6 + j) * N:(7 + j) * N]
            rr_, rk_, rv_ = rxx[j], rxx[3 + j], rxx[6 + j]
            b = self.nb()
            self.mm(B_[b][:, :N], low[0:32, cs_], lo_b[0:32], rd=[rsw, rlo], wr=[bres[b]])
            self.act(tf['sgw'], B_[b][:, :N], AF.Sigmoid, rd=[bres[b], self.rvec], wr=[rf['sgw']], bias=self.vc(l, 'w0', j))
            self.k.op('dve', lambda e, o=tf['cs'], d0=cf[:, CF['reset']:CF['reset'] + N], d1=tf['sgw']: e.tensor_tensor_scan(
                out=o, data0=d0, data1=d1, initial=0.0, op0=ALU.mult, op1=ALU.add), [rf['sgw'], rc], [rf['cs']])
            self.tt('pool', tf['csx'], tf['cs'], tf['sgw'], ALU.subtract, rd=[rf['cs'], rf['sgw']], wr=[rf['csx']])
            self.act(tf['Pinc'], tf['cs'], AF.Exp, rd=[rf['cs']], wr=[rf['Pinc']], scale=-C0)
            self.act(tf['Pexc'], tf['csx'], AF.Exp, rd=[rf['csx']], wr=[rf['Pexc']], scale=-C0)
            self.act(tf['Pinv'], tf['cs'], AF.Exp, rd=[rf['cs']], wr=[rf['Pinv']], scale=C0)
            for q in range(NQ):
                self.ts('dve', nbv[:, q:q + 1], tf['cs'][:, q * 128 + 127:q * 128 + 128], -C0, None, ALU.mult, rd=[rf['cs']], wr=[rnb])
            for q in range(NQ):
                tq = slice(q * 128, (q + 1) * 128)
                self.act(tf['Pend'][:, tq], tf['cs'][:, tq], AF.Exp, rd=[rf['cs'], rnb], wr=[rf['Pend']], scale=C0, bias=nbv[:, q:q + 1])
            self.act(Ptot[:, j * NQ:(j + 1) * NQ], nbv[:, 0:NQ], AF.Exp, rd=[rnb], wr=[rPt])
            if self.dbg.get('stop', 99) <= 3:
                continue
            b = self.nb()
            self.mm(B_[b][:, :N], low[32:64, cs_], lo_b[32:64], rd=[rsw, rlo], wr=[bres[b]])
            self.act(tf['a'], B_[b][:, :N], AF.Sigmoid, rd=[bres[b], self.rvec], wr=[rf['a']], bias=self.vc(l, 'a0', j))
            b = self.nb()
            self.mm(B_[b][:, :N], low[64:128, cs_], lo_b[64:128], rd=[rsw, rlo], wr=[bres[b]])
            self.cp('act', gT[:, fs], B_[b][:, :N], rd=[bres[b]], wr=[rg])
            self.act(tb16['sqk'], k_j, AF.Square, rd=[rk_, self.rvec], wr=[rb16['sqk']], scale=self.vc(l, 'kk', j))
            b = self.nb()
            self.mm(B_[b][:, :N], self.bones_b, tb16['sqk'], rd=[rc, rb16['sqk']], wr=[bres[b]])
            self.act(tf['nrm'], B_[b][:, :N], AF.Sqrt, rd=[bres[b]], wr=[rf['nrm']])
            self.ts('dve', tf['nrm'], tf['nrm'], 1e-12, None, ALU.max, rd=[rf['nrm']], wr=[rf['nrm']])
            self.recip(tf['nrm'], tf['nrm'], rd=[rf['nrm']], wr=[rf['nrm']])
            self.stt('dve', tf['kkn'], k_j, self.vc(l, 'kk', j), tf['nrm'], ALU.mult, ALU.mult, rd=[rk_, self.rvec, rf['nrm']], wr=[rf['kkn']])
            self.ts('dve', tf['t1'], tf['a'], self.vc(l, 'ka', j), P['omka'][:, j:j + 1], ALU.mult, ALU.add,
                    rd=[rf['a'], self.rvec, R['omka']], wr=[rf['t1']])
            self.tt('pool', tf['kp'], tf['t1'], k_j, ALU.mult, rd=[rf['t1'], rk_], wr=[rf['kp']])
            if self.dbg.get('stop', 99) <= 4:
                continue
            self.stt('dve', at_b[:, fs], tf['kkn'], -1.0, tf['Pexc'], ALU.mult, ALU.mult, rd=[rf['kkn'], rf['Pexc']], wr=[rat])
            self.tt('pool', tf['bb'], tf['kkn'], tf['a'], ALU.mult, rd=[rf['kkn'], rf['a']], wr=[rf['bb']])
            self.tt('dve', bt_b[:, fs], tf['bb'], tf['Pinv'], ALU.mult, rd=[rf['bb'], rf['Pinv']], wr=[rbt])
            self.tt('pool', tb16['bhT'], tf['bb'], tf['Pend'], ALU.mult, rd=[rf['bb'], rf['Pend']], wr=[rb16['bhT']])
            self.tt('dve', kt_b[:, fs], tf['kp'], tf['Pinv'], ALU.mult, rd=[rf['kp'], rf['Pinv']], wr=[rkt])
            self.tt('pool', tb16['khT'], tf['kp'], tf['Pend'], ALU.mult, rd=[rf['kp'], rf['Pend']], wr=[rb16['khT']])
            self.tt('dve', rt_b[:, fs], r_j, tf['Pinc'], ALU.mult, rd=[rr_, rf['Pinc']], wr=[rrt])
            if l == 0:
                self.dma('act', dr['vf'][:, j, t0:t0 + N], v_j, rd=[rv_], wr=[self.vfres[mb]])
            else:
                b = self.nb()
                self.mm(B_[b][:, :N], P['v2'][0:16, cs_], mv_b[0:16], rd=[rsw, rmv], wr=[bres[b]])
                self.act(tf['sgv'], B_[b][:, :N], AF.Sigmoid, rd=[bres[b], self.rvec], wr=[rf['sgv']], bias=self.vc(l, 'v0', j))
                self.tt('pool', tf['t1'], vfb[:, fs], v_j, ALU.subtract, rd=[rvfb, rv_, rf['t1']], wr=[rf['t1']])
                self.tt('dve', tf['t1'], tf['t1'], tf['sgv'], ALU.mult, rd=[rf['t1'], rf['sgv']], wr=[rf['t1']])
                self.tt('pool', v_j, v_j, tf['t1'], ALU.add, rd=[rv_, rf['t1']], wr=[rv_])
            self.cp('pool', tb16['vb'], v_j, rd=[rv_], wr=[rb16['vb']])
            self.stt('dve', tb16['rk'], r_j, self.vc(l, 'rk', j), tf['kp'], ALU.mult, ALU.mult, rd=[rr_, self.rvec, rf['kp']], wr=[rb16['rk']])
            b = self.nb()
            self.mm(B_[b][:, :N], self.bones_b, tb16['rk'], rd=[rc, rb16['rk']], wr=[bres[b]])
            self.tt('dve', bonus[:, fs], B_[b][:, :N], v_j, ALU.mult, rd=[bres[b], rv_], wr=[rbo])
            if self.dbg.get('stop', 99) <= 5:
                continue
            for q in range(NQ):
                tq = slice(q * 128, (q + 1) * 128)
                b = self.nb()
                for x, nm in enumerate(('bhT', 'khT', 'vb')):
                    self.mm(B_[b][:, x * 128:(x + 1) * 128], tb16[nm][:, tq], self.ident_b, rd=[rb16[nm], rc], wr=[bres[b]])
                self.cp('act', tok[q].rearrange("p (x f) -> p x f", x=3)[:, :, cs_],
                        B_[b][:, 0:384].rearrange("p (x f) -> p x f", x=3), rd=[bres[b]], wr=[rtok[q]])
        if self.dbg.get('stop', 99) <= 6:
            return
        y_sb = ar.f32(3 * N)
        ry = Res()
        kinds = [('N', at_b, rat, bt_b, rbt, 'maskL'), ('Nt', bt_b, rbt, at_b, rat, 'maskU'), ('Aak', kt_b, rkt, at_b, rat, 'maskU'),
                 ('Arb', bt_b, rbt, rt_b, rrt, 'maskUi'), ('Ark', kt_b, rkt, rt_b, rrt, 'maskUi')]
        Am = {kd[0]: ar.bf16(768) for kd in kinds}
        rAm = {kd[0]: Res() for kd in kinds}
        Mx = [ar.bf16(768) for _ in range(2)]
        Mtx = [ar.bf16(768) for _ in range(2)]
        Qx = [ar.bf16(768) for _ in range(2)]
        rMx = [Res(), Res()]
        rMtx = [Res(), Res()]
        rQx = [Res(), Res()]
        W_sb = ar.bf16(384)
        U_sb = ar.bf16(384)
        rW, rU = Res(), Res()
        Sf, Sb = P['Sf'], P['Sb']
        rSf, rSb = R['Sf'], R['Sb']
        ident6 = self.cb[:, CB['ident']:CB['ident'] + 768]
        for q in range(NQ):
            tq = slice(q * 128, (q + 1) * 128)
            tokq = tok[q]
            for (nm, Lt, rL, Rt, rR, mk) in kinds:
                for e in range(2):
                    pr = slice(e * 64, (e + 1) * 64)
                    b = self.nb()
                    for j in range(3):
                        cols = slice(j * N + q * 128, j * N + (q + 1) * 128)
                        self.mm(B_[b][:, j * 128:(j + 1) * 128], Lt[pr, cols], Rt[pr, cols], rd=[rL, rR], wr=[bres[b]])
                    self.tt('dve', Am[nm].rearrange("p (j e t) -> p j e t", j=3, e=2)[:, :, e, :],
                            B_[b][:, 0:384].rearrange("p (j t) -> p j t", j=3),
                            cf[:, CF[mk]:CF[mk] + 384].rearrange("p (j t) -> p j t", j=3), ALU.mult,
                            rd=[bres[b], rc], wr=[rAm[nm]])
            if self.dbg.get('stop', 99) <= 7:
                continue
            Mc, Mtc, rMc, rMtc = Am['N'], Am['Nt'], rAm['N'], rAm['Nt']
            qi = 0
            self.tt('pool', Qx[qi], Am['Nt'], ident6, ALU.add, rd=[rAm['Nt'], rc], wr=[rQx[qi]])
            for lev in range(1, 7):
                mi = lev % 2
                for half in range(2):
                    hs = slice(half * 384, (half + 1) * 384)
                    b = self.nb()
                    for hh in range(3):
                        c_ = slice((half * 3 + hh) * 128, (half * 3 + hh + 1) * 128)
                        self.mm(B_[b][:, hh * 128:(hh + 1) * 128], Mtc[:, c_], Mc[:, c_], rd=[rMc, rMtc], wr=[bres[b]])
                    self.cp('act', Mx[mi][:, hs], B_[b][:, 0:384], rd=[bres[b]], wr=[rMx[mi]])
                    if lev < 6:
                        b = self.nb()
                        for hh in range(3):
                            c_ = slice((half * 3 + hh) * 128, (half * 3 + hh + 1) * 128)
                            self.mm(B_[b][:, hh * 128:(hh + 1) * 128], Mc[:, c_], Mtc[:, c_], rd=[rMc, rMtc], wr=[bres[b]])
                        self.cp('act', Mtx[mi][:, hs], B_[b][:, 0:384], rd=[bres[b]], wr=[rMtx[mi]])
                Mc, rMc = Mx[mi], rMx[mi]
                if lev < 6:
                    Mtc, rMtc = Mtx[mi], rMtx[mi]
                qn = 1 - qi
                for half in range(2):
                    hs = slice(half * 384, (half + 1) * 384)
                    b = self.nb()
                    for hh in range(3):
                        c_ = slice((half * 3 + hh) * 128, (half * 3 + hh + 1) * 128)
                        o_ = B_[b][:, hh * 128:(hh + 1) * 128]
                        self.mm(o_, self.ident_b, Qx[qi][:, c_], start=True, stop=False, rd=[rc, rQx[qi]], wr=[bres[b]])
                        self.mm(o_, Mc[:, c_], Qx[qi][:, c_], start=False, stop=True, rd=[rMc, rQx[qi]], wr=[bres[b]])
                    self.cp('dve', Qx[qn][:, hs], B_[b][:, 0:384], rd=[bres[b]], wr=[rQx[qn]])
                qi = qn
            Tt, rTt = Qx[qi], rQx[qi]
            if self.dbg.get('stop', 99) <= 8:
                continue
            b = self.nb()
            for j in range(3):
                cols = slice(j * N + q * 128, j * N + (q + 1) * 128)
                self.mm(B_[b][:, j * 128:(j + 1) * 128], at_b[:, cols], Sb[:, j * 128:(j + 1) * 128], start=True, stop=False,
                        rd=[rat, rSb], wr=[bres[b]])
                for e in range(2):
                    h = 2 * j + e
                    self.mm(B_[b][:, h * 64:(h + 1) * 64], Am['Aak'][:, h * 128:(h + 1) * 128], tokq[:, 768 + h * 64:768 + (h + 1) * 64],
                            start=False, stop=(e == 1), rd=[rAm['Aak'], rtok[q]], wr=[bres[b]])
            self.cp('act', W_sb, B_[b][:, 0:384], rd=[bres[b]], wr=[rW])
            b = self.nb()
            for h in range(6):
                self.mm(B_[b][:, h * 64:(h + 1) * 64], Tt[:, h * 128:(h + 1) * 128], W_sb[:, h * 64:(h + 1) * 64], rd=[rTt, rW], wr=[bres[b]])
            self.cp('dve', U_sb, B_[b][:, 0:384], rd=[bres[b]], wr=[rU])
            if self.dbg.get('stop', 99) <= 9:
                continue
            b = self.nb()
            for j in range(3):
                cols = slice(j * N + q * 128, j * N + (q + 1) * 128)
                self.mm(B_[b][:, j * 128:(j + 1) * 128], Sb[:, j * 128:(j + 1) * 128], rt_b[:, cols], start=True, stop=False,
                        rd=[rSb, rrt], wr=[bres[b]])
                for e in range(2):
                    h = 2 * j + e
                    pr = slice(e * 64, (e + 1) * 64)
                    o_ = B_[b][pr, j * 128:(j + 1) * 128]
                    self.mm(o_, U_sb[:, h * 64:(h + 1) * 64], Am['Arb'][:, h * 128:(h + 1) * 128], start=False, stop=False,
                            rd=[rU, rAm['Arb']], wr=[bres[b]])
                    self.mm(o_, tokq[:, 768 + h * 64:768 + (h + 1) * 64], Am['Ark'][:, h * 128:(h + 1) * 128], start=False, stop=True,
                            rd=[rtok[q], rAm['Ark']], wr=[bres[b]])
            self.cp('act', y_sb.rearrange("p (j t) -> p j t", j=3)[:, :, tq], B_[b][:, 0:384].rearrange("p (j t) -> p j t", j=3),
                    rd=[bres[b]], wr=[ry])
            if self.dbg.get('stop', 99) <= 10:
                continue
            b = self.nb()
            for h in range(6):
                j, e = h // 2, h % 2
                pr = slice(e * 64, (e + 1) * 64)
                o_ = B_[b][pr, j * 64:(j + 1) * 64]
                self.mm(o_, tokq[:, 0 + h * 64:0 + (h + 1) * 64], U_sb[:, h * 64:(h + 1) * 64], start=True, stop=False,
                        rd=[rtok[q], rU], wr=[bres[b]])
                self.mm(o_, tokq[:, 384 + h * 64:384 + (h + 1) * 64], tokq[:, 768 + h * 64:768 + (h + 1) * 64], start=False, stop=True,
                        rd=[rtok[q]], wr=[bres[b]])
            for j in range(3):
                for e in range(2):
                    pr = slice(e * 64, (e + 1) * 64)
                    sc = slice(j * 128 + e * 64, j * 128 + (e + 1) * 64)
                    self.stt('dve', Sf[pr, sc], Sf[pr, sc], Ptot[pr, j * NQ + q:j * NQ + q + 1],
                             B_[b][pr, j * 64:(j + 1) * 64], ALU.mult, ALU.add, rd=[rSf, rPt, bres[b]], wr=[rSf])
            self.cp('dve', Sb, Sf, rd=[rSf], wr=[rSb])
        if self.dbg.get('stop', 99) <= 11:
            return
        yb = ar.bf16(N)
        ryb = Res()
        yc = ar.f32(N)
        ryc = Res()
        sd = ar.f32(N)
        rsd = Res()
        for j in range(3):
            fs = slice(j * N, (j + 1) * N)
            yj = y_sb[:, fs]
            self.cp('act', yb, yj, rd=[ry], wr=[ryb])
            b = self.nb()
            self.mm(B_[b][:, :N], self.bmean_b, yb, rd=[rc, ryb], wr=[bres[b]])
            self.tt('dve', yc, yj, B_[b][:, :N], ALU.subtract, rd=[ry, bres[b]], wr=[ryc])
            self.act(yb, yc, AF.Square, rd=[ryc], wr=[ryb])
            b = self.nb()
            self.mm(B_[b][:, :N], self.bmean_b, yb, rd=[rc, ryb], wr=[bres[b]])
            self.act(sd, B_[b][:, :N], AF.Sqrt, rd=[bres[b]], wr=[rsd], bias=GN_EPS)
            self.recip(sd, sd, rd=[rsd], wr=[rsd])
            self.tt('dve', yc, yc, sd, ALU.mult, rd=[ryc, rsd], wr=[ryc])
            self.ts('dve', yc, yc, self.vc(l, 'lnw', j), self.vc(l, 'lnb', j), ALU.mult, ALU.add, rd=[ryc, self.rvec], wr=[ryc])
            self.tt('pool', yc, yc, bonus[:, fs], ALU.add, rd=[ryc, rbo], wr=[ryc])
            self.tt('dve', oT[:, j * N:(j + 1) * N], yc, gT[:, fs], ALU.mult, rd=[ryc, rg], wr=[ores[j]])

    def ret_block(self, l, mb, hT, hres, oT, ores):
        ar, P, R, dr = self.ar, self.P, self.R, self.dr
        t0 = mb * NM
        N = NM
        B_ = self.banks
        bres = self.bres
        cf = self.cf
        rc = self.rconst
        z = {}
        rz = {}
        for nm in ('cq', 'ck', 'cv', 'cg'):
            z[nm] = ar.f32(2 * N)
            rz[nm] = Res()
            for j in range(2):
                b = self.proj(l, nm + str(j), 128, hT, hres)
                self.cp('act', z[nm][:, j * N:(j + 1) * N], B_[b][:, :N], rd=[bres[b]], wr=[rz[nm]])
        rot = ar.f32(4 * N)
        rrot = Res()
        self.dma('act', rot.rearrange("p (c t) -> p c t", c=4), dr['rot'][:, :, t0:t0 + N], wr=[rrot])
        qr_b = ar.bf16(2 * N)
        qd_b = ar.bf16(2 * N)
        kr_b = ar.bf16(2 * N)
        kdT = ar.bf16(2 * N)
        cvb = ar.bf16(2 * N)
        rqr, rqd, rkr, rkd, rcvb = [Res() for _ in range(5)]
        t1 = ar.f32(N)
        t2 = ar.f32(N)
        zr = ar.f32(N)
        rt1, rt2, rzr = Res(), Res(), Res()
        prot = cf[:, CF['prot']:CF['prot'] + 128]
        for (nm, ci, si) in (('cq', 0, 1), ('ck', 2, 3)):
            for j in range(2):
                fs = slice(j * N, (j + 1) * N)
                zj = z[nm][:, fs]
                b = self.nb()
                self.mm(B_[b][:, :N], prot, zj, rd=[rc, rz[nm]], wr=[bres[b]])
                self.tt('pool', t1, zj, rot[:, ci * N:(ci + 1) * N], ALU.mult, rd=[rz[nm], rrot], wr=[rt1])
                self.tt('dve', t2, B_[b][:, :N], rot[:, si * N:(si + 1) * N], ALU.mult, rd=[bres[b], rrot], wr=[rt2])
                self.tt('dve', zr, t1, t2, ALU.add, rd=[rt1, rt2], wr=[rzr])
                if nm == 'cq':
                    self.cp('act', qr_b[:, fs], zr, rd=[rzr], wr=[rqr])
                    self.tt('pool', qd_b[:, fs], zr, cf[:, CF['qdec'] + j * N:CF['qdec'] + (j + 1) * N], ALU.mult, rd=[rzr, rc], wr=[rqd])
                else:
                    self.cp('act', kr_b[:, fs], zr, rd=[rzr], wr=[rkr])
                    self.tt('pool', kdT[:, fs], zr, cf[:, CF['kdec'] + j * N:CF['kdec'] + (j + 1) * N], ALU.mult, rd=[rzr, rc], wr=[rkd])
        self.cp('pool', cvb, z['cv'], rd=[rz['cv']], wr=[rcvb])
        tokr = [ar.bf16(512) for _ in range(NQ)]
        rtokr = [Res() for _ in range(NQ)]
        for q in range(NQ):
            b = self.nb()
            for x, (src, rs) in enumerate(((kdT, rkd), (cvb, rcvb))):
                for j in range(2):
                    self.mm(B_[b][:, (x * 2 + j) * 128:(x * 2 + j + 1) * 128], src[:, j * N + q * 128:j * N + (q + 1) * 128], self.ident_b,
                            rd=[rs, rc], wr=[bres[b]])
            self.cp('act', tokr[q], B_[b][:, 0:512], rd=[bres[b]], wr=[rtokr[q]])
        yret = ar.f32(2 * N)
        ryr = Res()
        sm = ar.bf16(512)
        rsm = Res()
        Sf, Sb = P['rSf'], P['rSb']
        rSf, rSb = R['rSf'], R['rSb']
        for q in range(NQ):
            tq = slice(q * 128, (q + 1) * 128)
            for e in range(2):
                pr = slice(e * 64, (e + 1) * 64)
                b = self.nb()
                for j in range(2):
                    cols = slice(j * N + q * 128, j * N + (q + 1) * 128)
                    self.mm(B_[b][:, j * 128:(j + 1) * 128], kr_b[pr, cols], qr_b[pr, cols], rd=[rkr, rqr], wr=[bres[b]])
                self.tt('dve', sm.rearrange("p (j e t) -> p j e t", j=2, e=2)[:, :, e, :],
                        B_[b][:, 0:256].rearrange("p (j t) -> p j t", j=2),
                        cf[:, CF['intraT']:CF['intraT'] + 512].rearrange("p (j e t) -> p j e t", j=2, e=2)[:, :, e, :], ALU.mult,
                        rd=[bres[b], rc], wr=[rsm])
            b = self.nb()
            for j in range(2):
                cols = slice(j * N + q * 128, j * N + (q + 1) * 128)
                self.mm(B_[b][:, j * 128:(j + 1) * 128], Sb[:, j * 128:(j + 1) * 128], qd_b[:, cols], start=True, stop=False,
                        rd=[rSb, rqd], wr=[bres[b]])
                for e in range(2):
                    h = 2 * j + e
                    pr = slice(e * 64, (e + 1) * 64)
                    self.mm(B_[b][pr, j * 128:(j + 1) * 128], tokr[q][:, 256 + h * 64:256 + (h + 1) * 64], sm[:, h * 128:(h + 1) * 128],
                            start=False, stop=True, rd=[rtokr[q], rsm], wr=[bres[b]])
            self.cp('act', yret.rearrange("p (j t) -> p j t", j=2)[:, :, tq], B_[b][:, 0:256].rearrange("p (j t) -> p j t", j=2),
                    rd=[bres[b]], wr=[ryr])
            b = self.nb()
            for h in range(4):
                j, e = h // 2, h % 2
                pr = slice(e * 64, (e + 1) * 64)
                self.mm(B_[b][pr, j * 64:(j + 1) * 64], tokr[q][:, h * 64:(h + 1) * 64], tokr[q][:, 256 + h * 64:256 + (h + 1) * 64],
                        rd=[rtokr[q]], wr=[bres[b]])
            for j in range(2):
                for e in range(2):
                    pr = slice(e * 64, (e + 1) * 64)
                    sc = slice(j * 128 + e * 64, j * 128 + (e + 1) * 64)
                    self.stt('dve', Sf[pr, sc], Sf[pr, sc], cf[pr, CF['cdec'] + j:CF['cdec'] + j + 1],
                             B_[b][pr, j * 64:(j + 1) * 64], ALU.mult, ALU.add, rd=[rSf, rc, bres[b]], wr=[rSf])
            self.cp('dve', Sb, Sf, rd=[rSf], wr=[rSb])
        sqb = ar.bf16(N)
        rsq = Res()
        sd = ar.f32(N)
        rsd = Res()
        sg = ar.f32(N)
        rsg = Res()
        for j in range(2):
            fs = slice(j * N, (j + 1) * N)
            self.act(sqb, yret[:, fs], AF.Square, rd=[ryr], wr=[rsq])
            b = self.nb()
            self.mm(B_[b][:, :N], self.bmean_b, sqb, rd=[rc, rsq], wr=[bres[b]])
            self.act(sd, B_[b][:, :N], AF.Sqrt, rd=[bres[b]], wr=[rsd], bias=EPS)
            self.recip(sd, sd, rd=[rsd], wr=[rsd])
            self.tt('dve', sd, sd, yret[:, fs], ALU.mult, rd=[rsd, ryr], wr=[rsd])
            self.act(sg, z['cg'][:, fs], AF.Silu, rd=[rz['cg']], wr=[rsg])
            self.tt('pool', oT[:, (6 + j) * N:(7 + j) * N], sd, sg, ALU.mult, rd=[rsd, rsg], wr=[ores[6 + j]])

    def dsa_block(self, l, mb, hT, hres, oT, ores):
        ar, P, R, dr = self.ar, self.P, self.R, self.dr
        t0 = mb * NM
        N = NM
        B_ = self.banks
        bres = self.bres
        cf = self.cf
        rc = self.rconst
        cT, ctok, ik2 = P['cT'], P['ctok'], P['ik2']
        rcT, rctok, rik2 = R['cT'], R['ctok'], R['ik2']
        self.rr = [0, 1, 2, 3]
        qT_b = ar.bf16(3 * N)
        rq = Res()
        for j in range(3):
            b = self.proj(l, 'bq%d' % j, 128, hT, hres)
            self.cp('act', qT_b[:, j * N:(j + 1) * N], B_[b][:, :N], rd=[bres[b]], wr=[rq])
        ckv = ar.f32(N)
        rckv = Res()
        b = self.proj(l, 'bc', 128, hT, hres)
        self.cp('act', ckv, B_[b][:, :N], rd=[bres[b]], wr=[rckv])
        sqb = ar.bf16(N)
        rsq = Res()
        sd = ar.f32(N)
        rsd = Res()
        self.act(sqb, ckv, AF.Square, rd=[rckv], wr=[rsq])
        b = self.nb()
        self.mm(B_[b][:, :N], self.ones_b, sqb, rd=[rc, rsq], wr=[bres[b]])
        self.act(sd, B_[b][:, :N], AF.Sqrt, rd=[bres[b]], wr=[rsd], bias=EPS, scale=1.0 / 128)
        self.recip(sd, sd, rd=[rsd], wr=[rsd])
        self.stt('dve', cT[:, t0:t0 + N], ckv, self.vc(l, 'kvn'), sd, ALU.mult, ALU.mult, rd=[rckv, self.rvec, rsd], wr=[rcT])
        for q in range(NQ):
            gq = t0 // 128 + q
            b = self.nb()
            self.mm(B_[b][:, :128], cT[:, gq * 128:(gq + 1) * 128], self.ident_b, rd=[rcT, rc], wr=[bres[b]])
            self.cp('act', ctok[:, gq * 128:(gq + 1) * 128], B_[b][:, :128], rd=[bres[b]], wr=[rctok])
        iq_b = ar.bf16(4 * N)
        riq = Res()
        for j in range(4):
            b = self.proj(l, 'biq%d' % j, 128, hT, hres)
            self.cp('act', iq_b[:, j * N:(j + 1) * N], B_[b][:, :N], rd=[bres[b]], wr=[riq])
        b = self.proj(l, 'bik2', 128, hT, hres)
        self.cp('act', ik2[:, t0:t0 + N], B_[b][:, :N], rd=[bres[b]], wr=[rik2])
        iw_f = ar.f32(N)
        riw = Res()
        b = self.proj(l, 'biw', 8, hT, hres)
        self.cp('act', iw_f[:8], B_[b][:8, :N], rd=[bres[b]], wr=[riw])
        iwbc = ar.f32(8 * N)
        riwbc = Res()
        SC = (8.0 ** -0.5) * (64.0 ** -0.5)
        for h8 in range(8):
            b = self.nb()
            self.mm(B_[b][:, :N], cf[0:8, CF['sel'] + h8 * 128:CF['sel'] + (h8 + 1) * 128], iw_f[:8], rd=[rc, riw], wr=[bres[b]])
            self.act(iwbc[:, h8 * N:(h8 + 1) * N], B_[b][:, :N], AF.Copy, rd=[bres[b]], wr=[riwbc], scale=SC)
        qlT = ar.bf16(NQ * 768)
        rql = Res()
        for h in range(6):
            j, e = h // 2, h % 2
            pr = slice(e * 64, (e + 1) * 64)
            b = self.nb()
            self.mm(B_[b][:, :N], P['wuk'][pr, j * 128:(j + 1) * 128], qT_b[pr, j * N:(j + 1) * N], rd=[R['smw'], rq], wr=[bres[b]])
            for q in range(NQ):
                self.act(qlT[:, (q * 6 + h) * 128:(q * 6 + h + 1) * 128], B_[b][:, q * 128:(q + 1) * 128], AF.Copy,
                         rd=[bres[b]], wr=[rql], scale=0.125)
        score = [ar.f32(T) for _ in range(NQ)]
        rsc = [Res() for _ in range(NQ)]
        junk = ar.bf16(T)
        rjk = Res()
        bsq = [ar.f32(8) for _ in range(NQ)]
        rbsq = [Res() for _ in range(NQ)]
        negmq = [ar.bf16(T) for _ in range(NQ)]
        rngq = [Res() for _ in range(NQ)]
        tmp = [ar.bf16(1024) for _ in range(2)]
        rtmp = [Res(), Res()]
        ex = [ar.bf16(768) for _ in range(2)]
        rex = [Res(), Res()]
        rden = ar.f32(768)
        rrd = Res()
        oln = ar.bf16(768)
        roln = Res()
        ident4 = self.cb[:, CB['ident']:CB['ident'] + 512]
        iq3 = iq_b.rearrange("p (j t) -> p j t", j=4)

        def idx(q, bg=None):
            gq = t0 // 128 + q
            nS = gq + 1
            tq = slice(q * 128, (q + 1) * 128)
            self.rr = [0, 1, 2, 3]
            sb = None
            for si in range(nS):
                zb = [self.nb(), self.nb()]
                for e in range(2):
                    pr = slice(e * 64, (e + 1) * 64)
                    self.mm(B_[zb[e]][:, 0:512], ik2[pr, si * 128:(si + 1) * 128], iq3[pr, :, tq], rd=[rik2, riq], wr=[bres[zb[e]]])
                ts_ = si % 2
                for half in range(2):
                    self.stt('dve', tmp[ts_].rearrange("p (j e t) -> p j e t", j=4, e=2)[:, :, half, :],
                             B_[zb[half]][:, 0:512].rearrange("p (h t) -> p h t", h=4), 0.0,
                             iwbc.rearrange("p (j e t) -> p j e t", j=4, e=2)[:, :, half, tq], ALU.max, ALU.mult,
                             rd=[bres[zb[half]], riwbc], wr=[rtmp[ts_]])
                if bg is not None:
                    next(bg, None)
                if si % 4 == 0:
                    sb = 4 + (si // 4) % 2
                for h8 in range(8):
                    self.mm(B_[sb][:, (si % 4) * 128:(si % 4 + 1) * 128], tmp[ts_][:, h8 * 128:(h8 + 1) * 128], self.ident_b,
                            start=(h8 == 0), stop=(h8 == 7), rd=[rtmp[ts_], rc], wr=[bres[sb]])
                if si % 4 == 3 or si == nS - 1:
                    c0 = (si // 4) * 512
                    nc_ = (si % 4 + 1) * 128
                    self.cp('act', score[q][:, c0:c0 + nc_], B_[sb][:, 0:nc_], rd=[bres[sb]], wr=[rsc[q]])
            if gq >= 2 and not self.dbg.get('notopk'):
                ncols_ = nS * 128
                bs_, rbs_ = bsq[q], rbsq[q]
                self.k.op('dve', lambda e, o=bs_[:, 1:2], i=score[q][:, :ncols_]: e.tensor_reduce(out=o, in_=i, axis=AX.X, op=ALU.max),
                          [rsc[q]], [rbs_])
                self.k.op('dve', lambda e, o=bs_[:, 0:1], i=score[q][:, :ncols_]: e.tensor_reduce(out=o, in_=i, axis=AX.X, op=ALU.min),
                          [rsc[q]], [rbs_])
            self.tt('pool', score[q][:, gq * 128:(gq + 1) * 128], score[q][:, gq * 128:(gq + 1) * 128],
                    cf[:, CF['cmask']:CF['cmask'] + 128], ALU.add, rd=[rsc[q], rc], wr=[rsc[q]])

        def topk(q):
            gq = t0 // 128 + q
            ncols = (gq + 1) * 128
            sc_ = score[q][:, :ncols]
            bs, rbs = bsq[q], rbsq[q]
            negm, rng = negmq[q], rngq[q]
            if gq >= 2 and not self.dbg.get('notopk'):
                self.ts('dve', bs[:, 0:1], bs[:, 0:1], -1.0, None, ALU.add, rd=[rbs], wr=[rbs])
                self.ts('dve', bs[:, 1:2], bs[:, 1:2], 1.0, None, ALU.add, rd=[rbs], wr=[rbs])
                self.tt('dve', bs[:, 1:2], bs[:, 1:2], bs[:, 0:1], ALU.subtract, rd=[rbs], wr=[rbs])
                for it in range(NIT_TOPK):
                    c_ = 2.0 ** -(it + 1)
                    self.stt('dve', bs[:, 2:3], bs[:, 1:2], c_, bs[:, 0:1], ALU.mult, ALU.add, rd=[rbs], wr=[rbs])
                    self.k.op('dve', lambda e, o=junk[:, :ncols], i=sc_, m_=bs[:, 2:3], a_=bs[:, 3:4]: e.tensor_scalar(
                        out=o, in0=i, scalar1=m_, scalar2=0.0, op0=ALU.is_ge, op1=ALU.add, accum_out=a_), [rsc[q], rbs, rjk], [rjk, rbs])
                    self.ts('dve', bs[:, 4:5], bs[:, 3:4], 255.5, c_, ALU.is_ge, ALU.mult, rd=[rbs], wr=[rbs])
                    self.stt('dve', bs[:, 0:1], bs[:, 4:5], bs[:, 1:2], bs[:, 0:1], ALU.mult, ALU.add, rd=[rbs], wr=[rbs])
                    yield
                self.ts('dve', negm[:, :ncols], sc_, bs[:, 0:1], -30000.0, ALU.is_lt, ALU.mult, rd=[rsc[q], rbs], wr=[rng])
            else:
                self.ts('dve', negm[:, :ncols], sc_, -1e29, -30000.0, ALU.is_lt, ALU.mult, rd=[rsc[q]], wr=[rng])
            yield

        def attm(q):
            gq = t0 // 128 + q
            nS = gq + 1
            negm, rng = negmq[q], rngq[q]
            self.rr = [0, 1, 2, 3]
            for si in range(nS):
                s_ = slice(si * 128, (si + 1) * 128)
                la, lb = self.nb(), self.nb()
                xs = si % 2
                self.mm(B_[la][:, 0:512], cT[:, s_], qlT[:, q * 768:q * 768 + 512], start=True, stop=False, rd=[rcT, rql], wr=[bres[la]])
                self.mm(B_[la][:, 0:512], negm[:, s_], ident4, start=False, stop=True, rd=[rng, rc], wr=[bres[la]])
                self.mm(B_[lb][:, 0:256], cT[:, s_], qlT[:, q * 768 + 512:q * 768 + 768], start=True, stop=False, rd=[rcT, rql], wr=[bres[lb]])
                self.mm(B_[lb][:, 0:256], negm[:, s_], ident4[:, 0:256], start=False, stop=True, rd=[rng, rc], wr=[bres[lb]])
                self.act(ex[xs][:, 0:512], B_[la][:, 0:512], AF.Exp, rd=[bres[la]], wr=[rex[xs]])
                self.act(ex[xs][:, 512:768], B_[lb][:, 0:256], AF.Exp, rd=[bres[lb]], wr=[rex[xs]])
                st_, sp_ = (si == 0), (si == nS - 1)
                self.mm(B_[4][:, 0:512], ctok[:, s_], ex[xs][:, 0:512], start=st_, stop=sp_, rd=[rctok, rex[xs]], wr=[bres[4]])
                self.mm(B_[5][:, 0:256], ctok[:, s_], ex[xs][:, 512:768], start=st_, stop=sp_, rd=[rctok, rex[xs]], wr=[bres[5]])
                self.mm(B_[6][:, 0:512], self.ones_b, ex[xs][:, 0:512], start=st_, stop=sp_, rd=[rc, rex[xs]], wr=[bres[6]])
                self.mm(B_[7][:, 0:256], self.ones_b, ex[xs][:, 512:768], start=st_, stop=sp_, rd=[rc, rex[xs]], wr=[bres[7]])

        def attf(q):
            self.rr = [0, 1, 2, 3]
            self.recip(rden[:, 0:512], B_[6][:, 0:512], rd=[bres[6]], wr=[rrd])
            self.recip(rden[:, 512:768], B_[7][:, 0:256], rd=[bres[7]], wr=[rrd])
            self.tt('dve', oln[:, 0:512], B_[4][:, 0:512], rden[:, 0:512], ALU.mult, rd=[bres[4], rrd], wr=[roln])
            self.tt('dve', oln[:, 512:768], B_[5][:, 0:256], rden[:, 512:768], ALU.mult, rd=[bres[5], rrd], wr=[roln])
            b = self.nb()
            for h in range(6):
                j, e = h // 2, h % 2
                pr = slice(e * 64, (e + 1) * 64)
                self.mm(B_[b][pr, j * 128:(j + 1) * 128], P['wuv'][:, h * 64:(h + 1) * 64], oln[:, h * 128:(h + 1) * 128],
                        rd=[R['smw'], roln], wr=[bres[b]])
            for j in range(3):
                self.cp('act', oT[:, (3 + j) * N + q * 128:(3 + j) * N + (q + 1) * 128], B_[b][:, j * 128:(j + 1) * 128],
                        rd=[bres[b]], wr=[ores[3 + j]])

        def drain(g):
            for _ in g:
                pass

        idx(0)
        g0 = topk(0)
        idx(1, bg=g0)
        drain(g0)
        attm(0)
        drain(topk(1))
        attf(0)
        attm(1)
        attf(1)
        self.rr = list(range(8))


def _prep_ffn(wg, wu, wd):
    def gu(w):
        wp = np.zeros((D, DFFP), np.float32)
        wp[:, :DFF] = w
        return np.ascontiguousarray(wp.reshape(8, 128, NFC, 128).transpose(2, 1, 0, 3)).reshape(NFC, 128, 8 * 128)
    wdp = np.zeros((DFFP, D), np.float32)
    wdp[:DFF] = wd
    wdt = np.ascontiguousarray(wdp.reshape(NFC, 128, 8, 128).transpose(2, 1, 0, 3)).reshape(8, 128, NFC * 128)
    return np.stack([gu(wg), gu(wu)]), wdt


def _col(v, n):
    return np.ascontiguousarray(np.asarray(v, np.float32).reshape(n, 128).T)


def _consts():
    cf = np.zeros((128, CFN), np.float32)
    cb = np.zeros((128, CBN), np.float32)
    r = np.arange(128)[:, None]
    c = np.arange(128)[None, :]
    cf[:, CF['maskL']:CF['maskL'] + 384] = np.tile((c < r).astype(np.float32), (1, 3))
    cf[:, CF['maskU']:CF['maskU'] + 384] = np.tile((r < c).astype(np.float32), (1, 3))
    cf[:, CF['maskUi']:CF['maskUi'] + 384] = np.tile((r <= c).astype(np.float32), (1, 3))
    gam = 1.0 - 2.0 ** (-5.0 - np.arange(4, dtype=np.float64))
    lg = np.log(gam)
    for h in range(4):
        dm = (c - r).astype(np.float64)
        cf[:, CF['intraT'] + h * 128:CF['intraT'] + (h + 1) * 128] = np.where(dm >= 0, np.exp(np.maximum(dm, 0) * lg[h]), 0.0)
    p = np.arange(128)
    partner = np.where((p % 64) < 32, p + 32, p - 32)
    prot = np.zeros((128, 128), np.float32)
    prot[partner, p] = 1.0
    cf[:, CF['prot']:CF['prot'] + 128] = prot
    rs = np.ones((128, NM), np.float32)
    rs[:, 0::128] = 0.0
    cf[:, CF['reset']:CF['reset'] + NM] = rs
    cf[:, CF['cmask']:CF['cmask'] + 128] = np.where(c <= r, 0.0, -1e30)
    sel = np.zeros((128, 1024), np.float32)
    for h in range(8):
        sel[h, h * 128:(h + 1) * 128] = 1.0
    cf[:, CF['sel']:CF['sel'] + 1024] = sel
    n = (np.arange(NM) % 128).astype(np.float64)
    for j in range(2):
        hh = 2 * j + (p // 64)
        cf[:, CF['qdec'] + j * NM:CF['qdec'] + (j + 1) * NM] = np.exp((n[None, :] + 1.0) * lg[hh][:, None])
        cf[:, CF['kdec'] + j * NM:CF['kdec'] + (j + 1) * NM] = np.exp((127.0 - n[None, :]) * lg[hh][:, None])
        cf[:, CF['cdec'] + j] = np.exp(128.0 * lg[hh])
    cf[:, CF['negbig']] = -1e29
    cb[:, CB['ident']:CB['ident'] + 768] = np.tile(np.eye(128, dtype=np.float32), (1, 6))
    bo = np.zeros((128, 128), np.float32)
    bo[:64, :64] = 1.0
    bo[64:, 64:] = 1.0
    cb[:, CB['bones']:CB['bones'] + 128] = bo
    cb[:, CB['bmean']:CB['bmean'] + 128] = bo / 64.0
    cb[:, CB['ones']:CB['ones'] + 128] = 1.0
    half = 32
    theta = 10000.0 ** (-np.linspace(0.0, 1.0, half))
    i = p % 32
    ang = np.arange(T, dtype=np.float64)[None, :] * theta[i][:, None]
    ang32 = (np.arange(T, dtype=np.float32)[None, :] * theta.astype(np.float32)[i][:, None]).astype(np.float64)
    sign = np.where((p % 64) < 32, -1.0, 1.0)[:, None]
    rot = np.zeros((128, 4, T), np.float32)
    rot[:, 0] = np.cos(ang32)
    rot[:, 1] = np.sin(ang32) * sign
    rot[:, 2] = np.cos(ang32) * 0.125
    rot[:, 3] = np.sin(ang32) * sign * 0.125
    return cf, cb, rot


def make_inputs(bld, inp):
    nl = bld.nlayers
    wgu = np.zeros((nl * 2, 2, NFC, 128, 8 * 128), np.float32)
    wd = np.zeros((nl * 2, 8, 128, NFC * 128), np.float32)
    win = np.zeros((nl, NCH, 128, 8 * 128), np.float32)
    wout = np.zeros((nl, 8, 128, 8 * 128), np.float32)
    smw = np.zeros((nl, 128, 4 * 384), np.float32)
    vec = np.zeros((128, VLN * nl + 8), np.float32)
    for l in range(nl):
        for j, nm in enumerate(('ffn1', 'ffn2')):
            a, b = _prep_ffn(inp[nm + '_w_gate'][l], inp[nm + '_w_up'][l], inp[nm + '_w_down'][l])
            wgu[2 * l + j] = a
            wd[2 * l + j] = b
        w_in = inp['w_in'][l]
        for ci, (name, cols) in enumerate(CHUNKS):
            if cols == 'mv':
                if l == 0:
                    continue
                src = inp['rwkv_vres_w_in'][l - 1]
            else:
                src = w_in[:, cols]
            M = src.shape[1]
            img = np.zeros((8, 128, 128), np.float32)
            img[:, :, :M] = src.reshape(8, 128, M)
            win[l, ci] = img.transpose(1, 0, 2).reshape(128, 8 * 128)
        wo = inp['w_out'][l]
        wout[l] = wo.reshape(8, 128, 8, 128).transpose(2, 1, 0, 3).reshape(8, 128, 8 * 128)
        smw[l, 0:32, 0:384] = inp['rwkv_w2'][l]
        smw[l, 32:64, 0:384] = inp['rwkv_a2'][l]
        smw[l, 64:128, 0:384] = inp['rwkv_g2'][l]
        if l > 0:
            smw[l, 0:16, 384:768] = inp['rwkv_v2'][l - 1]
        wuk = inp['dsa_w_uk'][l]
        for h in range(6):
            j, e = h // 2, h % 2
            smw[l, e * 64:(e + 1) * 64, 768 + j * 128:768 + (j + 1) * 128] = wuk[h]
        wuv = inp['dsa_w_uv'][l]
        for h in range(6):
            smw[l, :, 1152 + h * 64:1152 + (h + 1) * 64] = wuv[h]
        o = VLN * l
        vec[:, o + VL['ffn1']:o + VL['ffn1'] + 8] = _col(inp['ffn1_norm'][l], 8)
        vec[:, o + VL['ffn2']:o + VL['ffn2'] + 8] = _col(inp['ffn2_norm'][l], 8)
        vec[:, o + VL['mix']:o + VL['mix'] + 8] = _col(inp['mix_norm'][l], 8)
        vec[:, o + VL['mu']:o + VL['mu'] + 10] = _col(inp['rwkv_mu'][l], 10)
        if l > 0:
            vec[:16, o + VL['mumv']] = inp['rwkv_vres_mu'][l - 1]
            vec[:, o + VL['v0']:o + VL['v0'] + 3] = _col(inp['rwkv_v0'][l - 1], 3)
        vec[:, o + VL['w0']:o + VL['w0'] + 3] = _col(inp['rwkv_w0'][l], 3)
        vec[:, o + VL['a0']:o + VL['a0'] + 3] = _col(inp['rwkv_a0'][l], 3)
        vec[:, o + VL['kk']:o + VL['kk'] + 3] = _col(inp['rwkv_k_k'][l], 3)
        vec[:, o + VL['ka']:o + VL['ka'] + 3] = _col(inp['rwkv_k_a'][l], 3)
        vec[:, o + VL['rk']:o + VL['rk'] + 3] = _col(inp['rwkv_r_k'][l].reshape(-1), 3)
        vec[:, o + VL['lnw']:o + VL['lnw'] + 3] = _col(inp['rwkv_ln_w'][l], 3)
        vec[:, o + VL['lnb']:o + VL['lnb'] + 3] = _col(inp['rwkv_ln_b'][l], 3)
        vec[:, o + VL['kvn']] = inp['dsa_kv_norm'][l]
    vec[:, VLN * nl:VLN * nl + 8] = _col(inp['final_norm'], 8)
    cf, cb, rot = _consts()
    return {'wgu': wgu, 'wd': wd, 'win': win, 'wout': wout, 'smw': smw, 'vec': vec, 'cf': cf, 'cb': cb, 'rot': rot}


def kernel(**inputs):
    inp = {k_: np.asarray(v) for k_, v in inputs.items()}
    bld = B()
    nc = bld.build()
    shared = make_inputs(bld, inp)
    x = inp['x']
    in_maps = []
    for b in range(8):
        m = dict(shared)
        m['xT'] = np.ascontiguousarray(x[b].T).reshape(8, 128, T)
        in_maps.append(m)
    res = run_bass_kernel_spmd(nc, in_maps, core_ids=list(range(8)))
    out = np.stack([np.ascontiguousarray(np.asarray(r['outT']).reshape(D, T).T) for r in res.results])
    return out.astype(np.float32)
```
